# Optimizing a Trainium2 kernel written in Bass

```python
import math
import jax, jax.numpy as jnp
from jax import lax
import numpy as np

D_MODEL = 1024
BATCH = 8
SEQ = 2048
DEPTH = 2

D_PLE = 256
EPS = 1e-6
ATT_HEADS = 8
ATT_HD = 64
ATT_VD = 2 * ATT_HD
ATT_W = ATT_HEADS * ATT_VD
Q_BLOCK = 128
DN_HEADS = 4
DN_HD = 128
DN_W = DN_HEADS * DN_HD
DN_CONV = 4
DN_CHUNK = 64
GLA_HEADS = 4
GLA_KD = 64
GLA_VD = 128
GLA_KW = GLA_HEADS * GLA_KD
GLA_W = GLA_HEADS * GLA_VD
GLA_RANK = 16
GLA_TAU = 16.0
GLA_CHUNK = 64
D_MIX = ATT_W + DN_W + GLA_W

IN_SPLITS = (ATT_W, ATT_W, ATT_W, ATT_W,
             DN_W, DN_W, DN_W, DN_W,
             DN_HEADS, DN_HEADS,
             GLA_KW, GLA_KW, GLA_W, GLA_W,
             GLA_RANK)
D_IN = sum(IN_SPLITS)
SPLIT_IDX = tuple(int(i) for i in np.cumsum(IN_SPLITS)[:-1])

kernel_name = "hybrid_diffattn_gdn_gla_parallel"


def rmsnorm(x, g):
    xf = x.astype(jnp.float32)
    y = xf * lax.rsqrt(jnp.mean(xf * xf, axis=-1, keepdims=True) + EPS)
    return (y * g.astype(jnp.float32)).astype(x.dtype)


def l2norm(x):
    xf = x.astype(jnp.float32)
    return xf * lax.rsqrt(jnp.sum(xf * xf, axis=-1, keepdims=True) + EPS)


def heads(x, n):
    b, s, w = x.shape
    return x.reshape(b, s, n, w // n).transpose(0, 2, 1, 3)


def merge(x):
    b, n, s, d = x.shape
    return x.transpose(0, 2, 1, 3).reshape(b, s, n * d)


def alibi_slopes(n):
    return 2.0 ** (-8.0 * jnp.arange(1, n + 1, dtype=jnp.float32) / n)


def causal_conv(x, w):
    k, c = w.shape
    return lax.conv_general_dilated(
        x, w[:, None, :].astype(x.dtype), window_strides=(1,), padding=[(k - 1, 0)],
        dimension_numbers=('NWC', 'WIO', 'NWC'), feature_group_count=c)


def diff_attention(q, k, v, lam):
    s = q.shape[3]
    scale = ATT_HD ** -0.5
    slopes = alibi_slopes(ATT_HEADS)[:, None, None, None]
    kf = k.astype(jnp.float32)
    vf = v.astype(jnp.float32)
    outs = []
    for start in range(0, s, Q_BLOCK):
        end = min(start + Q_BLOCK, s)
        qb = q[:, :, :, start:end].astype(jnp.float32)
        sc = jnp.einsum('bhmqd,bhmkd->bhmqk', qb, kf[:, :, :, :end]) * scale
        dist = (jnp.arange(start, end)[:, None] - jnp.arange(end)[None, :]).astype(jnp.float32)
        sc = jnp.where(dist >= 0, sc - slopes * dist, -jnp.inf)
        pr = jax.nn.softmax(sc, axis=-1)
        a = pr[:, :, 0] - lam * pr[:, :, 1]
        outs.append(jnp.einsum('bhqk,bhkd->bhqd', a, vf[:, :, :end]))
    return jnp.concatenate(outs, axis=2)


def gated_delta_rule(q, k, v, beta, g):
    b, h, s, dk = q.shape
    dv = v.shape[-1]
    c = DN_CHUNK
    n = s // c
    q = q * dk ** -0.5
    rs = lambda t: t.reshape(b, h, n, c, *t.shape[3:])
    q, k, v, beta, g = rs(q), rs(k), rs(v), rs(beta), rs(g)
    gc = jnp.cumsum(g, axis=-1)
    tri_incl = jnp.tril(jnp.ones((c, c), bool))
    tri_strict = jnp.tril(jnp.ones((c, c), bool), -1)
    decay = jnp.exp(jnp.where(tri_incl, gc[..., :, None] - gc[..., None, :], -jnp.inf))
    kb = k * beta[..., None]
    m = jnp.where(tri_strict, jnp.einsum('bhnid,bhnjd->bhnij', kb, k) * decay, 0.0)
    eye = jnp.eye(c, dtype=jnp.float32)
    t_inv = lax.linalg.triangular_solve(eye + m, jnp.broadcast_to(eye, m.shape),
                                        left_side=True, lower=True, unit_diagonal=True)
    u = t_inv @ (v * beta[..., None])
    w = t_inv @ (kb * jnp.exp(gc)[..., None])
    a_intra = jnp.einsum('bhnid,bhnjd->bhnij', q, k) * decay
    g_last = gc[..., -1]
    k_dec = k * jnp.exp(g_last[..., None] - gc)[..., None]
    q_dec = q * jnp.exp(gc)[..., None]

    def step(state, xs):
        w_c, u_c, q_c, k_c, a_c, gl = xs
        v_new = u_c - jnp.einsum('bhcd,bhde->bhce', w_c, state)
        o = jnp.einsum('bhcd,bhde->bhce', q_c, state) + jnp.einsum('bhij,bhje->bhie', a_c, v_new)
        state = state * jnp.exp(gl)[..., None, None] + jnp.einsum('bhcd,bhce->bhde', k_c, v_new)
        return state, o

    xs = (jnp.moveaxis(w, 2, 0), jnp.moveaxis(u, 2, 0), jnp.moveaxis(q_dec, 2, 0),
          jnp.moveaxis(k_dec, 2, 0), jnp.moveaxis(a_intra, 2, 0), jnp.moveaxis(g_last, 2, 0))
    s0 = jnp.zeros((b, h, dk, dv), jnp.float32)
    _, o = lax.scan(step, s0, xs)
    return jnp.moveaxis(o, 0, 2).reshape(b, h, s, dv)


def gla_chunked(q, k, v, gk):
    b, h, s, dk = q.shape
    dv = v.shape[-1]
    c = GLA_CHUNK
    n = s // c
    q = q * dk ** -0.5
    rs = lambda t: jnp.moveaxis(t.reshape(b, h, n, c, t.shape[-1]), 2, 0)
    q, k, v, bc = rs(q), rs(k), rs(v), rs(gk)
    bc = jnp.cumsum(bc, axis=3)
    tri = jnp.tril(jnp.ones((c, c), bool))[..., None]

    def step(state, xs):
        q_c, k_c, v_c, b_c = xs
        inter = jnp.einsum('bhcd,bhde->bhce', q_c * jnp.exp(b_c), state)
        rel = jnp.exp(jnp.where(tri, b_c[:, :, :, None, :] - b_c[:, :, None, :, :], -jnp.inf))
        att = jnp.einsum('bhid,bhjd,bhijd->bhij', q_c, k_c, rel)
        o = inter + jnp.einsum('bhij,bhje->bhie', att, v_c)
        b_last = b_c[:, :, -1]
        state = state * jnp.exp(b_last)[..., None] + jnp.einsum(
            'bhcd,bhce->bhde', k_c * jnp.exp(b_last[:, :, None] - b_c), v_c)
        return state, o

    s0 = jnp.zeros((b, h, dk, dv), jnp.float32)
    _, o = lax.scan(step, s0, (q, k, v, bc))
    return jnp.moveaxis(o, 0, 2).reshape(b, h, s, dv)


def hybrid_layer(x, p_l, w_in, w_out, pre_g, post_g, lq1, lk1, lq2, lk2, att_subln,
                 dn_conv, dn_a_log, dn_dt_bias, dn_norm, gla_w2, gla_b, gla_norm,
                 ple_proj, ple_gate, ple_norm, layer_idx):
    dt = x.dtype
    bsz, s, _ = x.shape
    h = rmsnorm(x, pre_g)
    proj = h @ w_in
    (a_q, a_k, a_v, a_z, d_q, d_k, d_v, d_z, d_b, d_a,
     g_q, g_k, g_v, g_z, g_r) = jnp.split(proj, SPLIT_IDX, axis=-1)

    split2 = lambda t: t.reshape(bsz, s, ATT_HEADS, 2, ATT_HD).transpose(0, 2, 3, 1, 4)
    lam_init = 0.8 - 0.6 * math.exp(-0.3 * layer_idx)
    lam = (jnp.exp(jnp.sum(lq1.astype(jnp.float32) * lk1.astype(jnp.float32)))
           - jnp.exp(jnp.sum(lq2.astype(jnp.float32) * lk2.astype(jnp.float32))) + lam_init)
    o_att = diff_attention(split2(a_q), split2(a_k), heads(a_v, ATT_HEADS), lam)
    o_att = rmsnorm(o_att, att_subln) * (1.0 - lam_init)
    y_att = merge(o_att).astype(dt) * jax.nn.silu(a_z)

    qkv = jax.nn.silu(causal_conv(jnp.concatenate([d_q, d_k, d_v], axis=-1), dn_conv))
    c_q, c_k, c_v = jnp.split(qkv, 3, axis=-1)
    beta = jax.nn.sigmoid(d_b.astype(jnp.float32)).transpose(0, 2, 1)
    g = (-jnp.exp(dn_a_log.astype(jnp.float32))
         * jax.nn.softplus(d_a.astype(jnp.float32) + dn_dt_bias.astype(jnp.float32))).transpose(0, 2, 1)
    o_dn = gated_delta_rule(l2norm(heads(c_q, DN_HEADS)), l2norm(heads(c_k, DN_HEADS)),
                            heads(c_v, DN_HEADS).astype(jnp.float32), beta, g)
    y_dn = merge(rmsnorm(o_dn, dn_norm)).astype(dt) * jax.nn.silu(d_z)

    gk = jax.nn.log_sigmoid((g_r @ gla_w2 + gla_b).astype(jnp.float32)) / GLA_TAU
    o_gla = gla_chunked(heads(g_q, GLA_HEADS).astype(jnp.float32),
                        heads(g_k, GLA_HEADS).astype(jnp.float32),
                        heads(g_v, GLA_HEADS).astype(jnp.float32),
                        heads(gk, GLA_HEADS))
    y_gla = merge(rmsnorm(o_gla, gla_norm)).astype(dt) * jax.nn.silu(g_z)

    y = jnp.concatenate([y_att, y_dn, y_gla], axis=-1) @ w_out
    x = x + rmsnorm(y, post_g)

    gate = jax.nn.sigmoid(x @ ple_gate)
    x = x + rmsnorm((p_l @ ple_proj) * gate, ple_norm)
    return x


def setup_inputs(seed: int = 0) -> dict:
    key = jax.random.key(seed)
    ks = jax.random.split(key, 24)
    nrm = lambda k, shape, scale: jax.random.normal(k, shape, jnp.float32) * scale
    gain = lambda k, shape: 1.0 + 0.02 * jax.random.normal(k, shape, jnp.float32)
    dt_init = jnp.exp(jax.random.uniform(ks[13], (DEPTH, DN_HEADS), jnp.float32,
                                         math.log(1e-3), math.log(1e-1)))
    return {
        "x": nrm(ks[0], (BATCH, SEQ, D_MODEL), 1.0),
        "p": nrm(ks[1], (DEPTH, BATCH, SEQ, D_PLE), 1.0),
        "w_in": nrm(ks[2], (DEPTH, D_MODEL, D_IN), D_MODEL ** -0.5),
        "w_out": nrm(ks[3], (DEPTH, D_MIX, D_MODEL), D_MIX ** -0.5),
        "pre_gain": gain(ks[4], (DEPTH, D_MODEL)),
        "post_gain": gain(ks[5], (DEPTH, D_MODEL)),
        "att_lq1": nrm(ks[6], (DEPTH, ATT_HD), 0.1),
        "att_lk1": nrm(ks[7], (DEPTH, ATT_HD), 0.1),
        "att_lq2": nrm(ks[8], (DEPTH, ATT_HD), 0.1),
        "att_lk2": nrm(ks[9], (DEPTH, ATT_HD), 0.1),
        "att_subln": gain(ks[10], (DEPTH, ATT_VD)),
        "dn_conv": nrm(ks[11], (DEPTH, DN_CONV, 3 * DN_W), DN_CONV ** -0.5),
        "dn_a_log": jnp.log(jax.random.uniform(ks[12], (DEPTH, DN_HEADS), jnp.float32, 1.0, 16.0)),
        "dn_dt_bias": dt_init + jnp.log(-jnp.expm1(-dt_init)),
        "dn_norm": gain(ks[14], (DEPTH, DN_HD)),
        "gla_w2": nrm(ks[15], (DEPTH, GLA_RANK, GLA_KW), GLA_RANK ** -0.5),
        "gla_b": nrm(ks[16], (DEPTH, GLA_KW), 0.01),
        "gla_norm": gain(ks[17], (DEPTH, GLA_VD)),
        "ple_proj": nrm(ks[18], (DEPTH, D_PLE, D_MODEL), D_PLE ** -0.5),
        "ple_gate": nrm(ks[19], (DEPTH, D_MODEL, D_MODEL), D_MODEL ** -0.5),
        "ple_norm": gain(ks[20], (DEPTH, D_MODEL)),
    }


def reference(x, p, w_in, w_out, pre_gain, post_gain, att_lq1, att_lk1, att_lq2, att_lk2,
              att_subln, dn_conv, dn_a_log, dn_dt_bias, dn_norm, gla_w2, gla_b, gla_norm,
              ple_proj, ple_gate, ple_norm):
    for i in range(DEPTH):
        x = hybrid_layer(x, p[i], w_in[i], w_out[i], pre_gain[i], post_gain[i],
                         att_lq1[i], att_lk1[i], att_lq2[i], att_lk2[i], att_subln[i],
                         dn_conv[i], dn_a_log[i], dn_dt_bias[i], dn_norm[i],
                         gla_w2[i], gla_b[i], gla_norm[i],
                         ple_proj[i], ple_gate[i], ple_norm[i], i)
    return x
```

```python
import math
from contextlib import ExitStack
import numpy as np
import concourse.bass as bass
import concourse.mybir as mybir
from concourse.bass_utils import run_bass_kernel_spmd

F32 = mybir.dt.float32
BF16 = mybir.dt.bfloat16
AF = mybir.ActivationFunctionType
ALU = mybir.AluOpType

S = 2048
D = 1024
NT = 16
DEPTH = 2
D_IN = 7704
EPS = 1e-6
O_AQ, O_AK, O_AV, O_AZ = 0, 1024, 2048, 3072
O_DQ, O_DK, O_DV, O_DZ, O_DB, O_DA = 4096, 4608, 5120, 5632, 6144, 6148
O_GQ, O_GK, O_GV, O_GZ, O_GR = 6152, 6408, 6664, 7176, 7688
C_ID, C_MATT, C_BDI, C_BDS, C_BLK, C_ALI, NC_CONST = 0, 128, 256, 384, 512, 640, 768
ATT_W = [128] * 8


class T:
    __slots__ = ("w", "r", "x")

    def __init__(self, x=False):
        self.w = None
        self.r = {}
        self.x = x


def TP():
    return T(True)


class Sched:
    def __init__(self, nc, es, n_dma_sems=8):
        self.nc = nc
        self.eng = {"pe": nc.tensor, "act": nc.scalar, "dve": nc.vector,
                    "pool": nc.gpsimd, "sp": nc.sync}
        self.sem = {}
        self.cnt = {}
        for k in self.eng:
            self.sem[k] = es.enter_context(nc.semaphore("s_" + k))
            self.cnt[k] = 0
        self.seen = {k: {} for k in self.eng}
        self.dq = {}
        for q in ("sp", "pool"):
            sems = []
            for i in range(n_dma_sems):
                key = "d_%s%d" % (q, i)
                self.sem[key] = es.enter_context(nc.semaphore(key))
                self.cnt[key] = 0
                sems.append(key)
            self.dq[q] = [sems, 0]
        self.n_instr = 0

    def _wait(self, e, ev):
        key, val = ev
        if self.seen[e].get(key, 0) >= val:
            return
        self.eng[e].wait_ge(self.sem[key], val)
        self.seen[e][key] = val

    def _deps(self, reads, writes):
        evs = {}
        for t in reads:
            if t.w is not None and evs.get(t.w[0], 0) < t.w[1]:
                evs[t.w[0]] = t.w[1]
        for t in writes:
            if t.w is not None and evs.get(t.w[0], 0) < t.w[1]:
                evs[t.w[0]] = t.w[1]
            for k, v in t.r.items():
                if evs.get(k, 0) < v:
                    evs[k] = v
        return evs

    def _commit(self, ev, reads, writes):
        k, v = ev
        for t in reads:
            if t.r.get(k, 0) < v:
                t.r[k] = v
        for t in writes:
            t.w = ev
            t.r = {}

    def op(self, e, fns, reads=(), writes=()):
        if callable(fns):
            fns = [fns]
        writes = list(writes) + [t for t in reads if t.x]
        reads = [t for t in reads if not t.x]
        for k, v in self._deps(reads, writes).items():
            self._wait(e, (k, v))
        h = self.eng[e]
        ins = None
        for f in fns:
            ins = f(h)
            self.n_instr += 1
        self.cnt[e] += 1
        ins.then_inc(self.sem[e], 1)
        ev = (e, self.cnt[e])
        self._commit(ev, reads, writes)
        return ev

    def dma(self, q, out, in_, reads=(), writes=(), **kw):
        sems, idx = self.dq[q]
        key = sems[idx]
        self.dq[q][1] = (idx + 1) % len(sems)
        if self.cnt[key] > 0:
            self._wait(q, (key, self.cnt[key]))
        for k, v in self._deps(reads, writes).items():
            self._wait(q, (k, v))
        ins = self.eng[q].dma_start(out=out, in_=in_, **kw)
        self.n_instr += 1
        self.cnt[key] += 16
        ins.then_inc(self.sem[key], 16)
        ev = (key, self.cnt[key])
        self._commit(ev, reads, writes)
        return ev

    def barrier(self):
        for e in self.eng:
            for k, v in self.cnt.items():
                if v > 0 and k != e:
                    self._wait(e, (k, v))

    def finish(self, e="sp"):
        for k, v in self.cnt.items():
            if v > 0 and k != e:
                self._wait(e, (k, v))


def make_consts():
    c = np.zeros((128, NC_CONST), np.float32)
    i = np.arange(128)
    c[:, C_ID:C_ID + 128] = np.eye(128, dtype=np.float32)
    c[:, C_MATT:C_MATT + 128] = (i[:, None] <= i[None, :]).astype(np.float32)
    same = (i[:, None] // 64) == (i[None, :] // 64)
    c[:, C_BDI:C_BDI + 128] = ((i[:, None] <= i[None, :]) & same).astype(np.float32)
    c[:, C_BDS:C_BDS + 128] = ((i[None, :] < i[:, None]) & same).astype(np.float32)
    c[:, C_BLK:C_BLK + 128] = same.astype(np.float32)
    slopes = 2.0 ** (-8.0 * np.arange(1, 9) / 8.0)
    for h in range(8):
        for dd in range(16):
            c[:, C_ALI + h * 16 + dd] = slopes[h] * (i - 127 - 128 * dd)
    return c


def build_program(depth=DEPTH, dbg=False, stop=None, skip=()):
    try:
        return _build_program(depth, dbg, stop, skip)
    except StopBuild as e:
        return e.nc


class StopBuild(Exception):
    def __init__(self, nc):
        self.nc = nc


def _build_program(depth=DEPTH, dbg=False, stop=None, skip=()):
    nc = bass.Bass("TRN2", target_bir_lowering=False)
    dr = lambda name, shape, kind="ExternalInput", dt=F32: nc.dram_tensor(name, shape, dt, kind=kind).ap()
    x_in = dr("x", [S, D])
    pT_in = dr("pT", [DEPTH, 256, S])
    w_in = dr("w_in", [DEPTH, D, D_IN])
    w_rep = dr("w_rep", [DEPTH, D, 512])
    w_out = dr("w_out", [DEPTH, 2048, D])
    ple_gate = dr("ple_gate", [DEPTH, D, D])
    ple_proj = dr("ple_proj", [DEPTH, 256, D])
    pre_gain = dr("pre_gain", [DEPTH, D])
    post_gain = dr("post_gain", [DEPTH, D])
    ple_norm = dr("ple_norm", [DEPTH, D])
    att_l = dr("att_l", [DEPTH, 4, 64])
    att_subln = dr("att_subln", [DEPTH, 128])
    dn_conv = dr("dn_conv", [DEPTH, 4, 1536])
    dn_a_log = dr("dn_a_log", [DEPTH, 4])
    dn_dt_bias = dr("dn_dt_bias", [DEPTH, 4])
    dn_norm = dr("dn_norm", [DEPTH, 128])
    gla_w2 = dr("gla_w2", [DEPTH, 16, 256])
    gla_b = dr("gla_b", [DEPTH, 256])
    gla_norm = dr("gla_norm", [DEPTH, 128])
    consts_in = dr("consts", [128, NC_CONST])
    out = dr("out", [S, D], kind="ExternalOutput")
    dbg_y = dr("dbg_y", [128, 16, S], kind="ExternalOutput") if dbg else None
    dbg_h = dr("dbg_h", [128, 8, S], kind="ExternalOutput") if dbg else None

    _uq = [0]


    def uq(name):
        _uq[0] += 1
        return "%s_%d" % (name, _uq[0])

    with ExitStack() as es:
        s = Sched(nc, es)
        sbp = lambda name, shape, dt: es.enter_context(nc.sbuf_tensor(uq(name), shape, dt))
        hT = sbp("hT", [128, 8, S], BF16)
        t_hT = T()
        yT = sbp("yT", [128, 16, S], BF16)
        t_yT = [T() for _ in range(16)]
        cst = sbp("cst", [128, NC_CONST], F32)
        t_cst = T()
        identb = sbp("identb", [128, 128], BF16)
        ident4 = sbp("ident4", [128, 4, 128], BF16)
        matt2 = sbp("matt2", [128, 2, 128], BF16)
        bdi4 = sbp("bdi4", [128, 4, 128], BF16)
        bds4 = sbp("bds4", [128, 4, 128], F32)
        ones_f = sbp("ones_f", [128, 128], F32)
        scanmask = sbp("scanmask", [128, S], F32)
        gb0 = sbp("gb0", [128, D], F32)
        t_gb0 = T()
        gb1 = sbp("gb1", [128, D], F32)
        t_gb1 = T()
        small = sbp("small", [128, 64], F32)
        t_small = T()

        ident = cst[:, C_ID:C_ID + 128]
        s.dma("sp", cst[:], consts_in, writes=[t_cst])
        s.op("dve", lambda v: v.tensor_copy(out=identb[:], in_=ident), [t_cst], [t_cst])
        for u in range(4):
            s.op("dve", lambda v, u=u: v.tensor_copy(out=ident4[:, u, :], in_=ident), [t_cst], [t_cst])
            s.op("dve", lambda v, u=u: v.tensor_copy(out=bdi4[:, u, :], in_=cst[:, C_BDI:C_BDI + 128]), [t_cst], [t_cst])
            s.op("dve", lambda v, u=u: v.tensor_copy(out=bds4[:, u, :], in_=cst[:, C_BDS:C_BDS + 128]), [t_cst], [t_cst])
        for u in range(2):
            s.op("dve", lambda v, u=u: v.tensor_copy(out=matt2[:, u, :], in_=cst[:, C_MATT:C_MATT + 128]), [t_cst], [t_cst])
        s.op("pool", lambda g: g.memset(ones_f[:], 1.0), [], [t_cst])
        s.op("pool", lambda g: g.memset(scanmask[:], 1.0), [], [t_cst])
        s.op("pool", lambda g: g.memset(scanmask[:].rearrange("p (c k) -> p c k", k=64)[:, :, 0:1], 0.0), [], [t_cst])

        def chk(name):
            if stop == name:
                s.finish("sp")
                print("STOP at", name, "instructions:", s.n_instr)
                raise StopBuild(nc)

        def load_w(dst, src2d, tw):
            src = src2d.rearrange("(k p) n -> p k n", p=128)
            nk = src.shape[1]
            per = max(1, 2048 // max(1, src.shape[2] * 4 // 512))
            per = min(per, nk)
            if src.shape[2] >= 1024:
                per = 1
            for k0 in range(0, nk, per):
                s.dma("pool", dst[:, k0:k0 + per], src[:, k0:k0 + per], writes=[tw])

        def finalize_tile(ph, o_ap, t_o, gaincol, szT_ap, t_sz, mix, t, extra_reads=()):
            junk, t_junk, st, t_st, an, t_an, psT, t_psT = ph["fin"]
            i = ph["fin_i"] = ph.get("fin_i", 0) + 1
            b = i % 2
            s.op("act", lambda a: a.activation(out=junk[:, b, :], in_=o_ap, func=AF.Square, accum_out=st[:, b, 0:1]),
                 [t_o], [t_junk[b], t_st[b]])
            s.op("act", lambda a: a.activation(out=st[:, b, 1:2], in_=st[:, b, 0:1], func=AF.Sqrt, bias=EPS, scale=1.0 / 128.0),
                 [t_st[b]], [t_st[b]])
            s.op("dve", lambda v: v.reciprocal(out=st[:, b, 2:3], in_=st[:, b, 1:2]), [t_st[b]], [t_st[b]])
            s.op("dve", lambda v: v.tensor_scalar(out=an[:, b, :], in0=o_ap, scalar1=st[:, b, 2:3], scalar2=None, op0=ALU.mult),
                 [t_o, t_st[b]], [t_an[b]])
            s.op("pe", lambda pe: pe.transpose(out=psT[:, b * 128:(b + 1) * 128], in_=an[:, b, :], identity=identb[:]),
                 [t_an[b], t_cst], [t_psT[b]])
            s.op("dve", lambda v: v.scalar_tensor_tensor(out=yT[:, mix, t * 128:(t + 1) * 128], in0=psT[:, b * 128:(b + 1) * 128],
                                                          scalar=gaincol, in1=szT_ap, op0=ALU.mult, op1=ALU.mult),
                 [t_psT[b], t_sz] + list(extra_reads), [t_yT[mix]])

        def alloc_fin(pes, ph):
            sb = lambda name, shape, dt: pes.enter_context(nc.sbuf_tensor(uq(name), shape, dt))
            junk = sb("fjunk", [128, 2, 128], BF16)
            st = sb("fst", [128, 2, 4], F32)
            an = sb("fan", [128, 2, 128], BF16)
            psT = pes.enter_context(nc.psum_tensor(uq("fpsT"), [128, 1024], BF16))
            _tp = TP()
            ph["fin"] = (junk, [T(), T()], st, [T(), T()], an, [T(), T()], psT, [_tp, _tp])

        x_src = x_in
        for l in range(depth):
            lam_init = 0.8 - 0.6 * math.exp(-0.3 * l)
            with ExitStack() as pes:
                sb = lambda name, shape, dt: pes.enter_context(nc.sbuf_tensor(uq(name), shape, dt))
                xb_ = sb("p1x", [128, 2, D], F32)
                t_x = [T(), T()]
                hb = sb("p1h", [128, 2, D], BF16)
                t_hb = [T(), T()]
                junk = sb("p1j", [128, D], BF16)
                t_j = T()
                st = sb("p1s", [128, 2, 4], F32)
                t_st = [T(), T()]
                psT = pes.enter_context(nc.psum_tensor(uq("p1ps"), [128, 2, 1024], BF16))
                t_ps = [TP(), TP()]
                s.dma("sp", gb0[:], pre_gain[l:l + 1, :].partition_broadcast(128), writes=[t_gb0])
                for t in range(NT):
                    b = t % 2
                    s.dma("sp", xb_[:, b, :], x_src[t * 128:(t + 1) * 128, :], writes=[t_x[b]])
                    s.op("act", lambda a: a.activation(out=junk[:], in_=xb_[:, b, :], func=AF.Square, accum_out=st[:, b, 0:1]),
                         [t_x[b]], [t_j, t_st[b]])
                    s.op("act", lambda a: a.activation(out=st[:, b, 1:2], in_=st[:, b, 0:1], func=AF.Sqrt, bias=EPS, scale=1.0 / D),
                         [t_st[b]], [t_st[b]])
                    s.op("dve", lambda v: v.reciprocal(out=st[:, b, 2:3], in_=st[:, b, 1:2]), [t_st[b]], [t_st[b]])
                    s.op("dve", lambda v: v.scalar_tensor_tensor(out=hb[:, b, :], in0=xb_[:, b, :], scalar=st[:, b, 2:3], in1=gb0[:],
                                                                  op0=ALU.mult, op1=ALU.mult),
                         [t_x[b], t_st[b], t_gb0], [t_hb[b]])
                    s.op("pe", [lambda pe, k=k: pe.transpose(out=psT[:, b, k * 128:(k + 1) * 128], in_=hb[:, b, k * 128:(k + 1) * 128],
                                                             identity=identb[:]) for k in range(8)],
                         [t_hb[b], t_cst], [t_ps[b]])
                    s.op("act", lambda a: a.copy(out=hT[:, :, t * 128:(t + 1) * 128],
                                                 in_=psT[:, b, :].rearrange("p (k n) -> p k n", k=8)),
                         [t_ps[b]], [t_hT])
                s.barrier()

            if stop == "p1":
                if dbg:
                    for c in range(16):
                        s.dma("pool", dbg_y[:, c, :], yT[:, c, :], reads=[t_yT[c]])
                    for k in range(8):
                        s.dma("pool", dbg_h[:, k, :], hT[:, k, :], reads=[t_hT])
                s.finish("sp")
                print("instructions:", s.n_instr)
                return nc
            with ExitStack() as pes:
                sb = lambda name, shape, dt: pes.enter_context(nc.sbuf_tensor(uq(name), shape, dt))
                ps = lambda name, shape, dt: pes.enter_context(nc.psum_tensor(uq(name), shape, dt))
                ph = {}
                alloc_fin(pes, ph)
                wbuf = sb("a_w", [128, 2, 4, 8, 128], BF16)
                t_w = [[T() for _ in range(4)] for _ in range(2)]
                qT = sb("a_qT", [128, S], BF16)
                t_q = T()
                kT = sb("a_kT", [128, S], BF16)
                t_k = T()
                szT = sb("a_szT", [128, S], BF16)
                t_sz = T()
                vaug = sb("a_v", [128, NT, 130], BF16)
                t_v = T()
                pt = sb("a_p", [128, 3, 2, 128], BF16)
                t_pt = [T(), T(), T()]
                osb = sb("a_o", [128, 2, 2, 128], F32)
                t_osb = [T(), T()]
                rr = sb("a_rr", [128, 2, 4], F32)
                t_rr = [T(), T()]
                lq = sb("a_lq", [128, 4, 64], F32)
                t_lq = T()
                gcol = sb("a_gc", [128, 2], F32)
                t_gc = T()
                _psA = ps("a_psA", [128, 512], F32)
                psA = [_psA, _psA]
                _tA = TP()
                t_psA = [_tA, _tA]
                psS = [ps("a_psS%d" % i, [128, 2, 512], F32) for i in range(2)]
                t_psS = [TP(), TP()]
                psO = [ps("a_psO%d" % i, [128, 2, 256], F32) for i in range(2)]
                t_psO = [TP(), TP()]

                s.dma("sp", lq[:], att_l[l:l + 1].partition_broadcast(128), writes=[t_lq])
                s.op("dve", lambda v: v.tensor_tensor(out=lq[:, 0, :], in0=lq[:, 0, :], in1=lq[:, 1, :], op=ALU.mult), [t_lq], [t_lq])
                s.op("dve", lambda v: v.tensor_tensor(out=lq[:, 2, :], in0=lq[:, 2, :], in1=lq[:, 3, :], op=ALU.mult), [t_lq], [t_lq])
                s.op("act", lambda a: a.activation(out=lq[:, 1, :], in_=lq[:, 0, :], func=AF.Copy, accum_out=small[:, 1:2]), [t_lq], [t_lq, t_small])
                s.op("act", lambda a: a.activation(out=lq[:, 3, :], in_=lq[:, 2, :], func=AF.Copy, accum_out=small[:, 2:3]), [t_lq], [t_lq, t_small])
                s.op("act", lambda a: a.activation(out=small[:, 3:5], in_=small[:, 1:3], func=AF.Exp), [t_small], [t_small])
                s.op("dve", lambda v: v.tensor_tensor(out=small[:, 5:6], in0=small[:, 4:5], in1=small[:, 3:4], op=ALU.subtract), [t_small], [t_small])
                s.op("dve", lambda v: v.tensor_scalar(out=small[:, 0:1], in0=small[:, 5:6], scalar1=-lam_init, scalar2=None, op0=ALU.add), [t_small], [t_small])
                s.dma("sp", gcol[:, 0:1], att_subln[l:l + 1, :].rearrange("o c -> c o"), writes=[t_gc], allow_slow_non_contiguous=True)
                s.op("dve", lambda v: v.tensor_scalar(out=gcol[:, 1:2], in0=gcol[:, 0:1], scalar1=1.0 - lam_init, scalar2=None, op0=ALU.mult), [t_gc], [t_gc])
                s.op("pool", lambda g: g.memset(vaug[:, :, 128:130], 1.0), [], [t_v])
                pA = [0]

                def proj_fm(wt, tw, evac):
                    for tc in range(4):
                        b = pA[0] % 2
                        pA[0] += 1
                        s.op("pe", [lambda pe, k=k: pe.matmul(psA[b][:], lhsT=wt[:, k, :], rhs=hT[:, k, tc * 512:(tc + 1) * 512],
                                                              start=(k == 0), stop=(k == 7)) for k in range(8)],
                             [tw, t_hT], [t_psA[b]])
                        evac(psA[b], t_psA[b], tc)

                offs = [O_AQ, O_AK, O_AV, O_AZ]
                for h in range(0 if "p3" not in skip else 8, 8):
                    wb = h % 2
                    for j in range(4):
                        load_w(wbuf[:, wb, j], w_in[l][:, offs[j] + h * 128: offs[j] + (h + 1) * 128], t_w[wb][j])
                    proj_fm(wbuf[:, wb, 0], t_w[wb][0],
                            lambda p, tp, tc: s.op("act", lambda a: a.copy(out=qT[:, tc * 512:(tc + 1) * 512], in_=p[:]), [tp], [t_q]))
                    proj_fm(wbuf[:, wb, 1], t_w[wb][1],
                            lambda p, tp, tc: s.op("dve", lambda v: v.tensor_copy(out=kT[:, tc * 512:(tc + 1) * 512], in_=p[:]), [tp], [t_k]))
                    proj_fm(wbuf[:, wb, 3], t_w[wb][3],
                            lambda p, tp, tc: s.op("act", lambda a: a.activation(out=szT[:, tc * 512:(tc + 1) * 512], in_=p[:], func=AF.Silu), [tp], [t_sz]))
                    for tg in range(4):
                        b = pA[0] % 2
                        pA[0] += 1
                        fns = []
                        for u in range(4):
                            t = tg * 4 + u
                            for k in range(8):
                                fns.append(lambda pe, k=k, t=t, u=u: pe.matmul(psA[b][:, u * 128:(u + 1) * 128], lhsT=hT[:, k, t * 128:(t + 1) * 128],
                                                                              rhs=wbuf[:, wb, 2, k, :], start=(k == 0), stop=(k == 7)))
                        s.op("pe", fns, [t_w[wb][2], t_hT], [t_psA[b]])
                        s.op("dve", lambda v: v.tensor_copy(out=vaug[:, tg * 4:(tg + 1) * 4, 0:128],
                                                            in_=psA[b][:].rearrange("p (u n) -> p u n", u=4)), [t_psA[b]], [t_v])
                    pi = 0
                    for t in range(NT):
                        ob = t % 2
                        for c in range(t + 1):
                            sbk = pi % 2
                            pb = pi % 3
                            pi += 1
                            dd = t - c
                            s.op("pe", [lambda pe, m=m: pe.matmul(psS[sbk][:, m, 0:128], lhsT=kT[m * 64:(m + 1) * 64, c * 128:(c + 1) * 128],
                                                                  rhs=qT[m * 64:(m + 1) * 64, t * 128:(t + 1) * 128], start=True, stop=True)
                                        for m in range(2)],
                                 [t_k, t_q], [t_psS[sbk]])
                            bcol = cst[:, C_ALI + h * 16 + dd: C_ALI + h * 16 + dd + 1]
                            s.op("act", lambda a: a.activation(out=pt[:, pb, :, :], in_=psS[sbk][:, :, 0:128], func=AF.Exp, bias=bcol, scale=0.125),
                                 [t_psS[sbk], t_cst], [t_pt[pb]])
                            if c == t:
                                s.op("pool", lambda g: g.tensor_tensor(out=pt[:, pb, :, :], in0=pt[:, pb, :, :], in1=matt2[:], op=ALU.mult),
                                     [t_pt[pb], t_cst], [t_pt[pb]])
                            s.op("pe", [lambda pe, m=m: pe.matmul(psO[ob][:, m, 0:129], lhsT=pt[:, pb, m, :], rhs=vaug[:, c, 0:129],
                                                                  start=(c == 0 and m == 0), stop=(c == t)) for m in range(2)],
                                 [t_pt[pb], t_v], [t_psO[ob]])
                        s.op("dve", lambda v: v.reciprocal(out=rr[:, ob, 0:1], in_=psO[ob][:, 0, 128:129]), [t_psO[ob]], [t_rr[ob]])
                        s.op("dve", lambda v: v.reciprocal(out=rr[:, ob, 1:2], in_=psO[ob][:, 1, 128:129]), [t_psO[ob]], [t_rr[ob]])
                        s.op("dve", lambda v: v.tensor_tensor(out=rr[:, ob, 2:3], in0=rr[:, ob, 1:2], in1=small[:, 0:1], op=ALU.mult),
                             [t_rr[ob], t_small], [t_rr[ob]])
                        s.op("dve", lambda v: v.tensor_scalar(out=osb[:, ob, 1, :], in0=psO[ob][:, 1, 0:128], scalar1=rr[:, ob, 2:3], scalar2=None, op0=ALU.mult),
                             [t_psO[ob], t_rr[ob]], [t_osb[ob]])
                        s.op("dve", lambda v: v.scalar_tensor_tensor(out=osb[:, ob, 0, :], in0=psO[ob][:, 0, 0:128], scalar=rr[:, ob, 0:1],
                                                                      in1=osb[:, ob, 1, :], op0=ALU.mult, op1=ALU.add),
                             [t_psO[ob], t_rr[ob], t_osb[ob]], [t_osb[ob]])
                        finalize_tile(ph, osb[:, ob, 0, :], t_osb[ob], gcol[:, 1:2], szT[:, t * 128:(t + 1) * 128], t_sz, h, t, [t_gc])
                s.barrier()

            if stop == "p3":
                if dbg:
                    for c in range(16):
                        s.dma("pool", dbg_y[:, c, :], yT[:, c, :], reads=[t_yT[c]])
                    for k in range(8):
                        s.dma("pool", dbg_h[:, k, :], hT[:, k, :], reads=[t_hT])
                s.finish("sp")
                print("instructions:", s.n_instr)
                return nc
            with ExitStack() as pes:
                sb = lambda name, shape, dt: pes.enter_context(nc.sbuf_tensor(uq(name), shape, dt))
                ps = lambda name, shape, dt: pes.enter_context(nc.psum_tensor(uq(name), shape, dt))
                ph = {}
                alloc_fin(pes, ph)
                psA = [ps("d_psA%d" % i, [128, 512], F32) for i in range(2)]
                t_psA = [TP(), TP()]
                psB = [ps("d_psB%d" % i, [128, 512], F32) for i in range(2)]
                t_psB = [TP(), TP()]
                psTr = ps("d_psT", [128, 1024], BF16)
                t_psTr = TP()
                psV = ps("d_psV", [128, 4, 128], F32)
                _tv = TP()
                t_psV = [_tv, _tv, _tv, _tv]
                pA = [0]

                wba = sb("d_wba", [128, 8, 8], BF16)
                t_wba = T()
                tok = sb("d_tok", [128, 8, 64], F32)
                t_tok = T()
                prm = sb("d_prm", [128, 16], F32)
                t_prm = T()
                gcolD = sb("d_gcol", [128, 1], F32)
                t_gcD = T()
                cw = sb("d_cw", [128, 3, 4], F32)
                t_cw = T()
                wd = sb("d_w", [128, 5, 8, 128], BF16)
                t_wd = [T() for _ in range(5)]
                raw = sb("d_raw", [128, S + 3], F32)
                t_raw = T()
                cv = sb("d_cv", [128, S], F32)
                t_cv = T()
                qnT = sb("d_qnT", [128, S], BF16)
                knT = sb("d_knT", [128, S], BF16)
                vcT = sb("d_vcT", [128, S], BF16)
                qdT = sb("d_qdT", [128, S], BF16)
                t_qn, t_kn, t_vc, t_qd = T(), T(), T(), T()
                szT = sb("d_szT", [128, S], BF16)
                t_sz = T()
                gcr = sb("d_gcr", [128, S], F32)
                t_gcr = T()
                egl = sb("d_egl", [128, 32], F32)
                t_egl = T()
                sd = sb("d_sd", [128, 2, 512], F32)
                t_sd = [T(), T()]
                tmpD = sb("d_tmpD", [128, 2, 4, 128], F32)
                t_tmpD = T()
                X = sb("d_X", [128, 2, 4, 128], BF16)
                Y = sb("d_Y", [128, 2, 4, 128], BF16)
                R = sb("d_R", [128, 2, 4, 128], BF16)
                t_X, t_Y, t_R = [T(), T()], [T(), T()], [T(), T()]
                aT = sb("d_aT", [128, 4, 128], BF16)
                t_aT = T()
                kbg = sb("d_kbg", [128, 4, 128], BF16)
                kdec = sb("d_kdec", [128, 4, 128], BF16)
                vb = sb("d_vb", [128, 4, 128], BF16)
                t_kbg, t_kdec, t_vb = T(), T(), T()
                usb = sb("d_u", [128, 4, 128], F32)
                t_u = T()
                wT = sb("d_wT", [128, 4, 128], BF16)
                t_wT = T()
                vnew = sb("d_vnew", [128, 128], BF16)
                t_vnew = T()
                St = sb("d_S", [128, 128], F32)
                Sb = sb("d_Sb", [128, 128], BF16)
                Se = sb("d_Se", [128, 128], F32)
                t_S, t_Sb, t_Se = T(), T(), T()
                osb = sb("d_o", [128, 2, 128], F32)
                t_osb = [T(), T()]

                def proj_fm(wt, tw, evac):
                    for tc in range(4):
                        b = pA[0] % 2
                        pA[0] += 1
                        s.op("pe", [lambda pe, k=k: pe.matmul(psA[b][:], lhsT=wt[:, k, :], rhs=hT[:, k, tc * 512:(tc + 1) * 512],
                                                              start=(k == 0), stop=(k == 7)) for k in range(8)],
                             [tw, t_hT], [t_psA[b]])
                        evac(psA[b], t_psA[b], tc)

                s.dma("pool", wba[:], w_in[l][:, O_DB:O_DB + 8].rearrange("(k p) n -> p k n", p=128), writes=[t_wba])
                s.dma("sp", prm[:, 0:4], dn_a_log[l:l + 1, :].partition_broadcast(128), writes=[t_prm])
                s.dma("sp", prm[:, 4:8], dn_dt_bias[l:l + 1, :].partition_broadcast(128), writes=[t_prm])
                s.op("act", lambda a: a.activation(out=prm[:, 8:12], in_=prm[:, 0:4], func=AF.Exp), [t_prm], [t_prm])
                s.op("dve", lambda v: v.tensor_scalar(out=prm[:, 8:12], in0=prm[:, 8:12], scalar1=-1.0, scalar2=None, op0=ALU.mult), [t_prm], [t_prm])
                s.dma("sp", gcolD[:, 0:1], dn_norm[l:l + 1, :].rearrange("o c -> c o"), writes=[t_gcD], allow_slow_non_contiguous=True)
                fns = []
                for t in range(NT):
                    for k in range(8):
                        fns.append(lambda pe, k=k, t=t: pe.matmul(psA[0][:, t * 8:(t + 1) * 8], lhsT=hT[:, k, t * 128:(t + 1) * 128],
                                                                  rhs=wba[:, k, :], start=(k == 0), stop=(k == 7)))
                s.op("pe", fns, [t_wba, t_hT], [t_psA[0]])
                pA[0] = 1
                ba = psA[0][:, 0:128].rearrange("p (t c) -> p t c", c=8)
                tk = lambda i: tok[:, i, :].rearrange("p (t c) -> p t c", c=4)
                s.op("act", lambda a: a.activation(out=tk(1), in_=ba[:, :, 0:4], func=AF.Sigmoid), [t_psA[0]], [t_tok])
                for hh in range(4):
                    s.op("act", lambda a, hh=hh: a.activation(out=tk(7)[:, :, hh], in_=ba[:, :, 4 + hh], func=AF.Exp, bias=prm[:, 4 + hh:5 + hh]),
                         [t_psA[0], t_prm], [t_tok])
                s.op("act", lambda a: a.activation(out=tok[:, 7, :], in_=tok[:, 7, :], func=AF.Ln, bias=1.0), [t_tok], [t_tok])
                for hh in range(4):
                    s.op("dve", lambda v, hh=hh: v.tensor_scalar(out=tk(2)[:, :, hh], in0=tk(7)[:, :, hh], scalar1=prm[:, 8 + hh:9 + hh], scalar2=None, op0=ALU.mult),
                         [t_tok, t_prm], [t_tok])
                s.op("pe", lambda pe: pe.matmul(psA[1][:, 0:64], lhsT=cst[:, C_BDI:C_BDI + 128], rhs=tok[:, 2, :], start=True, stop=True),
                     [t_tok, t_cst], [t_psA[1]])
                s.op("pe", lambda pe: pe.matmul(psA[1][:, 64:128], lhsT=cst[:, C_BLK:C_BLK + 128], rhs=tok[:, 2, :], start=True, stop=True),
                     [t_tok, t_cst], [t_psA[1]])
                s.op("dve", lambda v: v.tensor_copy(out=tok[:, 3, :], in_=psA[1][:, 0:64]), [t_psA[1]], [t_tok])
                s.op("dve", lambda v: v.tensor_copy(out=tok[:, 4, :], in_=psA[1][:, 64:128]), [t_psA[1]], [t_tok])
                s.op("act", lambda a: a.activation(out=tok[:, 5, :], in_=tok[:, 3, :], func=AF.Exp), [t_tok], [t_tok])
                s.op("dve", lambda v: v.tensor_tensor(out=tok[:, 5, :], in0=tok[:, 5, :], in1=tok[:, 1, :], op=ALU.mult), [t_tok], [t_tok])
                s.op("dve", lambda v: v.tensor_tensor(out=tok[:, 6, :], in0=tok[:, 4, :], in1=tok[:, 3, :], op=ALU.subtract), [t_tok], [t_tok])
                s.op("act", lambda a: a.activation(out=tok[:, 6, :], in_=tok[:, 6, :], func=AF.Exp), [t_tok], [t_tok])
                s.op("pool", lambda g: g.memset(raw[:, 0:3], 0.0), [], [t_raw])
                s.op("pool", lambda g: g.memset(vnew[:], 0.0), [], [t_vnew])
                col = lambda plane, t, hh: tok[:, plane, t * 4 + hh: t * 4 + hh + 1]
                chk("p4a")

                for h in range(0 if "p4" not in skip else 4, 4):
                    offs = [O_DQ, O_DK, O_DV, O_DZ]
                    for j in range(4):
                        load_w(wd[:, j], w_in[l][:, offs[j] + h * 128: offs[j] + (h + 1) * 128], t_wd[j])
                    load_w(wd[:, 4], w_rep[l][:, h * 128:(h + 1) * 128], t_wd[4])
                    for j in range(3):
                        s.dma("sp", cw[:, j, :], dn_conv[l][:, j * 512 + h * 128: j * 512 + (h + 1) * 128].rearrange("i c -> c i"),
                              writes=[t_cw], allow_slow_non_contiguous=True)
                    proj_fm(wd[:, 3], t_wd[3],
                            lambda p, tp, tc: s.op("act", lambda a: a.activation(out=szT[:, tc * 512:(tc + 1) * 512], in_=p[:], func=AF.Silu), [tp], [t_sz]))
                    proj_fm(wd[:, 4], t_wd[4],
                            lambda p, tp, tc: s.op("act", lambda a: a.activation(out=gcr[:, tc * 512:(tc + 1) * 512], in_=p[:], func=AF.Exp, bias=prm[:, 4 + h:5 + h]),
                                                   [tp, t_prm], [t_gcr]))
                    s.op("act", lambda a: a.activation(out=gcr[:], in_=gcr[:], func=AF.Ln, bias=1.0), [t_gcr], [t_gcr])
                    s.op("dve", lambda v: v.tensor_scalar(out=gcr[:], in0=gcr[:], scalar1=prm[:, 8 + h:9 + h], scalar2=None, op0=ALU.mult), [t_gcr, t_prm], [t_gcr])
                    s.op("dve", lambda v: v.tensor_tensor_scan(out=gcr[:], data0=scanmask[:], data1=gcr[:], initial=0.0, op0=ALU.mult, op1=ALU.add),
                         [t_gcr, t_cst], [t_gcr])
                    s.op("act", lambda a: a.activation(out=egl[:], in_=gcr[:].rearrange("p (n c) -> p n c", c=64)[:, :, 63], func=AF.Exp), [t_gcr], [t_egl])
                    for j in range(3):
                        proj_fm(wd[:, j], t_wd[j],
                                lambda p, tp, tc: s.op("act", lambda a: a.copy(out=raw[:, 3 + tc * 512: 3 + (tc + 1) * 512], in_=p[:]), [tp], [t_raw]))
                        s.op("dve", lambda v: v.tensor_scalar(out=cv[:], in0=raw[:, 3:S + 3], scalar1=cw[:, j, 3:4], scalar2=None, op0=ALU.mult),
                             [t_raw, t_cw], [t_cv])
                        for i in range(3):
                            s.op("dve", lambda v: v.scalar_tensor_tensor(out=cv[:], in0=raw[:, i:S + i], scalar=cw[:, j, i:i + 1], in1=cv[:],
                                                                          op0=ALU.mult, op1=ALU.add),
                                 [t_raw, t_cw, t_cv], [t_cv])
                        s.op("act", lambda a: a.activation(out=cv[:], in_=cv[:], func=AF.Silu), [t_cv], [t_cv])
                        if j == 2:
                            s.op("act", lambda a: a.copy(out=vcT[:], in_=cv[:]), [t_cv], [t_vc])
                            continue
                        s.op("pool", lambda g: g.tensor_tensor(out=raw[:, 3:S + 3], in0=cv[:], in1=cv[:], op=ALU.mult), [t_cv], [t_raw])
                        for tc in range(4):
                            b = pA[0] % 2
                            pA[0] += 1
                            s.op("pe", lambda pe: pe.matmul(psA[b][:], lhsT=ones_f[:], rhs=raw[:, 3 + tc * 512: 3 + (tc + 1) * 512], start=True, stop=True),
                                 [t_raw, t_cst], [t_psA[b]])
                            s.op("act", lambda a: a.activation(out=sd[:, b, :], in_=psA[b][:], func=AF.Sqrt, bias=EPS, scale=1.0), [t_psA[b]], [t_sd[b]])
                            s.op("dve", lambda v: v.reciprocal(out=sd[:, b, :], in_=sd[:, b, :]), [t_sd[b]], [t_sd[b]])
                            dst, td = (qnT, t_qn) if j == 0 else (knT, t_kn)
                            sc = 128.0 ** -0.5 if j == 0 else 1.0
                            s.op("dve", lambda v: v.scalar_tensor_tensor(out=dst[:, tc * 512:(tc + 1) * 512], in0=cv[:, tc * 512:(tc + 1) * 512], scalar=sc,
                                                                          in1=sd[:, b, :], op0=ALU.mult, op1=ALU.mult),
                                 [t_cv, t_sd[b]], [td])
                    s.op("act", lambda a: a.activation(out=cv[:], in_=gcr[:], func=AF.Exp), [t_gcr], [t_cv])
                    s.op("dve", lambda v: v.tensor_tensor(out=qdT[:], in0=qnT[:], in1=cv[:], op=ALU.mult), [t_qn, t_cv], [t_qd])
                    s.op("dve", lambda v: v.memset(St[:], 0.0), [], [t_S])
                    s.op("dve", lambda v: v.memset(Sb[:], 0.0), [], [t_Sb])
                    chk("p4b")

                    for tg in range(4):
                        tiles = [tg * 4 + u for u in range(4)]
                        sl = lambda t: slice(t * 128, (t + 1) * 128)
                        bA = pA[0] % 2
                        pA[0] += 1
                        s.op("pe", [lambda pe, u=u, t=t: pe.matmul(psA[bA][:, u * 128:(u + 1) * 128], lhsT=knT[:, sl(t)], rhs=knT[:, sl(t)], start=True, stop=True)
                                    for u, t in enumerate(tiles)], [t_kn], [t_psA[bA]])
                        s.op("pe", [lambda pe, u=u, t=t: pe.matmul(psB[0][:, u * 128:(u + 1) * 128], lhsT=knT[:, sl(t)], rhs=qnT[:, sl(t)], start=True, stop=True)
                                    for u, t in enumerate(tiles)], [t_kn, t_qn], [t_psB[0]])
                        for u, t in enumerate(tiles):
                            s.op("dve", lambda v, u=u, t=t: v.tensor_scalar(out=tmpD[:, 1, u, :], in0=gcr[:, sl(t)], scalar1=col(3, t, h), scalar2=None,
                                                                             op0=ALU.subtract), [t_gcr, t_tok], [t_tmpD])
                        s.op("dve", lambda v: v.tensor_scalar(out=tmpD[:, 0], in0=tmpD[:, 1], scalar1=0.0, scalar2=None, op0=ALU.max), [t_tmpD], [t_tmpD])
                        s.op("dve", lambda v: v.tensor_scalar(out=tmpD[:, 1], in0=tmpD[:, 1], scalar1=0.0, scalar2=None, op0=ALU.min), [t_tmpD], [t_tmpD])
                        s.op("act", lambda a: a.activation(out=tmpD[:, 0], in_=tmpD[:, 0], func=AF.Exp, scale=-1.0), [t_tmpD], [t_tmpD])
                        s.op("act", lambda a: a.activation(out=tmpD[:, 1], in_=tmpD[:, 1], func=AF.Exp), [t_tmpD], [t_tmpD])
                        s.op("dve", lambda v: v.tensor_tensor(out=tmpD[:, 0], in0=tmpD[:, 0], in1=bds4[:], op=ALU.mult), [t_tmpD, t_cst], [t_tmpD])
                        s.op("dve", lambda v: v.tensor_tensor(out=tmpD[:, 1], in0=tmpD[:, 1], in1=bdi4[:], op=ALU.mult), [t_tmpD, t_cst], [t_tmpD])
                        chk("p4c1")
                        for u, t in enumerate(tiles):
                            s.op("dve", lambda v, u=u, t=t: v.scalar_tensor_tensor(out=X[:, 0, u, :], in0=psA[bA][:, u * 128:(u + 1) * 128], scalar=col(1, t, h),
                                                                                    in1=tmpD[:, 0, u, :], op0=ALU.mult, op1=ALU.mult),
                                 [t_psA[bA], t_tok, t_tmpD], [t_X[0]])
                        s.op("dve", lambda v: v.tensor_tensor(out=aT[:], in0=psB[0][:].rearrange("p (u n) -> p u n", u=4), in1=tmpD[:, 1], op=ALU.mult),
                             [t_psB[0], t_tmpD], [t_aT])
                        chk("p4c2")
                        s.op("pe", [lambda pe, u=u: pe.transpose(out=psTr[:, u * 128:(u + 1) * 128], in_=X[:, 0, u, :], identity=identb[:]) for u in range(4)],
                             [t_X[0], t_cst], [t_psTr])
                        s.op("act", lambda a: a.copy(out=Y[:, 0], in_=psTr[:, 0:512].rearrange("p (u n) -> p u n", u=4)), [t_psTr], [t_Y[0]])
                        s.op("dve", lambda v: v.scalar_tensor_tensor(out=R[:, 0], in0=psTr[:, 0:512].rearrange("p (u n) -> p u n", u=4), scalar=-1.0, in1=ident4[:],
                                                                      op0=ALU.mult, op1=ALU.add),
                             [t_psTr, t_cst], [t_R[0]])
                        chk("p4c")
                        cur = 0
                        for p in range(1, 6):
                            nxt = 1 - cur
                            if p < 5:
                                s.op("pe", [lambda pe, u=u: pe.matmul(psB[1][:, u * 128:(u + 1) * 128], lhsT=X[:, cur, u, :], rhs=Y[:, cur, u, :], start=True, stop=True)
                                            for u in range(4)], [t_X[cur], t_Y[cur]], [t_psB[1]])
                            bX = pA[0] % 2
                            pA[0] += 1
                            s.op("pe", [lambda pe, u=u: pe.matmul(psA[bX][:, u * 128:(u + 1) * 128], lhsT=Y[:, cur, u, :], rhs=X[:, cur, u, :], start=True, stop=True)
                                        for u in range(4)], [t_X[cur], t_Y[cur]], [t_psA[bX]])
                            s.op("dve", lambda v: v.tensor_copy(out=X[:, nxt], in_=psA[bX][:].rearrange("p (u n) -> p u n", u=4)), [t_psA[bX]], [t_X[nxt]])
                            if p < 5:
                                s.op("act", lambda a: a.copy(out=Y[:, nxt], in_=psB[1][:].rearrange("p (u n) -> p u n", u=4)), [t_psB[1]], [t_Y[nxt]])
                            s.op("pe", [lambda pe, u=u: pe.matmul(psB[0][:, u * 128:(u + 1) * 128], lhsT=X[:, nxt, u, :], rhs=R[:, cur, u, :], start=True, stop=True)
                                        for u in range(4)], [t_X[nxt], t_R[cur]], [t_psB[0]])
                            s.op("dve", lambda v: v.tensor_tensor(out=R[:, nxt], in0=psB[0][:].rearrange("p (u n) -> p u n", u=4), in1=R[:, cur], op=ALU.add),
                                 [t_psB[0], t_R[cur]], [t_R[nxt]])
                            cur = nxt
                        TT = R[:, cur]
                        t_TT = t_R[cur]
                        s.op("pe", [lambda pe, u=u, t=t: pe.transpose(out=psTr[:, u * 128:(u + 1) * 128], in_=knT[:, sl(t)], identity=identb[:]) for u, t in enumerate(tiles)],
                             [t_kn, t_cst], [t_psTr])
                        s.op("pe", [lambda pe, u=u, t=t: pe.transpose(out=psTr[:, 512 + u * 128:512 + (u + 1) * 128], in_=vcT[:, sl(t)], identity=identb[:]) for u, t in enumerate(tiles)],
                             [t_vc, t_cst], [t_psTr])
                        for u, t in enumerate(tiles):
                            s.op("dve", lambda v, u=u, t=t: v.tensor_scalar(out=kbg[:, u, :], in0=psTr[:, u * 128:(u + 1) * 128], scalar1=col(5, t, h), scalar2=None, op0=ALU.mult),
                                 [t_psTr, t_tok], [t_kbg])
                            s.op("act", lambda a, u=u, t=t: a.mul(out=kdec[:, u, :], in_=psTr[:, u * 128:(u + 1) * 128], mul=col(6, t, h)),
                                 [t_psTr, t_tok], [t_kdec])
                            s.op("dve", lambda v, u=u, t=t: v.tensor_scalar(out=vb[:, u, :], in0=psTr[:, 512 + u * 128:512 + (u + 1) * 128], scalar1=col(1, t, h), scalar2=None, op0=ALU.mult),
                                 [t_psTr, t_tok], [t_vb])
                        bU = pA[0] % 2
                        pA[0] += 1
                        s.op("pe", [lambda pe, u=u: pe.matmul(psA[bU][:, u * 128:(u + 1) * 128], lhsT=TT[:, u, :], rhs=vb[:, u, :], start=True, stop=True) for u in range(4)],
                             [t_TT, t_vb], [t_psA[bU]])
                        s.op("act", lambda a: a.copy(out=usb[:], in_=psA[bU][:].rearrange("p (u n) -> p u n", u=4)), [t_psA[bU]], [t_u])
                        s.op("pe", [lambda pe, u=u: pe.matmul(psB[1][:, u * 128:(u + 1) * 128], lhsT=kbg[:, u, :], rhs=TT[:, u, :], start=True, stop=True) for u in range(4)],
                             [t_TT, t_kbg], [t_psB[1]])
                        s.op("dve", lambda v: v.tensor_copy(out=wT[:], in_=psB[1][:].rearrange("p (u n) -> p u n", u=4)), [t_psB[1]], [t_wT])
                        chk("p4d")
                        for u, t in enumerate(tiles):
                            ob = t % 2
                            for hf in range(2):
                                n = t * 2 + hf
                                r = slice(hf * 64, hf * 64 + 64)
                                s.op("pe", lambda pe: pe.matmul(psV[:, 0, :], lhsT=wT[:, u, :], rhs=Sb[:], start=True, stop=True), [t_wT, t_Sb], [t_psV[0]])
                                s.op("dve", lambda v: v.scalar_tensor_tensor(out=vnew[r, :], in0=psV[r, 0, :], scalar=-1.0, in1=usb[r, u, :], op0=ALU.mult, op1=ALU.add),
                                     [t_u, t_psV[0]], [t_vnew])
                                s.op("pe", [lambda pe: pe.matmul(psV[:, 1 + hf, :], lhsT=qdT[:, sl(t)], rhs=Sb[:], start=True, stop=False),
                                            lambda pe: pe.matmul(psV[:, 1 + hf, :], lhsT=aT[:, u, :], rhs=vnew[:], start=False, stop=True)],
                                     [t_qd, t_Sb, t_aT, t_vnew], [t_psV[1 + hf]])
                                s.op("act", lambda a: a.copy(out=osb[r, ob, :], in_=psV[r, 1 + hf, :]), [t_psV[1 + hf]], [t_osb[ob]])
                                s.op("pe", lambda pe: pe.matmul(psV[:, 3, :], lhsT=kdec[r, u, :], rhs=vnew[r, :], start=True, stop=True),
                                     [t_kdec, t_vnew], [t_psV[3]])
                                s.op("pool", lambda g: g.tensor_scalar(out=Se[:], in0=St[:], scalar1=egl[:, n:n + 1], scalar2=None, op0=ALU.mult),
                                     [t_S, t_egl], [t_Se])
                                s.op("dve", lambda v: v.tensor_tensor(out=Sb[:], in0=psV[:, 3, :], in1=Se[:], op=ALU.add),
                                     [t_Se, t_psV[3]], [t_Sb])
                                s.op("dve", lambda v: v.tensor_tensor(out=St[:], in0=psV[:, 3, :], in1=Se[:], op=ALU.add),
                                     [t_Se, t_psV[3]], [t_S])
                            finalize_tile(ph, osb[:, ob, :], t_osb[ob], gcolD[:, 0:1], szT[:, sl(t)], t_sz, 8 + h, t, [t_gcD])
                s.barrier()

            if stop == "p4":
                if dbg:
                    for c in range(16):
                        s.dma("pool", dbg_y[:, c, :], yT[:, c, :], reads=[t_yT[c]])
                    for k in range(8):
                        s.dma("pool", dbg_h[:, k, :], hT[:, k, :], reads=[t_hT])
                s.finish("sp")
                print("instructions:", s.n_instr)
                return nc
            with ExitStack() as pes:
                sb = lambda name, shape, dt: pes.enter_context(nc.sbuf_tensor(uq(name), shape, dt))
                ps = lambda name, shape, dt: pes.enter_context(nc.psum_tensor(uq(name), shape, dt))
                ph = {}
                alloc_fin(pes, ph)
                psA = [ps("g_psA%d" % i, [128, 512], F32) for i in range(2)]
                t_psA = [TP(), TP()]
                psB = [ps("g_psB%d" % i, [128, 512], F32) for i in range(2)]
                t_psB = [TP(), TP()]
                psTr = ps("g_psT", [128, 1024], BF16)
                t_psTr = TP()
                psV = ps("g_psV", [128, 4, 128], F32)
                _tv = TP()
                t_psV = [_tv, _tv]
                pA = [0]
                wr = sb("g_wr", [128, 8, 16], BF16)
                t_wr = T()
                grT = sb("g_grT", [16, S], BF16)
                t_gr = T()
                w2f = sb("g_w2f", [16, 256], F32)
                w2 = sb("g_w2", [16, 256], BF16)
                t_w2 = T()
                gbc = sb("g_gb", [128, 2, 2], F32)
                t_gb = T()
                gcolG = sb("g_gcol", [128, 1], F32)
                t_gcG = T()
                wg = sb("g_w", [128, 6, 8, 128], BF16)
                t_wg = [T() for _ in range(6)]
                cl = sb("g_cl", [128, S], F32)
                t_cl = T()
                E = sb("g_E", [128, S], F32)
                Ei = sb("g_Ei", [128, S], F32)
                t_E, t_Ei = T(), T()
                qtT = sb("g_qtT", [128, S], BF16)
                ktT = sb("g_ktT", [128, S], BF16)
                t_qt, t_kt = T(), T()
                vtok = sb("g_v", [128, NT, 256], BF16)
                t_vt = T()
                ktok = sb("g_ktok", [128, NT, 128], BF16)
                t_ktok = T()
                szT = sb("g_szT", [128, 2, S], BF16)
                t_sz = T()
                attT = sb("g_attT", [128, 2, 2, 128], BF16)
                t_att = [T(), T()]
                eD = sb("g_eD", [128, 4, 128], F32)
                t_eD = [T() for _ in range(4)]
                St = sb("g_S", [128, 128], F32)
                Sb = sb("g_Sb", [128, 128], BF16)
                t_S, t_Sb = T(), T()
                osb = sb("g_o", [128, 2, 2, 128], F32)
                t_osb = [[T(), T()], [T(), T()]]

                def proj_fm(wt, tw, evac, m=128):
                    for tc in range(4):
                        b = pA[0] % 2
                        pA[0] += 1
                        s.op("pe", [lambda pe, k=k: pe.matmul(psA[b][0:m, :], lhsT=wt[:, k, 0:m], rhs=hT[:, k, tc * 512:(tc + 1) * 512],
                                                              start=(k == 0), stop=(k == 7)) for k in range(8)],
                             [tw, t_hT], [t_psA[b]])
                        evac(psA[b], t_psA[b], tc)

                s.dma("pool", wr[:], w_in[l][:, O_GR:O_GR + 16].rearrange("(k p) n -> p k n", p=128), writes=[t_wr])
                proj_fm(wr, t_wr, lambda p, tp, tc: s.op("act", lambda a: a.copy(out=grT[:, tc * 512:(tc + 1) * 512], in_=p[0:16, :]), [tp], [t_gr]), m=16)
                s.dma("sp", w2f[:], gla_w2[l], writes=[t_w2])
                s.op("dve", lambda v: v.tensor_copy(out=w2[:], in_=w2f[:]), [t_w2], [t_w2])
                for pr in range(2):
                    s.dma("sp", gbc[:, pr, 0:1], gla_b[l:l + 1, pr * 128:(pr + 1) * 128].rearrange("o c -> c o"), writes=[t_gb], allow_slow_non_contiguous=True)
                s.op("dve", lambda v: v.tensor_scalar(out=gbc[:, :, 1:2], in0=gbc[:, :, 0:1], scalar1=-1.0, scalar2=None, op0=ALU.mult), [t_gb], [t_gb])
                s.dma("sp", gcolG[:, 0:1], gla_norm[l:l + 1, :].rearrange("o c -> c o"), writes=[t_gcG], allow_slow_non_contiguous=True)
                sl = lambda t: slice(t * 128, (t + 1) * 128)

                for pr in range(0 if "p5" not in skip else 2, 2):
                    load_w(wg[:, 0], w_in[l][:, O_GQ + pr * 128: O_GQ + (pr + 1) * 128], t_wg[0])
                    load_w(wg[:, 1], w_in[l][:, O_GK + pr * 128: O_GK + (pr + 1) * 128], t_wg[1])
                    for j in range(2):
                        load_w(wg[:, 2 + j], w_in[l][:, O_GV + pr * 256 + j * 128: O_GV + pr * 256 + (j + 1) * 128], t_wg[2 + j])
                        load_w(wg[:, 4 + j], w_in[l][:, O_GZ + pr * 256 + j * 128: O_GZ + pr * 256 + (j + 1) * 128], t_wg[4 + j])
                    for tc in range(4):
                        b = pA[0] % 2
                        pA[0] += 1
                        s.op("pe", lambda pe: pe.matmul(psA[b][:], lhsT=w2[0:16, pr * 128:(pr + 1) * 128], rhs=grT[0:16, tc * 512:(tc + 1) * 512], start=True, stop=True),
                             [t_w2, t_gr], [t_psA[b]])
                        s.op("act", lambda a: a.activation(out=cl[:, tc * 512:(tc + 1) * 512], in_=psA[b][:], func=AF.Exp, bias=gbc[:, pr, 1:2], scale=-1.0),
                             [t_psA[b], t_gb], [t_cl])
                    s.op("act", lambda a: a.activation(out=cl[:], in_=cl[:], func=AF.Ln, bias=1.0), [t_cl], [t_cl])
                    s.op("dve", lambda v: v.tensor_tensor_scan(out=cl[:], data0=scanmask[:], data1=cl[:], initial=0.0, op0=ALU.mult, op1=ALU.add), [t_cl, t_cst], [t_cl])
                    s.op("act", lambda a: a.activation(out=E[:], in_=cl[:], func=AF.Exp, scale=-1.0 / 16.0), [t_cl], [t_E])
                    s.op("act", lambda a: a.activation(out=Ei[:], in_=cl[:], func=AF.Exp, scale=1.0 / 16.0), [t_cl], [t_Ei])
                    proj_fm(wg[:, 0], t_wg[0],
                            lambda p, tp, tc: s.op("dve", lambda v: v.scalar_tensor_tensor(out=qtT[:, tc * 512:(tc + 1) * 512], in0=p[:], scalar=0.125, in1=E[:, tc * 512:(tc + 1) * 512],
                                                                                            op0=ALU.mult, op1=ALU.mult), [tp, t_E], [t_qt]))
                    proj_fm(wg[:, 1], t_wg[1],
                            lambda p, tp, tc: s.op("dve", lambda v: v.tensor_tensor(out=ktT[:, tc * 512:(tc + 1) * 512], in0=p[:], in1=Ei[:, tc * 512:(tc + 1) * 512], op=ALU.mult),
                                                   [tp, t_Ei], [t_kt]))
                    for j in range(2):
                        proj_fm(wg[:, 4 + j], t_wg[4 + j],
                                lambda p, tp, tc, j=j: s.op("act", lambda a: a.activation(out=szT[:, j, tc * 512:(tc + 1) * 512], in_=p[:], func=AF.Silu), [tp], [t_sz]))
                    for tg in range(8):
                        b = pA[0] % 2
                        pA[0] += 1
                        fns = []
                        for u in range(2):
                            t = tg * 2 + u
                            for j in range(2):
                                for k in range(8):
                                    fns.append(lambda pe, k=k, t=t, u=u, j=j: pe.matmul(psA[b][:, u * 256 + j * 128: u * 256 + (j + 1) * 128], lhsT=hT[:, k, sl(t)],
                                                                                        rhs=wg[:, 2 + j, k, :], start=(k == 0), stop=(k == 7)))
                        s.op("pe", fns, [t_wg[2], t_wg[3], t_hT], [t_psA[b]])
                        s.op("dve", lambda v: v.tensor_copy(out=vtok[:, tg * 2:(tg + 1) * 2, :], in_=psA[b][:].rearrange("p (u n) -> p u n", u=2)), [t_psA[b]], [t_vt])
                    for tg in range(2):
                        s.op("pe", [lambda pe, u=u: pe.transpose(out=psTr[:, u * 128:(u + 1) * 128], in_=ktT[:, sl(tg * 8 + u)], identity=identb[:]) for u in range(8)],
                             [t_kt, t_cst], [t_psTr])
                        s.op("act", lambda a: a.copy(out=ktok[:, tg * 8:(tg + 1) * 8, :], in_=psTr[:].rearrange("p (u n) -> p u n", u=8)), [t_psTr], [t_ktok])
                    s.op("dve", lambda v: v.memset(St[:], 0.0), [], [t_S])
                    s.op("dve", lambda v: v.memset(Sb[:], 0.0), [], [t_Sb])
                    for t in range(NT):
                        ab = t % 2
                        bB = t % 2
                        s.op("pe", [lambda pe, hh=hh: pe.matmul(psB[hh][:, 0:128], lhsT=ktT[hh * 64:(hh + 1) * 64, sl(t)],
                                                                rhs=qtT[hh * 64:(hh + 1) * 64, sl(t)], start=True, stop=True) for hh in range(2)],
                             [t_kt, t_qt], [t_psB[0], t_psB[1]])
                        for hh in range(2):
                            s.op("dve", lambda v, hh=hh: v.tensor_tensor(out=attT[:, ab, hh, :], in0=psB[hh][:, 0:128], in1=bdi4[:, 0, :], op=ALU.mult),
                                 [t_psB[hh], t_cst], [t_att[ab]])
                        for hf in range(2):
                            n = t * 2 + hf
                            r = slice(hf * 64, hf * 64 + 64)
                            db = n % 4
                            dA = n % 2
                            s.op("pe", lambda pe: pe.matmul(psA[dA][:, 0:256], lhsT=ktok[r, t, :], rhs=vtok[r, t, :], start=True, stop=True),
                                 [t_ktok, t_vt], [t_psA[dA]])
                            ecol = E[:, n * 64 + 63: n * 64 + 64]
                            s.op("act", lambda a: a.mul(out=eD[0:64, db, :], in_=psA[dA][0:64, 0:128], mul=ecol[0:64, :]), [t_psA[dA], t_E], [t_eD[db]])
                            s.op("act", lambda a: a.mul(out=eD[64:128, db, :], in_=psA[dA][64:128, 128:256], mul=ecol[64:128, :]), [t_psA[dA], t_E], [t_eD[db]])
                            for hh in range(2):
                                hr = slice(hh * 64, hh * 64 + 64)
                                s.op("pe", [lambda pe: pe.matmul(psV[:, hf * 2 + hh, :], lhsT=qtT[hr, sl(t)], rhs=Sb[hr, :], start=True, stop=False),
                                            lambda pe: pe.matmul(psV[:, hf * 2 + hh, :], lhsT=attT[:, ab, hh, :], rhs=vtok[:, t, hh * 128:(hh + 1) * 128], start=False, stop=True)],
                                     [t_qt, t_Sb, t_att[ab], t_vt], [t_psV[hf]])
                                s.op("act", lambda a: a.copy(out=osb[r, ab, hh, :], in_=psV[r, hf * 2 + hh, :]), [t_psV[hf]], [t_osb[ab][hh]])
                            s.op("dve", lambda v: v.scalar_tensor_tensor(out=St[:], in0=St[:], scalar=ecol, in1=eD[:, db, :], op0=ALU.mult, op1=ALU.add),
                                 [t_S, t_E, t_eD[db]], [t_S])
                            s.op("dve", lambda v: v.tensor_copy(out=Sb[:], in_=St[:]), [t_S], [t_Sb])
                        for hh in range(2):
                            finalize_tile(ph, osb[:, ab, hh, :], t_osb[ab][hh], gcolG[:, 0:1], szT[:, hh, sl(t)], t_sz, 12 + pr * 2 + hh, t, [t_gcG])
                s.barrier()

            if stop == "p5":
                if dbg:
                    for c in range(16):
                        s.dma("pool", dbg_y[:, c, :], yT[:, c, :], reads=[t_yT[c]])
                    for k in range(8):
                        s.dma("pool", dbg_h[:, k, :], hT[:, k, :], reads=[t_hT])
                s.finish("sp")
                print("instructions:", s.n_instr)
                return nc
            if dbg and l == 0:
                for c in range(16):
                    s.dma("pool", dbg_y[:, c, :], yT[:, c, :], reads=[t_yT[c]])

            with ExitStack() as pes:
                sb = lambda name, shape, dt: pes.enter_context(nc.sbuf_tensor(uq(name), shape, dt))
                ps = lambda name, shape, dt: pes.enter_context(nc.psum_tensor(uq(name), shape, dt))
                wo = hT[:].rearrange("p k s -> p (k s)").rearrange("p (c n) -> p c n", c=16)
                wgt = sb("o_wg", [128, 8, D], BF16)
                wpp = sb("o_wp", [128, 2, D], BF16)
                t_wo, t_wgt, t_wpp = t_hT, T(), T()
                pTb = sb("o_pT", [128, 2, S], BF16)
                t_pT = T()
                gb2 = sb("o_gb2", [128, D], F32)
                t_gb2 = T()
                xb_ = sb("o_x", [128, 2, D], F32)
                t_x = [T(), T()]
                x1 = sb("o_x1", [128, 2, D], F32)
                t_x1 = [T(), T()]
                x1b = sb("o_x1b", [128, 2, D], BF16)
                t_x1b = [T(), T()]
                x1T = sb("o_x1T", [128, 2, D], BF16)
                t_x1T = [T(), T()]
                gate = sb("o_gate", [128, D], F32)
                t_gate = T()
                mm_ = sb("o_m", [128, D], F32)
                t_m = T()
                tmp = sb("o_tmp", [128, D], F32)
                t_tmp = T()
                junk = sb("o_junk", [128, D], BF16)
                t_j = T()
                st = sb("o_st", [128, 2, 8], F32)
                t_st = [T(), T()]
                psY = ps("o_psY", [128, 1024], F32)
                psG = ps("o_psG", [128, 1024], F32)
                psP = ps("o_psP", [128, 1024], F32)
                psX = ps("o_psX", [128, 1024], BF16)
                t_psY, t_psG, t_psP, t_psX = TP(), TP(), TP(), TP()
                load_w(wo, w_out[l], t_wo)
                load_w(wgt[:], ple_gate[l], t_wgt)
                load_w(wpp[:], ple_proj[l], t_wpp)
                load_w(pTb[:], pT_in[l], t_pT)
                s.dma("sp", gb1[:], post_gain[l:l + 1, :].partition_broadcast(128), writes=[t_gb1])
                s.dma("sp", gb2[:], ple_norm[l:l + 1, :].partition_broadcast(128), writes=[t_gb2])
                for t in range(NT):
                    b = t % 2
                    tsl = slice(t * 128, (t + 1) * 128)
                    s.dma("sp", xb_[:, b, :], x_src[tsl, :], writes=[t_x[b]])
                    fns = []
                    for nh in range(2):
                        for c in range(16):
                            fns.append(lambda pe, c=c, nh=nh: pe.matmul(psY[:, nh * 512:(nh + 1) * 512], lhsT=yT[:, c, tsl], rhs=wo[:, c, nh * 512:(nh + 1) * 512],
                                                                        start=(c == 0), stop=(c == 15)))
                    s.op("pe", fns, t_yT + [t_wo], [t_psY])
                    s.op("act", lambda a: a.activation(out=junk[:], in_=psY[:], func=AF.Square, accum_out=st[:, b, 0:1]), [t_psY], [t_j, t_st[b]])
                    s.op("act", lambda a: a.activation(out=st[:, b, 1:2], in_=st[:, b, 0:1], func=AF.Sqrt, bias=EPS, scale=1.0 / D), [t_st[b]], [t_st[b]])
                    s.op("dve", lambda v: v.reciprocal(out=st[:, b, 2:3], in_=st[:, b, 1:2]), [t_st[b]], [t_st[b]])
                    s.op("dve", lambda v: v.scalar_tensor_tensor(out=tmp[:], in0=psY[:], scalar=st[:, b, 2:3], in1=gb1[:], op0=ALU.mult, op1=ALU.mult),
                         [t_psY, t_st[b], t_gb1], [t_tmp])
                    s.op("pool", lambda g: g.tensor_tensor(out=x1[:, b, :], in0=tmp[:], in1=xb_[:, b, :], op=ALU.add), [t_tmp, t_x[b]], [t_x1[b]])
                    s.op("act", lambda a: a.copy(out=x1b[:, b, :], in_=x1[:, b, :]), [t_x1[b]], [t_x1b[b]])
                    s.op("pe", [lambda pe, k=k: pe.transpose(out=psX[:, k * 128:(k + 1) * 128], in_=x1b[:, b, k * 128:(k + 1) * 128], identity=identb[:]) for k in range(8)],
                         [t_x1b[b], t_cst], [t_psX])
                    s.op("dve", lambda v: v.tensor_copy(out=x1T[:, b, :], in_=psX[:]), [t_psX], [t_x1T[b]])
                    fns = []
                    for nh in range(2):
                        for k in range(8):
                            fns.append(lambda pe, k=k, nh=nh: pe.matmul(psG[:, nh * 512:(nh + 1) * 512], lhsT=x1T[:, b, k * 128:(k + 1) * 128], rhs=wgt[:, k, nh * 512:(nh + 1) * 512],
                                                                        start=(k == 0), stop=(k == 7)))
                    s.op("pe", fns, [t_x1T[b], t_wgt], [t_psG])
                    s.op("act", lambda a: a.activation(out=gate[:], in_=psG[:], func=AF.Sigmoid), [t_psG], [t_gate])
                    fns = []
                    for nh in range(2):
                        for k in range(2):
                            fns.append(lambda pe, k=k, nh=nh: pe.matmul(psP[:, nh * 512:(nh + 1) * 512], lhsT=pTb[:, k, tsl], rhs=wpp[:, k, nh * 512:(nh + 1) * 512],
                                                                        start=(k == 0), stop=(k == 1)))
                    s.op("pe", fns, [t_pT, t_wpp], [t_psP])
                    s.op("dve", lambda v: v.tensor_tensor(out=mm_[:], in0=psP[:], in1=gate[:], op=ALU.mult), [t_psP, t_gate], [t_m])
                    s.op("act", lambda a: a.activation(out=junk[:], in_=mm_[:], func=AF.Square, accum_out=st[:, b, 4:5]), [t_m], [t_j, t_st[b]])
                    s.op("act", lambda a: a.activation(out=st[:, b, 5:6], in_=st[:, b, 4:5], func=AF.Sqrt, bias=EPS, scale=1.0 / D), [t_st[b]], [t_st[b]])
                    s.op("dve", lambda v: v.reciprocal(out=st[:, b, 6:7], in_=st[:, b, 5:6]), [t_st[b]], [t_st[b]])
                    s.op("dve", lambda v: v.scalar_tensor_tensor(out=mm_[:], in0=mm_[:], scalar=st[:, b, 6:7], in1=gb2[:], op0=ALU.mult, op1=ALU.mult),
                         [t_m, t_st[b], t_gb2], [t_m])
                    s.op("pool", lambda g: g.tensor_tensor(out=x1[:, b, :], in0=x1[:, b, :], in1=mm_[:], op=ALU.add), [t_m, t_x1[b]], [t_x1[b]])
                    s.dma("sp", out[tsl, :], x1[:, b, :], reads=[t_x1[b]])
                s.barrier()
            x_src = out
        s.finish("sp")
        print("instructions:", s.n_instr, {k: v for k, v in s.cnt.items() if v})
    return nc


_CACHE = {}


def prep_inputs(inputs):
    f = lambda a: np.ascontiguousarray(np.asarray(a, dtype=np.float32))
    x = f(inputs["x"])
    p = f(inputs["p"])
    w_in = f(inputs["w_in"])
    w_rep = np.ascontiguousarray(np.repeat(w_in[:, :, O_DA:O_DA + 4], 128, axis=2))
    att_l = np.ascontiguousarray(np.stack([f(inputs["att_lq1"]), f(inputs["att_lk1"]), f(inputs["att_lq2"]), f(inputs["att_lk2"])], axis=1))
    shared = {
        "w_in": w_in, "w_rep": w_rep, "w_out": f(inputs["w_out"]), "ple_gate": f(inputs["ple_gate"]),
        "ple_proj": f(inputs["ple_proj"]), "pre_gain": f(inputs["pre_gain"]), "post_gain": f(inputs["post_gain"]),
        "ple_norm": f(inputs["ple_norm"]), "att_l": att_l, "att_subln": f(inputs["att_subln"]),
        "dn_conv": f(inputs["dn_conv"]), "dn_a_log": f(inputs["dn_a_log"]), "dn_dt_bias": f(inputs["dn_dt_bias"]),
        "dn_norm": f(inputs["dn_norm"]), "gla_w2": f(inputs["gla_w2"]), "gla_b": f(inputs["gla_b"]),
        "gla_norm": f(inputs["gla_norm"]), "consts": make_consts(),
    }
    maps = []
    for b in range(x.shape[0]):
        m = dict(shared)
        m["x"] = np.ascontiguousarray(x[b])
        m["pT"] = np.ascontiguousarray(p[:, b].transpose(0, 2, 1))
        maps.append(m)
    return maps


def kernel(**inputs):
    maps = prep_inputs(inputs)
    if "nc" not in _CACHE:
        _CACHE["nc"] = build_program()
    res = run_bass_kernel_spmd(_CACHE["nc"], maps, core_ids=list(range(8)))
    return np.stack([np.asarray(r["out"], dtype=np.float32) for r in res.results], axis=0)
```

```python
import math
from contextlib import ExitStack
import numpy as np
import concourse.bass as bass
import concourse.mybir as mybir
from concourse.bass_utils import run_bass_kernel_spmd

F32 = mybir.dt.float32
BF16 = mybir.dt.bfloat16
AF = mybir.ActivationFunctionType
ALU = mybir.AluOpType

S = 2048
D = 1024
NT = 16
DEPTH = 2
D_IN = 7704
EPS = 1e-6
O_AQ, O_AK, O_AV, O_AZ = 0, 1024, 2048, 3072
O_DQ, O_DK, O_DV, O_DZ, O_DB, O_DA = 4096, 4608, 5120, 5632, 6144, 6148
O_GQ, O_GK, O_GV, O_GZ, O_GR = 6152, 6408, 6664, 7176, 7688
C_ID, C_MATT, C_BDI, C_BDS, C_BLK, C_ALI, C_NEG, NC_CONST = 0, 128, 256, 384, 512, 640, 768, 896
ATT_W = [128] * 8


class T:
    __slots__ = ("w", "r", "x")

    def __init__(self, x=False):
        self.w = None
        self.r = {}
        self.x = x


def TP():
    return T(True)


class Sched:
    def __init__(self, nc, es, n_dma_sems=8):
        self.nc = nc
        self.eng = {"pe": nc.tensor, "act": nc.scalar, "dve": nc.vector,
                    "pool": nc.gpsimd, "sp": nc.sync}
        self.sem = {}
        self.cnt = {}
        for k in self.eng:
            self.sem[k] = es.enter_context(nc.semaphore("s_" + k))
            self.cnt[k] = 0
        self.seen = {k: {} for k in self.eng}
        self.dq = {}
        for q in ("sp", "pool"):
            sems = []
            for i in range(n_dma_sems):
                key = "d_%s%d" % (q, i)
                self.sem[key] = es.enter_context(nc.semaphore(key))
                self.cnt[key] = 0
                sems.append(key)
            self.dq[q] = [sems, 0]
        self.n_instr = 0

    def _wait(self, e, ev):
        key, val = ev
        if key == "pe" and e == "pe":
            return
        if self.seen[e].get(key, 0) >= val:
            return
        self.eng[e].wait_ge(self.sem[key], val)
        self.seen[e][key] = val

    def _deps(self, reads, writes):
        evs = {}
        for t in reads:
            if t.w is not None and evs.get(t.w[0], 0) < t.w[1]:
                evs[t.w[0]] = t.w[1]
        for t in writes:
            if t.w is not None and evs.get(t.w[0], 0) < t.w[1]:
                evs[t.w[0]] = t.w[1]
            for k, v in t.r.items():
                if evs.get(k, 0) < v:
                    evs[k] = v
        return evs

    def _commit(self, ev, reads, writes):
        k, v = ev
        for t in reads:
            if t.r.get(k, 0) < v:
                t.r[k] = v
        for t in writes:
            t.w = ev
            t.r = {}

    def op(self, e, fns, reads=(), writes=()):
        if callable(fns):
            fns = [fns]
        writes = list(writes) + [t for t in reads if t.x]
        reads = [t for t in reads if not t.x]
        for k, v in self._deps(reads, writes).items():
            self._wait(e, (k, v))
        h = self.eng[e]
        ins = None
        for f in fns:
            ins = f(h)
            self.n_instr += 1
        self.cnt[e] += 1
        ins.then_inc(self.sem[e], 1)
        ev = (e, self.cnt[e])
        self._commit(ev, reads, writes)
        return ev

    def dma(self, q, out, in_, reads=(), writes=(), **kw):
        sems, idx = self.dq[q]
        key = sems[idx]
        self.dq[q][1] = (idx + 1) % len(sems)
        if self.cnt[key] > 0:
            self._wait(q, (key, self.cnt[key]))
        for k, v in self._deps(reads, writes).items():
            self._wait(q, (k, v))
        ins = self.eng[q].dma_start(out=out, in_=in_, **kw)
        self.n_instr += 1
        self.cnt[key] += 16
        ins.then_inc(self.sem[key], 16)
        ev = (key, self.cnt[key])
        self._commit(ev, reads, writes)
        return ev

    def barrier(self):
        for e in self.eng:
            for k, v in self.cnt.items():
                if v > 0 and k != e:
                    self._wait(e, (k, v))

    def finish(self, e="sp"):
        for k, v in self.cnt.items():
            if v > 0 and k != e:
                self._wait(e, (k, v))


def make_consts():
    c = np.zeros((128, NC_CONST), np.float32)
    i = np.arange(128)
    c[:, C_ID:C_ID + 128] = np.eye(128, dtype=np.float32)
    c[:, C_MATT:C_MATT + 128] = (i[:, None] <= i[None, :]).astype(np.float32)
    same = (i[:, None] // 64) == (i[None, :] // 64)
    c[:, C_BDI:C_BDI + 128] = ((i[:, None] <= i[None, :]) & same).astype(np.float32)
    c[:, C_BDS:C_BDS + 128] = ((i[None, :] < i[:, None]) & same).astype(np.float32)
    c[:, C_BLK:C_BLK + 128] = same.astype(np.float32)
    c[:, C_NEG:C_NEG + 128] = np.where(i[:, None] > i[None, :], -30000.0, 0.0).astype(np.float32)
    slopes = 2.0 ** (-8.0 * np.arange(1, 9) / 8.0)
    for h in range(8):
        for dd in range(16):
            c[:, C_ALI + h * 16 + dd] = slopes[h] * (i - 127 - 128 * dd)
    return c


def build_program(depth=DEPTH, dbg=False, stop=None, skip=()):
    try:
        return _build_program(depth, dbg, stop, skip)
    except StopBuild as e:
        return e.nc


class StopBuild(Exception):
    def __init__(self, nc):
        self.nc = nc


def _build_program(depth=DEPTH, dbg=False, stop=None, skip=()):
    nc = bass.Bass("TRN2", target_bir_lowering=False)
    dr = lambda name, shape, kind="ExternalInput", dt=F32: nc.dram_tensor(name, shape, dt, kind=kind).ap()
    x_in = dr("x", [S, D])
    pT_in = dr("pT", [DEPTH, 256, S])
    w_in = dr("w_in", [DEPTH, D, D_IN])
    w_rep = dr("w_rep", [DEPTH, D, 512])
    w_out = dr("w_out", [DEPTH, 2048, D])
    ple_gate = dr("ple_gate", [DEPTH, D, D])
    ple_proj = dr("ple_proj", [DEPTH, 256, D])
    pre_gain = dr("pre_gain", [DEPTH, D])
    post_gain = dr("post_gain", [DEPTH, D])
    ple_norm = dr("ple_norm", [DEPTH, D])
    att_l = dr("att_l", [DEPTH, 4, 64])
    att_subln = dr("att_subln", [DEPTH, 128])
    dn_conv = dr("dn_conv", [DEPTH, 4, 1536])
    dn_a_log = dr("dn_a_log", [DEPTH, 4])
    dn_dt_bias = dr("dn_dt_bias", [DEPTH, 4])
    dn_norm = dr("dn_norm", [DEPTH, 128])
    gla_w2 = dr("gla_w2", [DEPTH, 16, 256])
    gla_b = dr("gla_b", [DEPTH, 256])
    gla_norm = dr("gla_norm", [DEPTH, 128])
    consts_in = dr("consts", [128, NC_CONST])
    out = dr("out", [S, D], kind="ExternalOutput")
    dbg_y = dr("dbg_y", [128, 16, S], kind="ExternalOutput") if dbg else None
    dbg_h = dr("dbg_h", [128, 8, S], kind="ExternalOutput") if dbg else None

    _uq = [0]


    def uq(name):
        _uq[0] += 1
        return "%s_%d" % (name, _uq[0])

    with ExitStack() as es:
        s = Sched(nc, es)
        sbp = lambda name, shape, dt: es.enter_context(nc.sbuf_tensor(uq(name), shape, dt))
        hT = sbp("hT", [128, 8, S], BF16)
        t_hT = T()
        yT = sbp("yT", [128, 16, S], BF16)
        t_yT = [T() for _ in range(16)]
        cst = sbp("cst", [128, NC_CONST], F32)
        t_cst = T()
        identb = sbp("identb", [128, 128], BF16)
        ident4 = sbp("ident4", [128, 4, 128], BF16)
        matt2 = sbp("matt2", [128, 2, 128], BF16)
        negb = sbp("negb", [128, 128], BF16)
        bdi4 = sbp("bdi4", [128, 4, 128], BF16)
        bds4 = sbp("bds4", [128, 4, 128], F32)
        ones_f = sbp("ones_f", [128, 128], F32)
        scanmask = sbp("scanmask", [128, S], F32)
        gb0 = sbp("gb0", [128, D], F32)
        t_gb0 = T()
        gb1 = sbp("gb1", [128, D], F32)
        t_gb1 = T()
        small = sbp("small", [128, 64], F32)
        t_small = T()

        ident = cst[:, C_ID:C_ID + 128]
        s.dma("sp", cst[:], consts_in, writes=[t_cst])
        s.op("dve", lambda v: v.tensor_copy(out=identb[:], in_=ident), [t_cst], [t_cst])
        for u in range(4):
            s.op("dve", lambda v, u=u: v.tensor_copy(out=ident4[:, u, :], in_=ident), [t_cst], [t_cst])
            s.op("dve", lambda v, u=u: v.tensor_copy(out=bdi4[:, u, :], in_=cst[:, C_BDI:C_BDI + 128]), [t_cst], [t_cst])
            s.op("dve", lambda v, u=u: v.tensor_copy(out=bds4[:, u, :], in_=cst[:, C_BDS:C_BDS + 128]), [t_cst], [t_cst])
        for u in range(2):
            s.op("dve", lambda v, u=u: v.tensor_copy(out=matt2[:, u, :], in_=cst[:, C_MATT:C_MATT + 128]), [t_cst], [t_cst])
        s.op("dve", lambda v: v.tensor_copy(out=negb[:], in_=cst[:, C_NEG:C_NEG + 128]), [t_cst], [t_cst])
        s.op("pool", lambda g: g.memset(ones_f[:], 1.0), [], [t_cst])
        s.op("pool", lambda g: g.memset(scanmask[:], 1.0), [], [t_cst])
        s.op("pool", lambda g: g.memset(scanmask[:].rearrange("p (c k) -> p c k", k=64)[:, :, 0:1], 0.0), [], [t_cst])

        def chk(name):
            if stop == name:
                s.finish("sp")
                print("STOP at", name, "instructions:", s.n_instr)
                raise StopBuild(nc)

        def load_w(dst, src2d, tw):
            src = src2d.rearrange("(k p) n -> p k n", p=128)
            nk = src.shape[1]
            per = max(1, 2048 // max(1, src.shape[2] * 4 // 512))
            per = min(per, nk)
            if src.shape[2] >= 1024:
                per = 1
            for k0 in range(0, nk, per):
                s.dma("pool", dst[:, k0:k0 + per], src[:, k0:k0 + per], writes=[tw])

        def finalize_tile(ph, o_ap, t_o, gaincol, szT_ap, t_sz, mix, t, extra_reads=()):
            junk, t_junk, st, t_st, an, t_an, psT, t_psT = ph["fin"]
            i = ph["fin_i"] = ph.get("fin_i", 0) + 1
            b = i % 2
            s.op("act", lambda a: a.activation(out=junk[:, b, :], in_=o_ap, func=AF.Square, accum_out=st[:, b, 0:1]),
                 [t_o], [t_junk[b], t_st[b]])
            s.op("act", lambda a: a.activation(out=st[:, b, 1:2], in_=st[:, b, 0:1], func=AF.Sqrt, bias=EPS, scale=1.0 / 128.0),
                 [t_st[b]], [t_st[b]])
            s.op("dve", lambda v: v.reciprocal(out=st[:, b, 2:3], in_=st[:, b, 1:2]), [t_st[b]], [t_st[b]])
            s.op("dve", lambda v: v.tensor_scalar(out=an[:, b, :], in0=o_ap, scalar1=st[:, b, 2:3], scalar2=None, op0=ALU.mult),
                 [t_o, t_st[b]], [t_an[b]])
            s.op("pe", lambda pe: pe.transpose(out=psT[:, b * 128:(b + 1) * 128], in_=an[:, b, :], identity=identb[:]),
                 [t_an[b], t_cst], [t_psT[b]])
            s.op("dve", lambda v: v.scalar_tensor_tensor(out=yT[:, mix, t * 128:(t + 1) * 128], in0=psT[:, b * 128:(b + 1) * 128],
                                                          scalar=gaincol, in1=szT_ap, op0=ALU.mult, op1=ALU.mult),
                 [t_psT[b], t_sz] + list(extra_reads), [t_yT[mix]])

        def alloc_fin(pes, ph):
            sb = lambda name, shape, dt: pes.enter_context(nc.sbuf_tensor(uq(name), shape, dt))
            junk = sb("fjunk", [128, 2, 128], BF16)
            st = sb("fst", [128, 2, 4], F32)
            an = sb("fan", [128, 2, 128], BF16)
            psT = pes.enter_context(nc.psum_tensor(uq("fpsT"), [128, 1024], BF16))
            _tp = TP()
            ph["fin"] = (junk, [T(), T()], st, [T(), T()], an, [T(), T()], psT, [_tp, _tp])

        x_src = x_in
        for l in range(depth):
            lam_init = 0.8 - 0.6 * math.exp(-0.3 * l)
            with ExitStack() as pes:
                sb = lambda name, shape, dt: pes.enter_context(nc.sbuf_tensor(uq(name), shape, dt))
                xb_ = sb("p1x", [128, 2, D], F32)
                t_x = [T(), T()]
                hb = sb("p1h", [128, 2, D], BF16)
                t_hb = [T(), T()]
                junk = sb("p1j", [128, D], BF16)
                t_j = T()
                st = sb("p1s", [128, 2, 4], F32)
                t_st = [T(), T()]
                psT = pes.enter_context(nc.psum_tensor(uq("p1ps"), [128, 2, 1024], BF16))
                t_ps = [TP(), TP()]
                s.dma("sp", gb0[:], pre_gain[l:l + 1, :].partition_broadcast(128), writes=[t_gb0])
                for t in range(NT):
                    b = t % 2
                    s.dma("sp", xb_[:, b, :], x_src[t * 128:(t + 1) * 128, :], writes=[t_x[b]])
                    s.op("act", lambda a: a.activation(out=junk[:], in_=xb_[:, b, :], func=AF.Square, accum_out=st[:, b, 0:1]),
                         [t_x[b]], [t_j, t_st[b]])
                    s.op("act", lambda a: a.activation(out=st[:, b, 1:2], in_=st[:, b, 0:1], func=AF.Sqrt, bias=EPS, scale=1.0 / D),
                         [t_st[b]], [t_st[b]])
                    s.op("dve", lambda v: v.reciprocal(out=st[:, b, 2:3], in_=st[:, b, 1:2]), [t_st[b]], [t_st[b]])
                    s.op("dve", lambda v: v.scalar_tensor_tensor(out=hb[:, b, :], in0=xb_[:, b, :], scalar=st[:, b, 2:3], in1=gb0[:],
                                                                  op0=ALU.mult, op1=ALU.mult),
                         [t_x[b], t_st[b], t_gb0], [t_hb[b]])
                    s.op("pe", [lambda pe, k=k: pe.transpose(out=psT[:, b, k * 128:(k + 1) * 128], in_=hb[:, b, k * 128:(k + 1) * 128],
                                                             identity=identb[:]) for k in range(8)],
                         [t_hb[b], t_cst], [t_ps[b]])
                    s.op("act", lambda a: a.copy(out=hT[:, :, t * 128:(t + 1) * 128],
                                                 in_=psT[:, b, :].rearrange("p (k n) -> p k n", k=8)),
                         [t_ps[b]], [t_hT])
                s.barrier()

            if stop == "p1":
                if dbg:
                    for c in range(16):
                        s.dma("pool", dbg_y[:, c, :], yT[:, c, :], reads=[t_yT[c]])
                    for k in range(8):
                        s.dma("pool", dbg_h[:, k, :], hT[:, k, :], reads=[t_hT])
                s.finish("sp")
                print("instructions:", s.n_instr)
                return nc
            with ExitStack() as pes:
                sb = lambda name, shape, dt: pes.enter_context(nc.sbuf_tensor(uq(name), shape, dt))
                ps = lambda name, shape, dt: pes.enter_context(nc.psum_tensor(uq(name), shape, dt))
                wbuf = sb("a_w", [128, 2, 4, 8, 128], BF16)
                t_w = [[T() for _ in range(4)] for _ in range(2)]
                qT = sb("a_qT", [128, 2, S], BF16)
                t_q = [T(), T()]
                kT = sb("a_kT", [128, 2, S], BF16)
                t_k = [T(), T()]
                szT = sb("a_szT", [128, 2, S], BF16)
                t_sz = [T(), T()]
                vaug = sb("a_v", [128, 2, NT, 130], BF16)
                t_v = [T(), T()]
                pt = sb("a_p", [128, 3, 2, 128], BF16)
                t_pt = [T(), T(), T()]
                NF = 8
                osb = sb("a_o", [128, NF, 2, 128], F32)
                t_osb = [T() for _ in range(NF)]
                rr = sb("a_rr", [128, NF, 4], F32)
                t_rr = [T() for _ in range(NF)]
                fjunk = sb("a_fj", [128, NF, 128], BF16)
                t_fj = [T() for _ in range(NF)]
                fst = sb("a_fst", [128, NF, 4], F32)
                t_fst = [T() for _ in range(NF)]
                fan = sb("a_fan", [128, NF, 128], BF16)
                t_fan = [T() for _ in range(NF)]
                lq = sb("a_lq", [128, 4, 64], F32)
                t_lq = T()
                ztmp = sb("a_zt", [128, 512], F32)
                t_zt = T()
                gcol = sb("a_gc", [128, 2], F32)
                t_gc = T()
                psA = ps("a_psA", [128, 512], F32)
                t_psA = TP()
                psS = [ps("a_psS%d" % i, [128, 2, 512], F32) for i in range(2)]
                t_psS = [TP(), TP()]
                psO = [ps("a_psO%d" % i, [128, 2, 256], F32) for i in range(2)]
                t_psO = [TP(), TP()]
                psF = ps("a_psF", [128, 1024], BF16)
                t_psF = TP()

                s.dma("sp", lq[:], att_l[l:l + 1].partition_broadcast(128), writes=[t_lq])
                s.op("dve", lambda v: v.tensor_tensor(out=lq[:, 0, :], in0=lq[:, 0, :], in1=lq[:, 1, :], op=ALU.mult), [t_lq], [t_lq])
                s.op("dve", lambda v: v.tensor_tensor(out=lq[:, 2, :], in0=lq[:, 2, :], in1=lq[:, 3, :], op=ALU.mult), [t_lq], [t_lq])
                s.op("act", lambda a: a.activation(out=lq[:, 1, :], in_=lq[:, 0, :], func=AF.Copy, accum_out=small[:, 1:2]), [t_lq], [t_lq, t_small])
                s.op("act", lambda a: a.activation(out=lq[:, 3, :], in_=lq[:, 2, :], func=AF.Copy, accum_out=small[:, 2:3]), [t_lq], [t_lq, t_small])
                s.op("act", lambda a: a.activation(out=small[:, 3:5], in_=small[:, 1:3], func=AF.Exp), [t_small], [t_small])
                s.op("dve", lambda v: v.tensor_tensor(out=small[:, 5:6], in0=small[:, 4:5], in1=small[:, 3:4], op=ALU.subtract), [t_small], [t_small])
                s.op("dve", lambda v: v.tensor_scalar(out=small[:, 0:1], in0=small[:, 5:6], scalar1=-lam_init, scalar2=None, op0=ALU.add), [t_small], [t_small])
                s.dma("sp", gcol[:, 0:1], att_subln[l:l + 1, :].rearrange("o c -> c o"), writes=[t_gc], allow_slow_non_contiguous=True)
                s.op("dve", lambda v: v.tensor_scalar(out=gcol[:, 1:2], in0=gcol[:, 0:1], scalar1=1.0 - lam_init, scalar2=None, op0=ALU.mult), [t_gc], [t_gc])
                for hb in range(2):
                    s.op("pool", lambda g, hb=hb: g.memset(vaug[:, hb, :, 128:130], 1.0), [], [t_v[hb]])

                offs = [O_AQ, O_AK, O_AV, O_AZ]

                def proj_gen(h):
                    hb = h % 2
                    for j in range(4):
                        load_w(wbuf[:, hb, j], w_in[l][:, offs[j] + h * 128: offs[j] + (h + 1) * 128], t_w[hb][j])
                    yield
                    for j, dst, td in ((0, qT, t_q), (1, kT, t_k), (3, szT, t_sz)):
                        for tc in range(4):
                            s.op("pe", [lambda pe, k=k: pe.matmul(psA[:], lhsT=wbuf[:, hb, j, k, :], rhs=hT[:, k, tc * 512:(tc + 1) * 512],
                                                                  start=(k == 0), stop=(k == 7)) for k in range(8)],
                                 [t_w[hb][j], t_hT], [t_psA])
                            if j == 0:
                                s.op("dve", lambda v: v.tensor_copy(out=dst[:, hb, tc * 512:(tc + 1) * 512], in_=psA[:]), [t_psA], [td[hb]])
                            elif j == 1:
                                s.op("dve", lambda v: v.tensor_copy(out=dst[:, hb, tc * 512:(tc + 1) * 512], in_=psA[:]), [t_psA], [td[hb]])
                            else:
                                s.op("act", lambda a: a.activation(out=ztmp[:], in_=psA[:], func=AF.Exp, scale=-1.0), [t_psA], [t_zt])
                                s.op("dve", lambda v: v.tensor_scalar(out=ztmp[:], in0=ztmp[:], scalar1=1.0, scalar2=None, op0=ALU.add), [t_zt], [t_zt])
                                s.op("dve", lambda v: v.reciprocal(out=ztmp[:], in_=ztmp[:]), [t_zt], [t_zt])
                                s.op("dve", lambda v: v.tensor_tensor(out=dst[:, hb, tc * 512:(tc + 1) * 512], in0=psA[:], in1=ztmp[:], op=ALU.mult),
                                     [t_psA, t_zt], [td[hb]])
                            yield
                    for tg in range(4):
                        fns = []
                        for u in range(4):
                            t = tg * 4 + u
                            for k in range(8):
                                fns.append(lambda pe, k=k, t=t, u=u: pe.matmul(psA[:, u * 128:(u + 1) * 128], lhsT=hT[:, k, t * 128:(t + 1) * 128],
                                                                              rhs=wbuf[:, hb, 2, k, :], start=(k == 0), stop=(k == 7)))
                        s.op("pe", fns, [t_w[hb][2], t_hT], [t_psA])
                        s.op("dve", lambda v: v.tensor_copy(out=vaug[:, hb, tg * 4:(tg + 1) * 4, 0:128],
                                                            in_=psA[:].rearrange("p (u n) -> p u n", u=4)), [t_psA], [t_v[hb]])
                        yield

                fin_i = [0]

                def fin_stages(h, t, ob):
                    hb = h % 2
                    f = fin_i[0] % NF
                    fin_i[0] += 1
                    tsl = slice(t * 128, (t + 1) * 128)

                    def st1():
                        s.op("dve", lambda v: v.reciprocal(out=rr[:, f, 0:2], in_=psO[ob][:, :, 128]), [t_psO[ob]], [t_rr[f]])
                        s.op("dve", lambda v: v.tensor_tensor(out=rr[:, f, 2:3], in0=rr[:, f, 1:2], in1=small[:, 0:1], op=ALU.mult),
                             [t_rr[f], t_small], [t_rr[f]])
                        s.op("dve", lambda v: v.tensor_scalar(out=osb[:, f, 1, :], in0=psO[ob][:, 1, 0:128], scalar1=rr[:, f, 2:3], scalar2=None, op0=ALU.mult),
                             [t_psO[ob], t_rr[f]], [t_osb[f]])
                        s.op("dve", lambda v: v.scalar_tensor_tensor(out=osb[:, f, 0, :], in0=psO[ob][:, 0, 0:128], scalar=rr[:, f, 0:1],
                                                                      in1=osb[:, f, 1, :], op0=ALU.mult, op1=ALU.add),
                             [t_psO[ob], t_rr[f], t_osb[f]], [t_osb[f]])
                        s.op("dve", lambda v: v.scalar_tensor_tensor(out=osb[:, f, 1, :], in0=osb[:, f, 0, :], scalar=1.0, in1=osb[:, f, 0, :],
                                                                      op0=ALU.mult, op1=ALU.mult, accum_out=fst[:, f, 0:1]),
                             [t_osb[f]], [t_osb[f], t_fst[f]])

                    def st2():
                        s.op("act", lambda a: a.activation(out=fst[:, f, 1:2], in_=fst[:, f, 0:1], func=AF.Ln, bias=EPS, scale=1.0 / 128.0),
                             [t_fst[f]], [t_fst[f]])
                        s.op("act", lambda a: a.activation(out=fst[:, f, 2:3], in_=fst[:, f, 1:2], func=AF.Exp, scale=-0.5),
                             [t_fst[f]], [t_fst[f]])

                    def st3():
                        s.op("dve", lambda v: v.tensor_scalar(out=fan[:, f, :], in0=osb[:, f, 0, :], scalar1=fst[:, f, 2:3], scalar2=None, op0=ALU.mult),
                             [t_osb[f], t_fst[f]], [t_fan[f]])

                    def st4():
                        s.op("pe", lambda pe: pe.transpose(out=psF[:, f * 128:(f + 1) * 128], in_=fan[:, f, :], identity=identb[:]),
                             [t_fan[f], t_cst], [t_psF])

                    def st5():
                        s.op("dve", lambda v: v.scalar_tensor_tensor(out=yT[:, h, tsl], in0=psF[:, f * 128:(f + 1) * 128],
                                                                      scalar=gcol[:, 1:2], in1=szT[:, hb, tsl], op0=ALU.mult, op1=ALU.mult),
                             [t_psF, t_sz[hb], t_gc], [t_yT[h]])
                    return [st1, st2, st3, st4, st5]

                h0 = 0 if "p3" not in skip else 8
                if h0 < 8:
                    for _ in proj_gen(h0):
                        pass
                for h in range(h0, 8):
                    hb = h % 2
                    gen = proj_gen(h + 1) if h + 1 < 8 else iter(())
                    blocks = [(t, c) for t in range(NT) for c in range(t + 1)]
                    pending = []

                    def score(i):
                        t, c = blocks[i]
                        sbk = i % 2
                        diag = (c == t)
                        fns = [lambda pe, m=m: pe.matmul(psS[sbk][:, m, 0:128], lhsT=kT[m * 64:(m + 1) * 64, hb, c * 128:(c + 1) * 128],
                                                         rhs=qT[m * 64:(m + 1) * 64, hb, t * 128:(t + 1) * 128], start=True, stop=not diag)
                               for m in range(2)]
                        if diag:
                            fns += [lambda pe, m=m: pe.matmul(psS[sbk][:, m, 0:128], lhsT=identb[:], rhs=negb[:], start=False, stop=True)
                                    for m in range(2)]
                        s.op("pe", fns, [t_k[hb], t_q[hb], t_cst], [t_psS[sbk]])

                    score(0)
                    for i, (t, c) in enumerate(blocks):
                        sbk = i % 2
                        pb = i % 3
                        ob = t % 2
                        if i + 1 < len(blocks):
                            score(i + 1)
                        bcol = cst[:, C_ALI + h * 16 + (t - c): C_ALI + h * 16 + (t - c) + 1]
                        s.op("act", lambda a: a.activation(out=pt[:, pb, :, :], in_=psS[sbk][:, :, 0:128], func=AF.Exp, bias=bcol, scale=0.125),
                             [t_psS[sbk], t_cst], [t_pt[pb]])
                        while pending and pending[0][0] <= i:
                            pending.pop(0)[1]()
                        if i % 8 == 4:
                            next(gen, None)
                        s.op("pe", [lambda pe, m=m: pe.matmul(psO[ob][:, m, 0:129], lhsT=pt[:, pb, m, :], rhs=vaug[:, hb, c, 0:129],
                                                              start=(c == 0 and m == 0), stop=(c == t and m == 1)) for m in range(2)],
                             [t_pt[pb], t_v[hb]], [t_psO[ob]])
                        if c == t:
                            fnext = fin_i[0] % NF
                            for e_ in [e for e in pending if e[2] == fnext]:
                                pending.remove(e_)
                                e_[1]()
                            for dly, st_ in zip((1, 6, 8, 10, 12), fin_stages(h, t, ob)):
                                pending.append([i + dly, st_, fnext])
                            pending.sort(key=lambda e: e[0])
                    for _ in gen:
                        pass
                    while pending:
                        pending.pop(0)[1]()
                s.barrier()
            if stop == "p3":
                if dbg:
                    for c in range(16):
                        s.dma("pool", dbg_y[:, c, :], yT[:, c, :], reads=[t_yT[c]])
                    for k in range(8):
                        s.dma("pool", dbg_h[:, k, :], hT[:, k, :], reads=[t_hT])
                s.finish("sp")
                print("instructions:", s.n_instr)
                return nc
            with ExitStack() as pes:
                sb = lambda name, shape, dt: pes.enter_context(nc.sbuf_tensor(uq(name), shape, dt))
                ps = lambda name, shape, dt: pes.enter_context(nc.psum_tensor(uq(name), shape, dt))
                ph = {}
                alloc_fin(pes, ph)
                psA = [ps("d_psA%d" % i, [128, 512], F32) for i in range(2)]
                t_psA = [TP(), TP()]
                psB = [ps("d_psB%d" % i, [128, 512], F32) for i in range(2)]
                t_psB = [TP(), TP()]
                psTr = ps("d_psT", [128, 1024], BF16)
                t_psTr = TP()
                psV = ps("d_psV", [128, 4, 128], F32)
                _tv = TP()
                t_psV = [_tv, _tv, _tv, _tv]
                pA = [0]

                wba = sb("d_wba", [128, 8, 8], BF16)
                t_wba = T()
                tok = sb("d_tok", [128, 8, 64], F32)
                t_tok = T()
                prm = sb("d_prm", [128, 16], F32)
                t_prm = T()
                gcolD = sb("d_gcol", [128, 1], F32)
                t_gcD = T()
                cw = sb("d_cw", [128, 3, 4], F32)
                t_cw = T()
                wd = sb("d_w", [128, 5, 8, 128], BF16)
                t_wd = [T() for _ in range(5)]
                raw = sb("d_raw", [128, S + 3], F32)
                t_raw = T()
                cv = sb("d_cv", [128, S], F32)
                t_cv = T()
                qnT = sb("d_qnT", [128, S], BF16)
                knT = sb("d_knT", [128, S], BF16)
                vcT = sb("d_vcT", [128, S], BF16)
                qdT = sb("d_qdT", [128, S], BF16)
                t_qn, t_kn, t_vc, t_qd = T(), T(), T(), T()
                szT = sb("d_szT", [128, S], BF16)
                t_sz = T()
                gcr = sb("d_gcr", [128, S], F32)
                t_gcr = T()
                egl = sb("d_egl", [128, 32], F32)
                t_egl = T()
                sd = sb("d_sd", [128, 2, 512], F32)
                t_sd = [T(), T()]
                tmpD = sb("d_tmpD", [128, 2, 4, 128], F32)
                t_tmpD = T()
                X = sb("d_X", [128, 2, 4, 128], BF16)
                Y = sb("d_Y", [128, 2, 4, 128], BF16)
                R = sb("d_R", [128, 2, 4, 128], BF16)
                t_X, t_Y, t_R = [T(), T()], [T(), T()], [T(), T()]
                aT = sb("d_aT", [128, 4, 128], BF16)
                t_aT = T()
                kbg = sb("d_kbg", [128, 4, 128], BF16)
                kdec = sb("d_kdec", [128, 4, 128], BF16)
                vb = sb("d_vb", [128, 4, 128], BF16)
                t_kbg, t_kdec, t_vb = T(), T(), T()
                usb = sb("d_u", [128, 4, 128], F32)
                t_u = T()
                wT = sb("d_wT", [128, 4, 128], BF16)
                t_wT = T()
                vnew = sb("d_vnew", [128, 128], BF16)
                t_vnew = T()
                St = sb("d_S", [128, 128], F32)
                Sb = sb("d_Sb", [128, 128], BF16)
                Se = sb("d_Se", [128, 128], F32)
                t_S, t_Sb, t_Se = T(), T(), T()
                osb = sb("d_o", [128, 2, 128], F32)
                t_osb = [T(), T()]

                def proj_fm(wt, tw, evac):
                    for tc in range(4):
                        b = pA[0] % 2
                        pA[0] += 1
                        s.op("pe", [lambda pe, k=k: pe.matmul(psA[b][:], lhsT=wt[:, k, :], rhs=hT[:, k, tc * 512:(tc + 1) * 512],
                                                              start=(k == 0), stop=(k == 7)) for k in range(8)],
                             [tw, t_hT], [t_psA[b]])
                        evac(psA[b], t_psA[b], tc)

                s.dma("pool", wba[:], w_in[l][:, O_DB:O_DB + 8].rearrange("(k p) n -> p k n", p=128), writes=[t_wba])
                s.dma("sp", prm[:, 0:4], dn_a_log[l:l + 1, :].partition_broadcast(128), writes=[t_prm])
                s.dma("sp", prm[:, 4:8], dn_dt_bias[l:l + 1, :].partition_broadcast(128), writes=[t_prm])
                s.op("act", lambda a: a.activation(out=prm[:, 8:12], in_=prm[:, 0:4], func=AF.Exp), [t_prm], [t_prm])
                s.op("dve", lambda v: v.tensor_scalar(out=prm[:, 8:12], in0=prm[:, 8:12], scalar1=-1.0, scalar2=None, op0=ALU.mult), [t_prm], [t_prm])
                s.dma("sp", gcolD[:, 0:1], dn_norm[l:l + 1, :].rearrange("o c -> c o"), writes=[t_gcD], allow_slow_non_contiguous=True)
                fns = []
                for t in range(NT):
                    for k in range(8):
                        fns.append(lambda pe, k=k, t=t: pe.matmul(psA[0][:, t * 8:(t + 1) * 8], lhsT=hT[:, k, t * 128:(t + 1) * 128],
                                                                  rhs=wba[:, k, :], start=(k == 0), stop=(k == 7)))
                s.op("pe", fns, [t_wba, t_hT], [t_psA[0]])
                pA[0] = 1
                ba = psA[0][:, 0:128].rearrange("p (t c) -> p t c", c=8)
                tk = lambda i: tok[:, i, :].rearrange("p (t c) -> p t c", c=4)
                s.op("act", lambda a: a.activation(out=tk(1), in_=ba[:, :, 0:4], func=AF.Sigmoid), [t_psA[0]], [t_tok])
                for hh in range(4):
                    s.op("act", lambda a, hh=hh: a.activation(out=tk(7)[:, :, hh], in_=ba[:, :, 4 + hh], func=AF.Exp, bias=prm[:, 4 + hh:5 + hh]),
                         [t_psA[0], t_prm], [t_tok])
                s.op("act", lambda a: a.activation(out=tok[:, 7, :], in_=tok[:, 7, :], func=AF.Ln, bias=1.0), [t_tok], [t_tok])
                for hh in range(4):
                    s.op("dve", lambda v, hh=hh: v.tensor_scalar(out=tk(2)[:, :, hh], in0=tk(7)[:, :, hh], scalar1=prm[:, 8 + hh:9 + hh], scalar2=None, op0=ALU.mult),
                         [t_tok, t_prm], [t_tok])
                s.op("pe", lambda pe: pe.matmul(psA[1][:, 0:64], lhsT=cst[:, C_BDI:C_BDI + 128], rhs=tok[:, 2, :], start=True, stop=True),
                     [t_tok, t_cst], [t_psA[1]])
                s.op("pe", lambda pe: pe.matmul(psA[1][:, 64:128], lhsT=cst[:, C_BLK:C_BLK + 128], rhs=tok[:, 2, :], start=True, stop=True),
                     [t_tok, t_cst], [t_psA[1]])
                s.op("dve", lambda v: v.tensor_copy(out=tok[:, 3, :], in_=psA[1][:, 0:64]), [t_psA[1]], [t_tok])
                s.op("dve", lambda v: v.tensor_copy(out=tok[:, 4, :], in_=psA[1][:, 64:128]), [t_psA[1]], [t_tok])
                s.op("act", lambda a: a.activation(out=tok[:, 5, :], in_=tok[:, 3, :], func=AF.Exp), [t_tok], [t_tok])
                s.op("dve", lambda v: v.tensor_tensor(out=tok[:, 5, :], in0=tok[:, 5, :], in1=tok[:, 1, :], op=ALU.mult), [t_tok], [t_tok])
                s.op("dve", lambda v: v.tensor_tensor(out=tok[:, 6, :], in0=tok[:, 4, :], in1=tok[:, 3, :], op=ALU.subtract), [t_tok], [t_tok])
                s.op("act", lambda a: a.activation(out=tok[:, 6, :], in_=tok[:, 6, :], func=AF.Exp), [t_tok], [t_tok])
                s.op("pool", lambda g: g.memset(raw[:, 0:3], 0.0), [], [t_raw])
                s.op("pool", lambda g: g.memset(vnew[:], 0.0), [], [t_vnew])
                col = lambda plane, t, hh: tok[:, plane, t * 4 + hh: t * 4 + hh + 1]
                chk("p4a")

                for h in range(0 if "p4" not in skip else 4, 4):
                    offs = [O_DQ, O_DK, O_DV, O_DZ]
                    for j in range(4):
                        load_w(wd[:, j], w_in[l][:, offs[j] + h * 128: offs[j] + (h + 1) * 128], t_wd[j])
                    load_w(wd[:, 4], w_rep[l][:, h * 128:(h + 1) * 128], t_wd[4])
                    for j in range(3):
                        s.dma("sp", cw[:, j, :], dn_conv[l][:, j * 512 + h * 128: j * 512 + (h + 1) * 128].rearrange("i c -> c i"),
                              writes=[t_cw], allow_slow_non_contiguous=True)
                    proj_fm(wd[:, 3], t_wd[3],
                            lambda p, tp, tc: s.op("act", lambda a: a.activation(out=szT[:, tc * 512:(tc + 1) * 512], in_=p[:], func=AF.Silu), [tp], [t_sz]))
                    proj_fm(wd[:, 4], t_wd[4],
                            lambda p, tp, tc: s.op("act", lambda a: a.activation(out=gcr[:, tc * 512:(tc + 1) * 512], in_=p[:], func=AF.Exp, bias=prm[:, 4 + h:5 + h]),
                                                   [tp, t_prm], [t_gcr]))
                    s.op("act", lambda a: a.activation(out=gcr[:], in_=gcr[:], func=AF.Ln, bias=1.0), [t_gcr], [t_gcr])
                    s.op("dve", lambda v: v.tensor_scalar(out=gcr[:], in0=gcr[:], scalar1=prm[:, 8 + h:9 + h], scalar2=None, op0=ALU.mult), [t_gcr, t_prm], [t_gcr])
                    s.op("dve", lambda v: v.tensor_tensor_scan(out=gcr[:], data0=scanmask[:], data1=gcr[:], initial=0.0, op0=ALU.mult, op1=ALU.add),
                         [t_gcr, t_cst], [t_gcr])
                    s.op("act", lambda a: a.activation(out=egl[:], in_=gcr[:].rearrange("p (n c) -> p n c", c=64)[:, :, 63], func=AF.Exp), [t_gcr], [t_egl])
                    for j in range(3):
                        proj_fm(wd[:, j], t_wd[j],
                                lambda p, tp, tc: s.op("act", lambda a: a.copy(out=raw[:, 3 + tc * 512: 3 + (tc + 1) * 512], in_=p[:]), [tp], [t_raw]))
                        s.op("dve", lambda v: v.tensor_scalar(out=cv[:], in0=raw[:, 3:S + 3], scalar1=cw[:, j, 3:4], scalar2=None, op0=ALU.mult),
                             [t_raw, t_cw], [t_cv])
                        for i in range(3):
                            s.op("dve", lambda v: v.scalar_tensor_tensor(out=cv[:], in0=raw[:, i:S + i], scalar=cw[:, j, i:i + 1], in1=cv[:],
                                                                          op0=ALU.mult, op1=ALU.add),
                                 [t_raw, t_cw, t_cv], [t_cv])
                        s.op("act", lambda a: a.activation(out=cv[:], in_=cv[:], func=AF.Silu), [t_cv], [t_cv])
                        if j == 2:
                            s.op("act", lambda a: a.copy(out=vcT[:], in_=cv[:]), [t_cv], [t_vc])
                            continue
                        s.op("pool", lambda g: g.tensor_tensor(out=raw[:, 3:S + 3], in0=cv[:], in1=cv[:], op=ALU.mult), [t_cv], [t_raw])
                        for tc in range(4):
                            b = pA[0] % 2
                            pA[0] += 1
                            s.op("pe", lambda pe: pe.matmul(psA[b][:], lhsT=ones_f[:], rhs=raw[:, 3 + tc * 512: 3 + (tc + 1) * 512], start=True, stop=True),
                                 [t_raw, t_cst], [t_psA[b]])
                            s.op("act", lambda a: a.activation(out=sd[:, b, :], in_=psA[b][:], func=AF.Sqrt, bias=EPS, scale=1.0), [t_psA[b]], [t_sd[b]])
                            s.op("dve", lambda v: v.reciprocal(out=sd[:, b, :], in_=sd[:, b, :]), [t_sd[b]], [t_sd[b]])
                            dst, td = (qnT, t_qn) if j == 0 else (knT, t_kn)
                            sc = 128.0 ** -0.5 if j == 0 else 1.0
                            s.op("dve", lambda v: v.scalar_tensor_tensor(out=dst[:, tc * 512:(tc + 1) * 512], in0=cv[:, tc * 512:(tc + 1) * 512], scalar=sc,
                                                                          in1=sd[:, b, :], op0=ALU.mult, op1=ALU.mult),
                                 [t_cv, t_sd[b]], [td])
                    s.op("act", lambda a: a.activation(out=cv[:], in_=gcr[:], func=AF.Exp), [t_gcr], [t_cv])
                    s.op("dve", lambda v: v.tensor_tensor(out=qdT[:], in0=qnT[:], in1=cv[:], op=ALU.mult), [t_qn, t_cv], [t_qd])
                    s.op("dve", lambda v: v.memset(St[:], 0.0), [], [t_S])
                    s.op("dve", lambda v: v.memset(Sb[:], 0.0), [], [t_Sb])
                    chk("p4b")

                    for tg in range(4):
                        tiles = [tg * 4 + u for u in range(4)]
                        sl = lambda t: slice(t * 128, (t + 1) * 128)
                        bA = pA[0] % 2
                        pA[0] += 1
                        s.op("pe", [lambda pe, u=u, t=t: pe.matmul(psA[bA][:, u * 128:(u + 1) * 128], lhsT=knT[:, sl(t)], rhs=knT[:, sl(t)], start=True, stop=True)
                                    for u, t in enumerate(tiles)], [t_kn], [t_psA[bA]])
                        s.op("pe", [lambda pe, u=u, t=t: pe.matmul(psB[0][:, u * 128:(u + 1) * 128], lhsT=knT[:, sl(t)], rhs=qnT[:, sl(t)], start=True, stop=True)
                                    for u, t in enumerate(tiles)], [t_kn, t_qn], [t_psB[0]])
                        for u, t in enumerate(tiles):
                            s.op("dve", lambda v, u=u, t=t: v.tensor_scalar(out=tmpD[:, 1, u, :], in0=gcr[:, sl(t)], scalar1=col(3, t, h), scalar2=None,
                                                                             op0=ALU.subtract), [t_gcr, t_tok], [t_tmpD])
                        s.op("dve", lambda v: v.tensor_scalar(out=tmpD[:, 0], in0=tmpD[:, 1], scalar1=0.0, scalar2=None, op0=ALU.max), [t_tmpD], [t_tmpD])
                        s.op("dve", lambda v: v.tensor_scalar(out=tmpD[:, 1], in0=tmpD[:, 1], scalar1=0.0, scalar2=None, op0=ALU.min), [t_tmpD], [t_tmpD])
                        s.op("act", lambda a: a.activation(out=tmpD[:, 0], in_=tmpD[:, 0], func=AF.Exp, scale=-1.0), [t_tmpD], [t_tmpD])
                        s.op("act", lambda a: a.activation(out=tmpD[:, 1], in_=tmpD[:, 1], func=AF.Exp), [t_tmpD], [t_tmpD])
                        s.op("dve", lambda v: v.tensor_tensor(out=tmpD[:, 0], in0=tmpD[:, 0], in1=bds4[:], op=ALU.mult), [t_tmpD, t_cst], [t_tmpD])
                        s.op("dve", lambda v: v.tensor_tensor(out=tmpD[:, 1], in0=tmpD[:, 1], in1=bdi4[:], op=ALU.mult), [t_tmpD, t_cst], [t_tmpD])
                        chk("p4c1")
                        for u, t in enumerate(tiles):
                            s.op("dve", lambda v, u=u, t=t: v.scalar_tensor_tensor(out=X[:, 0, u, :], in0=psA[bA][:, u * 128:(u + 1) * 128], scalar=col(1, t, h),
                                                                                    in1=tmpD[:, 0, u, :], op0=ALU.mult, op1=ALU.mult),
                                 [t_psA[bA], t_tok, t_tmpD], [t_X[0]])
                        s.op("dve", lambda v: v.tensor_tensor(out=aT[:], in0=psB[0][:].rearrange("p (u n) -> p u n", u=4), in1=tmpD[:, 1], op=ALU.mult),
                             [t_psB[0], t_tmpD], [t_aT])
                        chk("p4c2")
                        s.op("pe", [lambda pe, u=u: pe.transpose(out=psTr[:, u * 128:(u + 1) * 128], in_=X[:, 0, u, :], identity=identb[:]) for u in range(4)],
                             [t_X[0], t_cst], [t_psTr])
                        s.op("act", lambda a: a.copy(out=Y[:, 0], in_=psTr[:, 0:512].rearrange("p (u n) -> p u n", u=4)), [t_psTr], [t_Y[0]])
                        s.op("dve", lambda v: v.scalar_tensor_tensor(out=R[:, 0], in0=psTr[:, 0:512].rearrange("p (u n) -> p u n", u=4), scalar=-1.0, in1=ident4[:],
                                                                      op0=ALU.mult, op1=ALU.add),
                             [t_psTr, t_cst], [t_R[0]])
                        chk("p4c")
                        cur = 0
                        for p in range(1, 6):
                            nxt = 1 - cur
                            if p < 5:
                                s.op("pe", [lambda pe, u=u: pe.matmul(psB[1][:, u * 128:(u + 1) * 128], lhsT=X[:, cur, u, :], rhs=Y[:, cur, u, :], start=True, stop=True)
                                            for u in range(4)], [t_X[cur], t_Y[cur]], [t_psB[1]])
                            bX = pA[0] % 2
                            pA[0] += 1
                            s.op("pe", [lambda pe, u=u: pe.matmul(psA[bX][:, u * 128:(u + 1) * 128], lhsT=Y[:, cur, u, :], rhs=X[:, cur, u, :], start=True, stop=True)
                                        for u in range(4)], [t_X[cur], t_Y[cur]], [t_psA[bX]])
                            s.op("dve", lambda v: v.tensor_copy(out=X[:, nxt], in_=psA[bX][:].rearrange("p (u n) -> p u n", u=4)), [t_psA[bX]], [t_X[nxt]])
                            if p < 5:
                                s.op("act", lambda a: a.copy(out=Y[:, nxt], in_=psB[1][:].rearrange("p (u n) -> p u n", u=4)), [t_psB[1]], [t_Y[nxt]])
                            s.op("pe", [lambda pe, u=u: pe.matmul(psB[0][:, u * 128:(u + 1) * 128], lhsT=X[:, nxt, u, :], rhs=R[:, cur, u, :], start=True, stop=True)
                                        for u in range(4)], [t_X[nxt], t_R[cur]], [t_psB[0]])
                            s.op("dve", lambda v: v.tensor_tensor(out=R[:, nxt], in0=psB[0][:].rearrange("p (u n) -> p u n", u=4), in1=R[:, cur], op=ALU.add),
                                 [t_psB[0], t_R[cur]], [t_R[nxt]])
                            cur = nxt
                        TT = R[:, cur]
                        t_TT = t_R[cur]
                        s.op("pe", [lambda pe, u=u, t=t: pe.transpose(out=psTr[:, u * 128:(u + 1) * 128], in_=knT[:, sl(t)], identity=identb[:]) for u, t in enumerate(tiles)],
                             [t_kn, t_cst], [t_psTr])
                        s.op("pe", [lambda pe, u=u, t=t: pe.transpose(out=psTr[:, 512 + u * 128:512 + (u + 1) * 128], in_=vcT[:, sl(t)], identity=identb[:]) for u, t in enumerate(tiles)],
                             [t_vc, t_cst], [t_psTr])
                        for u, t in enumerate(tiles):
                            s.op("dve", lambda v, u=u, t=t: v.tensor_scalar(out=kbg[:, u, :], in0=psTr[:, u * 128:(u + 1) * 128], scalar1=col(5, t, h), scalar2=None, op0=ALU.mult),
                                 [t_psTr, t_tok], [t_kbg])
                            s.op("act", lambda a, u=u, t=t: a.mul(out=kdec[:, u, :], in_=psTr[:, u * 128:(u + 1) * 128], mul=col(6, t, h)),
                                 [t_psTr, t_tok], [t_kdec])
                            s.op("dve", lambda v, u=u, t=t: v.tensor_scalar(out=vb[:, u, :], in0=psTr[:, 512 + u * 128:512 + (u + 1) * 128], scalar1=col(1, t, h), scalar2=None, op0=ALU.mult),
                                 [t_psTr, t_tok], [t_vb])
                        bU = pA[0] % 2
                        pA[0] += 1
                        s.op("pe", [lambda pe, u=u: pe.matmul(psA[bU][:, u * 128:(u + 1) * 128], lhsT=TT[:, u, :], rhs=vb[:, u, :], start=True, stop=True) for u in range(4)],
                             [t_TT, t_vb], [t_psA[bU]])
                        s.op("act", lambda a: a.copy(out=usb[:], in_=psA[bU][:].rearrange("p (u n) -> p u n", u=4)), [t_psA[bU]], [t_u])
                        s.op("pe", [lambda pe, u=u: pe.matmul(psB[1][:, u * 128:(u + 1) * 128], lhsT=kbg[:, u, :], rhs=TT[:, u, :], start=True, stop=True) for u in range(4)],
                             [t_TT, t_kbg], [t_psB[1]])
                        s.op("dve", lambda v: v.tensor_copy(out=wT[:], in_=psB[1][:].rearrange("p (u n) -> p u n", u=4)), [t_psB[1]], [t_wT])
                        chk("p4d")
                        for u, t in enumerate(tiles):
                            ob = t % 2
                            for hf in range(2):
                                n = t * 2 + hf
                                r = slice(hf * 64, hf * 64 + 64)
                                s.op("pe", lambda pe: pe.matmul(psV[:, 0, :], lhsT=wT[:, u, :], rhs=Sb[:], start=True, stop=True), [t_wT, t_Sb], [t_psV[0]])
                                s.op("dve", lambda v: v.scalar_tensor_tensor(out=vnew[r, :], in0=psV[r, 0, :], scalar=-1.0, in1=usb[r, u, :], op0=ALU.mult, op1=ALU.add),
                                     [t_u, t_psV[0]], [t_vnew])
                                s.op("pe", [lambda pe: pe.matmul(psV[:, 1 + hf, :], lhsT=qdT[:, sl(t)], rhs=Sb[:], start=True, stop=False),
                                            lambda pe: pe.matmul(psV[:, 1 + hf, :], lhsT=aT[:, u, :], rhs=vnew[:], start=False, stop=True)],
                                     [t_qd, t_Sb, t_aT, t_vnew], [t_psV[1 + hf]])
                                s.op("act", lambda a: a.copy(out=osb[r, ob, :], in_=psV[r, 1 + hf, :]), [t_psV[1 + hf]], [t_osb[ob]])
                                s.op("pe", lambda pe: pe.matmul(psV[:, 3, :], lhsT=kdec[r, u, :], rhs=vnew[r, :], start=True, stop=True),
                                     [t_kdec, t_vnew], [t_psV[3]])
                                s.op("pool", lambda g: g.tensor_scalar(out=Se[:], in0=St[:], scalar1=egl[:, n:n + 1], scalar2=None, op0=ALU.mult),
                                     [t_S, t_egl], [t_Se])
                                s.op("dve", lambda v: v.tensor_tensor(out=Sb[:], in0=psV[:, 3, :], in1=Se[:], op=ALU.add),
                                     [t_Se, t_psV[3]], [t_Sb])
                                s.op("dve", lambda v: v.tensor_tensor(out=St[:], in0=psV[:, 3, :], in1=Se[:], op=ALU.add),
                                     [t_Se, t_psV[3]], [t_S])
                            finalize_tile(ph, osb[:, ob, :], t_osb[ob], gcolD[:, 0:1], szT[:, sl(t)], t_sz, 8 + h, t, [t_gcD])
                s.barrier()

            if stop == "p4":
                if dbg:
                    for c in range(16):
                        s.dma("pool", dbg_y[:, c, :], yT[:, c, :], reads=[t_yT[c]])
                    for k in range(8):
                        s.dma("pool", dbg_h[:, k, :], hT[:, k, :], reads=[t_hT])
                s.finish("sp")
                print("instructions:", s.n_instr)
                return nc
            with ExitStack() as pes:
                sb = lambda name, shape, dt: pes.enter_context(nc.sbuf_tensor(uq(name), shape, dt))
                ps = lambda name, shape, dt: pes.enter_context(nc.psum_tensor(uq(name), shape, dt))
                ph = {}
                alloc_fin(pes, ph)
                psA = [ps("g_psA%d" % i, [128, 512], F32) for i in range(2)]
                t_psA = [TP(), TP()]
                psB = [ps("g_psB%d" % i, [128, 512], F32) for i in range(2)]
                t_psB = [TP(), TP()]
                psTr = ps("g_psT", [128, 1024], BF16)
                t_psTr = TP()
                psV = ps("g_psV", [128, 4, 128], F32)
                _tv = TP()
                t_psV = [_tv, _tv]
                pA = [0]
                wr = sb("g_wr", [128, 8, 16], BF16)
                t_wr = T()
                grT = sb("g_grT", [16, S], BF16)
                t_gr = T()
                w2f = sb("g_w2f", [16, 256], F32)
                w2 = sb("g_w2", [16, 256], BF16)
                t_w2 = T()
                gbc = sb("g_gb", [128, 2, 2], F32)
                t_gb = T()
                gcolG = sb("g_gcol", [128, 1], F32)
                t_gcG = T()
                wg = sb("g_w", [128, 6, 8, 128], BF16)
                t_wg = [T() for _ in range(6)]
                cl = sb("g_cl", [128, S], F32)
                t_cl = T()
                E = sb("g_E", [128, S], F32)
                Ei = sb("g_Ei", [128, S], F32)
                t_E, t_Ei = T(), T()
                qtT = sb("g_qtT", [128, S], BF16)
                ktT = sb("g_ktT", [128, S], BF16)
                t_qt, t_kt = T(), T()
                vtok = sb("g_v", [128, NT, 256], BF16)
                t_vt = T()
                ktok = sb("g_ktok", [128, NT, 128], BF16)
                t_ktok = T()
                szT = sb("g_szT", [128, 2, S], BF16)
                t_sz = T()
                attT = sb("g_attT", [128, 2, 2, 128], BF16)
                t_att = [T(), T()]
                eD = sb("g_eD", [128, 4, 128], F32)
                t_eD = [T() for _ in range(4)]
                St = sb("g_S", [128, 128], F32)
                Sb = sb("g_Sb", [128, 128], BF16)
                t_S, t_Sb = T(), T()
                osb = sb("g_o", [128, 2, 2, 128], F32)
                t_osb = [[T(), T()], [T(), T()]]

                def proj_fm(wt, tw, evac, m=128):
                    for tc in range(4):
                        b = pA[0] % 2
                        pA[0] += 1
                        s.op("pe", [lambda pe, k=k: pe.matmul(psA[b][0:m, :], lhsT=wt[:, k, 0:m], rhs=hT[:, k, tc * 512:(tc + 1) * 512],
                                                              start=(k == 0), stop=(k == 7)) for k in range(8)],
                             [tw, t_hT], [t_psA[b]])
                        evac(psA[b], t_psA[b], tc)

                s.dma("pool", wr[:], w_in[l][:, O_GR:O_GR + 16].rearrange("(k p) n -> p k n", p=128), writes=[t_wr])
                proj_fm(wr, t_wr, lambda p, tp, tc: s.op("act", lambda a: a.copy(out=grT[:, tc * 512:(tc + 1) * 512], in_=p[0:16, :]), [tp], [t_gr]), m=16)
                s.dma("sp", w2f[:], gla_w2[l], writes=[t_w2])
                s.op("dve", lambda v: v.tensor_copy(out=w2[:], in_=w2f[:]), [t_w2], [t_w2])
                for pr in range(2):
                    s.dma("sp", gbc[:, pr, 0:1], gla_b[l:l + 1, pr * 128:(pr + 1) * 128].rearrange("o c -> c o"), writes=[t_gb], allow_slow_non_contiguous=True)
                s.op("dve", lambda v: v.tensor_scalar(out=gbc[:, :, 1:2], in0=gbc[:, :, 0:1], scalar1=-1.0, scalar2=None, op0=ALU.mult), [t_gb], [t_gb])
                s.dma("sp", gcolG[:, 0:1], gla_norm[l:l + 1, :].rearrange("o c -> c o"), writes=[t_gcG], allow_slow_non_contiguous=True)
                sl = lambda t: slice(t * 128, (t + 1) * 128)

                for pr in range(0 if "p5" not in skip else 2, 2):
                    load_w(wg[:, 0], w_in[l][:, O_GQ + pr * 128: O_GQ + (pr + 1) * 128], t_wg[0])
                    load_w(wg[:, 1], w_in[l][:, O_GK + pr * 128: O_GK + (pr + 1) * 128], t_wg[1])
                    for j in range(2):
                        load_w(wg[:, 2 + j], w_in[l][:, O_GV + pr * 256 + j * 128: O_GV + pr * 256 + (j + 1) * 128], t_wg[2 + j])
                        load_w(wg[:, 4 + j], w_in[l][:, O_GZ + pr * 256 + j * 128: O_GZ + pr * 256 + (j + 1) * 128], t_wg[4 + j])
                    for tc in range(4):
                        b = pA[0] % 2
                        pA[0] += 1
                        s.op("pe", lambda pe: pe.matmul(psA[b][:], lhsT=w2[0:16, pr * 128:(pr + 1) * 128], rhs=grT[0:16, tc * 512:(tc + 1) * 512], start=True, stop=True),
                             [t_w2, t_gr], [t_psA[b]])
                        s.op("act", lambda a: a.activation(out=cl[:, tc * 512:(tc + 1) * 512], in_=psA[b][:], func=AF.Exp, bias=gbc[:, pr, 1:2], scale=-1.0),
                             [t_psA[b], t_gb], [t_cl])
                    s.op("act", lambda a: a.activation(out=cl[:], in_=cl[:], func=AF.Ln, bias=1.0), [t_cl], [t_cl])
                    s.op("dve", lambda v: v.tensor_tensor_scan(out=cl[:], data0=scanmask[:], data1=cl[:], initial=0.0, op0=ALU.mult, op1=ALU.add), [t_cl, t_cst], [t_cl])
                    s.op("act", lambda a: a.activation(out=E[:], in_=cl[:], func=AF.Exp, scale=-1.0 / 16.0), [t_cl], [t_E])
                    s.op("act", lambda a: a.activation(out=Ei[:], in_=cl[:], func=AF.Exp, scale=1.0 / 16.0), [t_cl], [t_Ei])
                    proj_fm(wg[:, 0], t_wg[0],
                            lambda p, tp, tc: s.op("dve", lambda v: v.scalar_tensor_tensor(out=qtT[:, tc * 512:(tc + 1) * 512], in0=p[:], scalar=0.125, in1=E[:, tc * 512:(tc + 1) * 512],
                                                                                            op0=ALU.mult, op1=ALU.mult), [tp, t_E], [t_qt]))
                    proj_fm(wg[:, 1], t_wg[1],
                            lambda p, tp, tc: s.op("dve", lambda v: v.tensor_tensor(out=ktT[:, tc * 512:(tc + 1) * 512], in0=p[:], in1=Ei[:, tc * 512:(tc + 1) * 512], op=ALU.mult),
                                                   [tp, t_Ei], [t_kt]))
                    for j in range(2):
                        proj_fm(wg[:, 4 + j], t_wg[4 + j],
                                lambda p, tp, tc, j=j: s.op("act", lambda a: a.activation(out=szT[:, j, tc * 512:(tc + 1) * 512], in_=p[:], func=AF.Silu), [tp], [t_sz]))
                    for tg in range(8):
                        b = pA[0] % 2
                        pA[0] += 1
                        fns = []
                        for u in range(2):
                            t = tg * 2 + u
                            for j in range(2):
                                for k in range(8):
                                    fns.append(lambda pe, k=k, t=t, u=u, j=j: pe.matmul(psA[b][:, u * 256 + j * 128: u * 256 + (j + 1) * 128], lhsT=hT[:, k, sl(t)],
                                                                                        rhs=wg[:, 2 + j, k, :], start=(k == 0), stop=(k == 7)))
                        s.op("pe", fns, [t_wg[2], t_wg[3], t_hT], [t_psA[b]])
                        s.op("dve", lambda v: v.tensor_copy(out=vtok[:, tg * 2:(tg + 1) * 2, :], in_=psA[b][:].rearrange("p (u n) -> p u n", u=2)), [t_psA[b]], [t_vt])
                    for tg in range(2):
                        s.op("pe", [lambda pe, u=u: pe.transpose(out=psTr[:, u * 128:(u + 1) * 128], in_=ktT[:, sl(tg * 8 + u)], identity=identb[:]) for u in range(8)],
                             [t_kt, t_cst], [t_psTr])
                        s.op("act", lambda a: a.copy(out=ktok[:, tg * 8:(tg + 1) * 8, :], in_=psTr[:].rearrange("p (u n) -> p u n", u=8)), [t_psTr], [t_ktok])
                    s.op("dve", lambda v: v.memset(St[:], 0.0), [], [t_S])
                    s.op("dve", lambda v: v.memset(Sb[:], 0.0), [], [t_Sb])
                    for t in range(NT):
                        ab = t % 2
                        bB = t % 2
                        s.op("pe", [lambda pe, hh=hh: pe.matmul(psB[hh][:, 0:128], lhsT=ktT[hh * 64:(hh + 1) * 64, sl(t)],
                                                                rhs=qtT[hh * 64:(hh + 1) * 64, sl(t)], start=True, stop=True) for hh in range(2)],
                             [t_kt, t_qt], [t_psB[0], t_psB[1]])
                        for hh in range(2):
                            s.op("dve", lambda v, hh=hh: v.tensor_tensor(out=attT[:, ab, hh, :], in0=psB[hh][:, 0:128], in1=bdi4[:, 0, :], op=ALU.mult),
                                 [t_psB[hh], t_cst], [t_att[ab]])
                        for hf in range(2):
                            n = t * 2 + hf
                            r = slice(hf * 64, hf * 64 + 64)
                            db = n % 4
                            dA = n % 2
                            s.op("pe", lambda pe: pe.matmul(psA[dA][:, 0:256], lhsT=ktok[r, t, :], rhs=vtok[r, t, :], start=True, stop=True),
                                 [t_ktok, t_vt], [t_psA[dA]])
                            ecol = E[:, n * 64 + 63: n * 64 + 64]
                            s.op("act", lambda a: a.mul(out=eD[0:64, db, :], in_=psA[dA][0:64, 0:128], mul=ecol[0:64, :]), [t_psA[dA], t_E], [t_eD[db]])
                            s.op("act", lambda a: a.mul(out=eD[64:128, db, :], in_=psA[dA][64:128, 128:256], mul=ecol[64:128, :]), [t_psA[dA], t_E], [t_eD[db]])
                            for hh in range(2):
                                hr = slice(hh * 64, hh * 64 + 64)
                                s.op("pe", [lambda pe: pe.matmul(psV[:, hf * 2 + hh, :], lhsT=qtT[hr, sl(t)], rhs=Sb[hr, :], start=True, stop=False),
                                            lambda pe: pe.matmul(psV[:, hf * 2 + hh, :], lhsT=attT[:, ab, hh, :], rhs=vtok[:, t, hh * 128:(hh + 1) * 128], start=False, stop=True)],
                                     [t_qt, t_Sb, t_att[ab], t_vt], [t_psV[hf]])
                                s.op("act", lambda a: a.copy(out=osb[r, ab, hh, :], in_=psV[r, hf * 2 + hh, :]), [t_psV[hf]], [t_osb[ab][hh]])
                            s.op("dve", lambda v: v.scalar_tensor_tensor(out=St[:], in0=St[:], scalar=ecol, in1=eD[:, db, :], op0=ALU.mult, op1=ALU.add),
                                 [t_S, t_E, t_eD[db]], [t_S])
                            s.op("dve", lambda v: v.tensor_copy(out=Sb[:], in_=St[:]), [t_S], [t_Sb])
                        for hh in range(2):
                            finalize_tile(ph, osb[:, ab, hh, :], t_osb[ab][hh], gcolG[:, 0:1], szT[:, hh, sl(t)], t_sz, 12 + pr * 2 + hh, t, [t_gcG])
                s.barrier()

            if stop == "p5":
                if dbg:
                    for c in range(16):
                        s.dma("pool", dbg_y[:, c, :], yT[:, c, :], reads=[t_yT[c]])
                    for k in range(8):
                        s.dma("pool", dbg_h[:, k, :], hT[:, k, :], reads=[t_hT])
                s.finish("sp")
                print("instructions:", s.n_instr)
                return nc
            if dbg and l == 0:
                for c in range(16):
                    s.dma("pool", dbg_y[:, c, :], yT[:, c, :], reads=[t_yT[c]])

            with ExitStack() as pes:
                sb = lambda name, shape, dt: pes.enter_context(nc.sbuf_tensor(uq(name), shape, dt))
                ps = lambda name, shape, dt: pes.enter_context(nc.psum_tensor(uq(name), shape, dt))
                wo = hT[:].rearrange("p k s -> p (k s)").rearrange("p (c n) -> p c n", c=16)
                wgt = sb("o_wg", [128, 8, D], BF16)
                wpp = sb("o_wp", [128, 2, D], BF16)
                t_wo, t_wgt, t_wpp = t_hT, T(), T()
                pTb = sb("o_pT", [128, 2, S], BF16)
                t_pT = T()
                gb2 = sb("o_gb2", [128, D], F32)
                t_gb2 = T()
                xb_ = sb("o_x", [128, 2, D], F32)
                t_x = [T(), T()]
                x1 = sb("o_x1", [128, 2, D], F32)
                t_x1 = [T(), T()]
                x1b = sb("o_x1b", [128, 2, D], BF16)
                t_x1b = [T(), T()]
                x1T = sb("o_x1T", [128, 2, D], BF16)
                t_x1T = [T(), T()]
                gate = sb("o_gate", [128, D], F32)
                t_gate = T()
                mm_ = sb("o_m", [128, D], F32)
                t_m = T()
                tmp = sb("o_tmp", [128, D], F32)
                t_tmp = T()
                junk = sb("o_junk", [128, D], BF16)
                t_j = T()
                st = sb("o_st", [128, 2, 8], F32)
                t_st = [T(), T()]
                psY = ps("o_psY", [128, 1024], F32)
                psG = ps("o_psG", [128, 1024], F32)
                psP = ps("o_psP", [128, 1024], F32)
                psX = ps("o_psX", [128, 1024], BF16)
                t_psY, t_psG, t_psP, t_psX = TP(), TP(), TP(), TP()
                load_w(wo, w_out[l], t_wo)
                load_w(wgt[:], ple_gate[l], t_wgt)
                load_w(wpp[:], ple_proj[l], t_wpp)
                load_w(pTb[:], pT_in[l], t_pT)
                s.dma("sp", gb1[:], post_gain[l:l + 1, :].partition_broadcast(128), writes=[t_gb1])
                s.dma("sp", gb2[:], ple_norm[l:l + 1, :].partition_broadcast(128), writes=[t_gb2])
                for t in range(NT):
                    b = t % 2
                    tsl = slice(t * 128, (t + 1) * 128)
                    s.dma("sp", xb_[:, b, :], x_src[tsl, :], writes=[t_x[b]])
                    fns = []
                    for nh in range(2):
                        for c in range(16):
                            fns.append(lambda pe, c=c, nh=nh: pe.matmul(psY[:, nh * 512:(nh + 1) * 512], lhsT=yT[:, c, tsl], rhs=wo[:, c, nh * 512:(nh + 1) * 512],
                                                                        start=(c == 0), stop=(c == 15)))
                    s.op("pe", fns, t_yT + [t_wo], [t_psY])
                    s.op("act", lambda a: a.activation(out=junk[:], in_=psY[:], func=AF.Square, accum_out=st[:, b, 0:1]), [t_psY], [t_j, t_st[b]])
                    s.op("act", lambda a: a.activation(out=st[:, b, 1:2], in_=st[:, b, 0:1], func=AF.Sqrt, bias=EPS, scale=1.0 / D), [t_st[b]], [t_st[b]])
                    s.op("dve", lambda v: v.reciprocal(out=st[:, b, 2:3], in_=st[:, b, 1:2]), [t_st[b]], [t_st[b]])
                    s.op("dve", lambda v: v.scalar_tensor_tensor(out=tmp[:], in0=psY[:], scalar=st[:, b, 2:3], in1=gb1[:], op0=ALU.mult, op1=ALU.mult),
                         [t_psY, t_st[b], t_gb1], [t_tmp])
                    s.op("pool", lambda g: g.tensor_tensor(out=x1[:, b, :], in0=tmp[:], in1=xb_[:, b, :], op=ALU.add), [t_tmp, t_x[b]], [t_x1[b]])
                    s.op("act", lambda a: a.copy(out=x1b[:, b, :], in_=x1[:, b, :]), [t_x1[b]], [t_x1b[b]])
                    s.op("pe", [lambda pe, k=k: pe.transpose(out=psX[:, k * 128:(k + 1) * 128], in_=x1b[:, b, k * 128:(k + 1) * 128], identity=identb[:]) for k in range(8)],
                         [t_x1b[b], t_cst], [t_psX])
                    s.op("dve", lambda v: v.tensor_copy(out=x1T[:, b, :], in_=psX[:]), [t_psX], [t_x1T[b]])
                    fns = []
                    for nh in range(2):
                        for k in range(8):
                            fns.append(lambda pe, k=k, nh=nh: pe.matmul(psG[:, nh * 512:(nh + 1) * 512], lhsT=x1T[:, b, k * 128:(k + 1) * 128], rhs=wgt[:, k, nh * 512:(nh + 1) * 512],
                                                                        start=(k == 0), stop=(k == 7)))
                    s.op("pe", fns, [t_x1T[b], t_wgt], [t_psG])
                    s.op("act", lambda a: a.activation(out=gate[:], in_=psG[:], func=AF.Sigmoid), [t_psG], [t_gate])
                    fns = []
                    for nh in range(2):
                        for k in range(2):
                            fns.append(lambda pe, k=k, nh=nh: pe.matmul(psP[:, nh * 512:(nh + 1) * 512], lhsT=pTb[:, k, tsl], rhs=wpp[:, k, nh * 512:(nh + 1) * 512],
                                                                        start=(k == 0), stop=(k == 1)))
                    s.op("pe", fns, [t_pT, t_wpp], [t_psP])
                    s.op("dve", lambda v: v.tensor_tensor(out=mm_[:], in0=psP[:], in1=gate[:], op=ALU.mult), [t_psP, t_gate], [t_m])
                    s.op("act", lambda a: a.activation(out=junk[:], in_=mm_[:], func=AF.Square, accum_out=st[:, b, 4:5]), [t_m], [t_j, t_st[b]])
                    s.op("act", lambda a: a.activation(out=st[:, b, 5:6], in_=st[:, b, 4:5], func=AF.Sqrt, bias=EPS, scale=1.0 / D), [t_st[b]], [t_st[b]])
                    s.op("dve", lambda v: v.reciprocal(out=st[:, b, 6:7], in_=st[:, b, 5:6]), [t_st[b]], [t_st[b]])
                    s.op("dve", lambda v: v.scalar_tensor_tensor(out=mm_[:], in0=mm_[:], scalar=st[:, b, 6:7], in1=gb2[:], op0=ALU.mult, op1=ALU.mult),
                         [t_m, t_st[b], t_gb2], [t_m])
                    s.op("pool", lambda g: g.tensor_tensor(out=x1[:, b, :], in0=x1[:, b, :], in1=mm_[:], op=ALU.add), [t_m, t_x1[b]], [t_x1[b]])
                    s.dma("sp", out[tsl, :], x1[:, b, :], reads=[t_x1[b]])
                s.barrier()
            x_src = out
        s.finish("sp")
        print("instructions:", s.n_instr, {k: v for k, v in s.cnt.items() if v})
    return nc


_CACHE = {}


def prep_inputs(inputs):
    f = lambda a: np.ascontiguousarray(np.asarray(a, dtype=np.float32))
    x = f(inputs["x"])
    p = f(inputs["p"])
    w_in = f(inputs["w_in"])
    w_rep = np.ascontiguousarray(np.repeat(w_in[:, :, O_DA:O_DA + 4], 128, axis=2))
    att_l = np.ascontiguousarray(np.stack([f(inputs["att_lq1"]), f(inputs["att_lk1"]), f(inputs["att_lq2"]), f(inputs["att_lk2"])], axis=1))
    shared = {
        "w_in": w_in, "w_rep": w_rep, "w_out": f(inputs["w_out"]), "ple_gate": f(inputs["ple_gate"]),
        "ple_proj": f(inputs["ple_proj"]), "pre_gain": f(inputs["pre_gain"]), "post_gain": f(inputs["post_gain"]),
        "ple_norm": f(inputs["ple_norm"]), "att_l": att_l, "att_subln": f(inputs["att_subln"]),
        "dn_conv": f(inputs["dn_conv"]), "dn_a_log": f(inputs["dn_a_log"]), "dn_dt_bias": f(inputs["dn_dt_bias"]),
        "dn_norm": f(inputs["dn_norm"]), "gla_w2": f(inputs["gla_w2"]), "gla_b": f(inputs["gla_b"]),
        "gla_norm": f(inputs["gla_norm"]), "consts": make_consts(),
    }
    maps = []
    for b in range(x.shape[0]):
        m = dict(shared)
        m["x"] = np.ascontiguousarray(x[b])
        m["pT"] = np.ascontiguousarray(p[:, b].transpose(0, 2, 1))
        maps.append(m)
    return maps


def kernel(**inputs):
    maps = prep_inputs(inputs)
    if "nc" not in _CACHE:
        _CACHE["nc"] = build_program()
    res = run_bass_kernel_spmd(_CACHE["nc"], maps, core_ids=list(range(8)))
    return np.stack([np.asarray(r["out"], dtype=np.float32) for r in res.results], axis=0)
```

```python
import math
from contextlib import ExitStack
import numpy as np
import concourse.bass as bass
import concourse.mybir as mybir
from concourse.bass_utils import run_bass_kernel_spmd

F32 = mybir.dt.float32
BF16 = mybir.dt.bfloat16
AF = mybir.ActivationFunctionType
ALU = mybir.AluOpType

S = 2048
D = 1024
NT = 16
DEPTH = 2
D_IN = 7704
EPS = 1e-6
O_AQ, O_AK, O_AV, O_AZ = 0, 1024, 2048, 3072
O_DQ, O_DK, O_DV, O_DZ, O_DB, O_DA = 4096, 4608, 5120, 5632, 6144, 6148
O_GQ, O_GK, O_GV, O_GZ, O_GR = 6152, 6408, 6664, 7176, 7688
C_ID, C_MATT, C_BDI, C_BDS, C_BLK, C_ALI, C_NEG, NC_CONST = 0, 128, 256, 384, 512, 640, 768, 896
ATT_W = [128] * 8


class T:
    __slots__ = ("w", "r", "x")

    def __init__(self, x=False):
        self.w = None
        self.r = {}
        self.x = x


def TP():
    return T(True)


class Sched:
    def __init__(self, nc, es, n_dma_sems=8):
        self.nc = nc
        self.eng = {"pe": nc.tensor, "act": nc.scalar, "dve": nc.vector,
                    "pool": nc.gpsimd, "sp": nc.sync}
        self.sem = {}
        self.cnt = {}
        for k in self.eng:
            self.sem[k] = es.enter_context(nc.semaphore("s_" + k))
            self.cnt[k] = 0
        self.seen = {k: {} for k in self.eng}
        self.dq = {}
        for q in ("sp", "pool"):
            sems = []
            for i in range(n_dma_sems):
                key = "d_%s%d" % (q, i)
                self.sem[key] = es.enter_context(nc.semaphore(key))
                self.cnt[key] = 0
                sems.append(key)
            self.dq[q] = [sems, 0]
        self.n_instr = 0

    def _wait(self, e, ev):
        key, val = ev
        if key == "pe" and e == "pe":
            return
        if self.seen[e].get(key, 0) >= val:
            return
        self.eng[e].wait_ge(self.sem[key], val)
        self.seen[e][key] = val

    def _deps(self, reads, writes):
        evs = {}
        for t in reads:
            if t.w is not None and evs.get(t.w[0], 0) < t.w[1]:
                evs[t.w[0]] = t.w[1]
        for t in writes:
            if t.w is not None and evs.get(t.w[0], 0) < t.w[1]:
                evs[t.w[0]] = t.w[1]
            for k, v in t.r.items():
                if evs.get(k, 0) < v:
                    evs[k] = v
        return evs

    def _commit(self, ev, reads, writes):
        k, v = ev
        for t in reads:
            if t.r.get(k, 0) < v:
                t.r[k] = v
        for t in writes:
            t.w = ev
            t.r = {}

    def op(self, e, fns, reads=(), writes=()):
        if callable(fns):
            fns = [fns]
        writes = list(writes) + [t for t in reads if t.x]
        reads = [t for t in reads if not t.x]
        for k, v in self._deps(reads, writes).items():
            self._wait(e, (k, v))
        h = self.eng[e]
        ins = None
        for f in fns:
            ins = f(h)
            self.n_instr += 1
        self.cnt[e] += 1
        ins.then_inc(self.sem[e], 1)
        ev = (e, self.cnt[e])
        self._commit(ev, reads, writes)
        return ev

    def dma(self, q, out, in_, reads=(), writes=(), **kw):
        sems, idx = self.dq[q]
        key = sems[idx]
        self.dq[q][1] = (idx + 1) % len(sems)
        if self.cnt[key] > 0:
            self._wait(q, (key, self.cnt[key]))
        for k, v in self._deps(reads, writes).items():
            self._wait(q, (k, v))
        ins = self.eng[q].dma_start(out=out, in_=in_, **kw)
        self.n_instr += 1
        self.cnt[key] += 16
        ins.then_inc(self.sem[key], 16)
        ev = (key, self.cnt[key])
        self._commit(ev, reads, writes)
        return ev

    def barrier(self):
        for e in self.eng:
            for k, v in self.cnt.items():
                if v > 0 and k != e:
                    self._wait(e, (k, v))

    def finish(self, e="sp"):
        for k, v in self.cnt.items():
            if v > 0 and k != e:
                self._wait(e, (k, v))


def make_consts():
    c = np.zeros((128, NC_CONST), np.float32)
    i = np.arange(128)
    c[:, C_ID:C_ID + 128] = np.eye(128, dtype=np.float32)
    c[:, C_MATT:C_MATT + 128] = (i[:, None] <= i[None, :]).astype(np.float32)
    same = (i[:, None] // 64) == (i[None, :] // 64)
    c[:, C_BDI:C_BDI + 128] = ((i[:, None] <= i[None, :]) & same).astype(np.float32)
    c[:, C_BDS:C_BDS + 128] = ((i[None, :] < i[:, None]) & same).astype(np.float32)
    c[:, C_BLK:C_BLK + 128] = same.astype(np.float32)
    c[:, C_NEG:C_NEG + 128] = np.where(i[:, None] > i[None, :], -30000.0, 0.0).astype(np.float32)
    slopes = 2.0 ** (-8.0 * np.arange(1, 9) / 8.0)
    for h in range(8):
        for dd in range(16):
            c[:, C_ALI + h * 16 + dd] = slopes[h] * (i - 127 - 128 * dd)
    return c


def build_program(depth=DEPTH, dbg=False, stop=None, skip=()):
    try:
        return _build_program(depth, dbg, stop, skip)
    except StopBuild as e:
        return e.nc


class StopBuild(Exception):
    def __init__(self, nc):
        self.nc = nc


def _build_program(depth=DEPTH, dbg=False, stop=None, skip=()):
    nc = bass.Bass("TRN2", target_bir_lowering=False)
    dr = lambda name, shape, kind="ExternalInput", dt=F32: nc.dram_tensor(name, shape, dt, kind=kind).ap()
    x_in = dr("x", [S, D])
    pT_in = dr("pT", [DEPTH, 256, S])
    w_in = dr("w_in", [DEPTH, D, D_IN])
    w_rep = dr("w_rep", [DEPTH, D, 512])
    w_out = dr("w_out", [DEPTH, 2048, D])
    ple_gate = dr("ple_gate", [DEPTH, D, D])
    ple_proj = dr("ple_proj", [DEPTH, 256, D])
    pre_gain = dr("pre_gain", [DEPTH, D])
    post_gain = dr("post_gain", [DEPTH, D])
    ple_norm = dr("ple_norm", [DEPTH, D])
    att_l = dr("att_l", [DEPTH, 4, 64])
    att_subln = dr("att_subln", [DEPTH, 128])
    dn_conv = dr("dn_conv", [DEPTH, 4, 1536])
    dn_a_log = dr("dn_a_log", [DEPTH, 4])
    dn_dt_bias = dr("dn_dt_bias", [DEPTH, 4])
    dn_norm = dr("dn_norm", [DEPTH, 128])
    gla_w2 = dr("gla_w2", [DEPTH, 16, 256])
    gla_b = dr("gla_b", [DEPTH, 256])
    gla_norm = dr("gla_norm", [DEPTH, 128])
    consts_in = dr("consts", [128, NC_CONST])
    out = dr("out", [S, D], kind="ExternalOutput")
    dbg_y = dr("dbg_y", [128, 16, S], kind="ExternalOutput") if dbg else None
    dbg_h = dr("dbg_h", [128, 8, S], kind="ExternalOutput") if dbg else None

    _uq = [0]


    def uq(name):
        _uq[0] += 1
        return "%s_%d" % (name, _uq[0])

    with ExitStack() as es:
        s = Sched(nc, es)
        sbp = lambda name, shape, dt: es.enter_context(nc.sbuf_tensor(uq(name), shape, dt))
        hT = sbp("hT", [128, 8, S], BF16)
        t_hT = T()
        yT = sbp("yT", [128, 16, S], BF16)
        t_yT = [T() for _ in range(16)]
        cst = sbp("cst", [128, NC_CONST], F32)
        t_cst = T()
        identb = sbp("identb", [128, 128], BF16)
        ident4 = sbp("ident4", [128, 4, 128], BF16)
        matt2 = sbp("matt2", [128, 2, 128], BF16)
        negb = sbp("negb", [128, 128], BF16)
        bdi4 = sbp("bdi4", [128, 4, 128], BF16)
        bds4 = sbp("bds4", [128, 4, 128], F32)
        ones_f = sbp("ones_f", [128, 128], F32)
        scanmask = sbp("scanmask", [128, S], F32)
        gb0 = sbp("gb0", [128, D], F32)
        t_gb0 = T()
        gb1 = sbp("gb1", [128, D], F32)
        t_gb1 = T()
        small = sbp("small", [128, 64], F32)
        t_small = T()

        ident = cst[:, C_ID:C_ID + 128]
        s.dma("sp", cst[:], consts_in, writes=[t_cst])
        s.op("dve", lambda v: v.tensor_copy(out=identb[:], in_=ident), [t_cst], [t_cst])
        for u in range(4):
            s.op("dve", lambda v, u=u: v.tensor_copy(out=ident4[:, u, :], in_=ident), [t_cst], [t_cst])
            s.op("dve", lambda v, u=u: v.tensor_copy(out=bdi4[:, u, :], in_=cst[:, C_BDI:C_BDI + 128]), [t_cst], [t_cst])
            s.op("dve", lambda v, u=u: v.tensor_copy(out=bds4[:, u, :], in_=cst[:, C_BDS:C_BDS + 128]), [t_cst], [t_cst])
        for u in range(2):
            s.op("dve", lambda v, u=u: v.tensor_copy(out=matt2[:, u, :], in_=cst[:, C_MATT:C_MATT + 128]), [t_cst], [t_cst])
        s.op("dve", lambda v: v.tensor_copy(out=negb[:], in_=cst[:, C_NEG:C_NEG + 128]), [t_cst], [t_cst])
        s.op("pool", lambda g: g.memset(ones_f[:], 1.0), [], [t_cst])
        s.op("pool", lambda g: g.memset(scanmask[:], 1.0), [], [t_cst])
        s.op("pool", lambda g: g.memset(scanmask[:].rearrange("p (c k) -> p c k", k=64)[:, :, 0:1], 0.0), [], [t_cst])

        def chk(name):
            if stop == name:
                s.finish("sp")
                print("STOP at", name, "instructions:", s.n_instr)
                raise StopBuild(nc)

        def load_w(dst, src2d, tw):
            src = src2d.rearrange("(k p) n -> p k n", p=128)
            nk = src.shape[1]
            per = max(1, 2048 // max(1, src.shape[2] * 4 // 512))
            per = min(per, nk)
            if src.shape[2] >= 1024:
                per = 1
            for k0 in range(0, nk, per):
                s.dma("pool", dst[:, k0:k0 + per], src[:, k0:k0 + per], writes=[tw])

        def finalize_tile(ph, o_ap, t_o, gaincol, szT_ap, t_sz, mix, t, extra_reads=()):
            junk, t_junk, st, t_st, an, t_an, psT, t_psT = ph["fin"]
            i = ph["fin_i"] = ph.get("fin_i", 0) + 1
            b = i % 2
            s.op("dve", lambda v: v.scalar_tensor_tensor(out=junk[:, b, :], in0=o_ap, scalar=1.0, in1=o_ap, op0=ALU.mult, op1=ALU.mult,
                                                          accum_out=st[:, b, 0:1]),
                 [t_o], [t_junk[b], t_st[b]])
            s.op("act", lambda a: a.activation(out=st[:, b, 1:2], in_=st[:, b, 0:1], func=AF.Ln, bias=EPS, scale=1.0 / 128.0),
                 [t_st[b]], [t_st[b]])
            s.op("act", lambda a: a.activation(out=st[:, b, 2:3], in_=st[:, b, 1:2], func=AF.Exp, scale=-0.5), [t_st[b]], [t_st[b]])
            s.op("dve", lambda v: v.tensor_scalar(out=an[:, b, :], in0=o_ap, scalar1=st[:, b, 2:3], scalar2=None, op0=ALU.mult),
                 [t_o, t_st[b]], [t_an[b]])
            s.op("pe", lambda pe: pe.transpose(out=psT[:, b * 128:(b + 1) * 128], in_=an[:, b, :], identity=identb[:]),
                 [t_an[b], t_cst], [t_psT[b]])
            s.op("dve", lambda v: v.scalar_tensor_tensor(out=yT[:, mix, t * 128:(t + 1) * 128], in0=psT[:, b * 128:(b + 1) * 128],
                                                          scalar=gaincol, in1=szT_ap, op0=ALU.mult, op1=ALU.mult),
                 [t_psT[b], t_sz] + list(extra_reads), [t_yT[mix]])

        def alloc_fin(pes, ph):
            sb = lambda name, shape, dt: pes.enter_context(nc.sbuf_tensor(uq(name), shape, dt))
            junk = sb("fjunk", [128, 2, 128], BF16)
            st = sb("fst", [128, 2, 4], F32)
            an = sb("fan", [128, 2, 128], BF16)
            psT = pes.enter_context(nc.psum_tensor(uq("fpsT"), [128, 1024], BF16))
            _tp = TP()
            ph["fin"] = (junk, [T(), T()], st, [T(), T()], an, [T(), T()], psT, [_tp, _tp])

        x_src = x_in
        for l in range(depth):
            lam_init = 0.8 - 0.6 * math.exp(-0.3 * l)
            with ExitStack() as pes:
                sb = lambda name, shape, dt: pes.enter_context(nc.sbuf_tensor(uq(name), shape, dt))
                xb_ = sb("p1x", [128, 2, D], F32)
                t_x = [T(), T()]
                hb = sb("p1h", [128, 2, D], BF16)
                t_hb = [T(), T()]
                junk = sb("p1j", [128, D], BF16)
                t_j = T()
                st = sb("p1s", [128, 2, 4], F32)
                t_st = [T(), T()]
                psT = pes.enter_context(nc.psum_tensor(uq("p1ps"), [128, 2, 1024], BF16))
                t_ps = [TP(), TP()]
                s.dma("sp", gb0[:], pre_gain[l:l + 1, :].partition_broadcast(128), writes=[t_gb0])
                for t in range(NT):
                    b = t % 2
                    s.dma("sp", xb_[:, b, :], x_src[t * 128:(t + 1) * 128, :], writes=[t_x[b]])
                    s.op("act", lambda a: a.activation(out=junk[:], in_=xb_[:, b, :], func=AF.Square, accum_out=st[:, b, 0:1]),
                         [t_x[b]], [t_j, t_st[b]])
                    s.op("act", lambda a: a.activation(out=st[:, b, 1:2], in_=st[:, b, 0:1], func=AF.Ln, bias=EPS, scale=1.0 / D),
                         [t_st[b]], [t_st[b]])
                    s.op("act", lambda a: a.activation(out=st[:, b, 2:3], in_=st[:, b, 1:2], func=AF.Exp, scale=-0.5), [t_st[b]], [t_st[b]])
                    s.op("dve", lambda v: v.scalar_tensor_tensor(out=hb[:, b, :], in0=xb_[:, b, :], scalar=st[:, b, 2:3], in1=gb0[:],
                                                                  op0=ALU.mult, op1=ALU.mult),
                         [t_x[b], t_st[b], t_gb0], [t_hb[b]])
                    s.op("pe", [lambda pe, k=k: pe.transpose(out=psT[:, b, k * 128:(k + 1) * 128], in_=hb[:, b, k * 128:(k + 1) * 128],
                                                             identity=identb[:]) for k in range(8)],
                         [t_hb[b], t_cst], [t_ps[b]])
                    s.op("act", lambda a: a.copy(out=hT[:, :, t * 128:(t + 1) * 128],
                                                 in_=psT[:, b, :].rearrange("p (k n) -> p k n", k=8)),
                         [t_ps[b]], [t_hT])
                s.barrier()

            if stop == "p1":
                if dbg:
                    for c in range(16):
                        s.dma("pool", dbg_y[:, c, :], yT[:, c, :], reads=[t_yT[c]])
                    for k in range(8):
                        s.dma("pool", dbg_h[:, k, :], hT[:, k, :], reads=[t_hT])
                s.finish("sp")
                print("instructions:", s.n_instr)
                return nc
            with ExitStack() as pes:
                sb = lambda name, shape, dt: pes.enter_context(nc.sbuf_tensor(uq(name), shape, dt))
                ps = lambda name, shape, dt: pes.enter_context(nc.psum_tensor(uq(name), shape, dt))
                wbuf = sb("a_w", [128, 2, 4, 8, 128], BF16)
                t_w = [[T() for _ in range(4)] for _ in range(2)]
                qT = sb("a_qT", [128, 2, S], BF16)
                t_q = [T(), T()]
                kT = sb("a_kT", [128, 2, S], BF16)
                t_k = [T(), T()]
                szT = sb("a_szT", [128, 2, S], BF16)
                t_sz = [T(), T()]
                vaug = sb("a_v", [128, 2, NT, 130], BF16)
                t_v = [T(), T()]
                pt = sb("a_p", [128, 3, 2, 128], BF16)
                t_pt = [T(), T(), T()]
                NF = 8
                osb = sb("a_o", [128, NF, 2, 128], F32)
                t_osb = [T() for _ in range(NF)]
                rr = sb("a_rr", [128, NF, 4], F32)
                t_rr = [T() for _ in range(NF)]
                fjunk = sb("a_fj", [128, NF, 128], BF16)
                t_fj = [T() for _ in range(NF)]
                fst = sb("a_fst", [128, NF, 4], F32)
                t_fst = [T() for _ in range(NF)]
                fan = sb("a_fan", [128, NF, 128], BF16)
                t_fan = [T() for _ in range(NF)]
                lq = sb("a_lq", [128, 4, 64], F32)
                t_lq = T()
                ztmp = sb("a_zt", [128, 512], F32)
                t_zt = T()
                gcol = sb("a_gc", [128, 2], F32)
                t_gc = T()
                psA = ps("a_psA", [128, 512], F32)
                t_psA = TP()
                psS = [ps("a_psS%d" % i, [128, 2, 512], F32) for i in range(2)]
                t_psS = [TP(), TP()]
                psO = [ps("a_psO%d" % i, [128, 2, 256], F32) for i in range(2)]
                t_psO = [TP(), TP()]
                psF = ps("a_psF", [128, 1024], BF16)
                t_psF = TP()

                s.dma("sp", lq[:], att_l[l:l + 1].partition_broadcast(128), writes=[t_lq])
                s.op("dve", lambda v: v.tensor_tensor(out=lq[:, 0, :], in0=lq[:, 0, :], in1=lq[:, 1, :], op=ALU.mult), [t_lq], [t_lq])
                s.op("dve", lambda v: v.tensor_tensor(out=lq[:, 2, :], in0=lq[:, 2, :], in1=lq[:, 3, :], op=ALU.mult), [t_lq], [t_lq])
                s.op("act", lambda a: a.activation(out=lq[:, 1, :], in_=lq[:, 0, :], func=AF.Copy, accum_out=small[:, 1:2]), [t_lq], [t_lq, t_small])
                s.op("act", lambda a: a.activation(out=lq[:, 3, :], in_=lq[:, 2, :], func=AF.Copy, accum_out=small[:, 2:3]), [t_lq], [t_lq, t_small])
                s.op("act", lambda a: a.activation(out=small[:, 3:5], in_=small[:, 1:3], func=AF.Exp), [t_small], [t_small])
                s.op("dve", lambda v: v.tensor_tensor(out=small[:, 5:6], in0=small[:, 4:5], in1=small[:, 3:4], op=ALU.subtract), [t_small], [t_small])
                s.op("dve", lambda v: v.tensor_scalar(out=small[:, 0:1], in0=small[:, 5:6], scalar1=-lam_init, scalar2=None, op0=ALU.add), [t_small], [t_small])
                s.dma("sp", gcol[:, 0:1], att_subln[l:l + 1, :].rearrange("o c -> c o"), writes=[t_gc], allow_slow_non_contiguous=True)
                s.op("dve", lambda v: v.tensor_scalar(out=gcol[:, 1:2], in0=gcol[:, 0:1], scalar1=1.0 - lam_init, scalar2=None, op0=ALU.mult), [t_gc], [t_gc])
                for hb in range(2):
                    s.op("pool", lambda g, hb=hb: g.memset(vaug[:, hb, :, 128:130], 1.0), [], [t_v[hb]])

                offs = [O_AQ, O_AK, O_AV, O_AZ]

                def proj_gen(h):
                    hb = h % 2
                    for j in range(4):
                        load_w(wbuf[:, hb, j], w_in[l][:, offs[j] + h * 128: offs[j] + (h + 1) * 128], t_w[hb][j])
                    yield
                    for j, dst, td in ((0, qT, t_q), (1, kT, t_k), (3, szT, t_sz)):
                        for tc in range(4):
                            s.op("pe", [lambda pe, k=k: pe.matmul(psA[:], lhsT=wbuf[:, hb, j, k, :], rhs=hT[:, k, tc * 512:(tc + 1) * 512],
                                                                  start=(k == 0), stop=(k == 7)) for k in range(8)],
                                 [t_w[hb][j], t_hT], [t_psA])
                            if j == 0:
                                s.op("dve", lambda v: v.tensor_copy(out=dst[:, hb, tc * 512:(tc + 1) * 512], in_=psA[:]), [t_psA], [td[hb]])
                            elif j == 1:
                                s.op("dve", lambda v: v.tensor_copy(out=dst[:, hb, tc * 512:(tc + 1) * 512], in_=psA[:]), [t_psA], [td[hb]])
                            else:
                                s.op("act", lambda a: a.activation(out=ztmp[:], in_=psA[:], func=AF.Exp, scale=-1.0), [t_psA], [t_zt])
                                s.op("act", lambda a: a.activation(out=ztmp[:], in_=ztmp[:], func=AF.Ln, bias=1.0), [t_zt], [t_zt])
                                s.op("act", lambda a: a.activation(out=ztmp[:], in_=ztmp[:], func=AF.Exp, scale=-1.0), [t_zt], [t_zt])
                                s.op("dve", lambda v: v.tensor_tensor(out=dst[:, hb, tc * 512:(tc + 1) * 512], in0=psA[:], in1=ztmp[:], op=ALU.mult),
                                     [t_psA, t_zt], [td[hb]])
                            yield
                    for tg in range(4):
                        fns = []
                        for u in range(4):
                            t = tg * 4 + u
                            for k in range(8):
                                fns.append(lambda pe, k=k, t=t, u=u: pe.matmul(psA[:, u * 128:(u + 1) * 128], lhsT=hT[:, k, t * 128:(t + 1) * 128],
                                                                              rhs=wbuf[:, hb, 2, k, :], start=(k == 0), stop=(k == 7)))
                        s.op("pe", fns, [t_w[hb][2], t_hT], [t_psA])
                        s.op("dve", lambda v: v.tensor_copy(out=vaug[:, hb, tg * 4:(tg + 1) * 4, 0:128],
                                                            in_=psA[:].rearrange("p (u n) -> p u n", u=4)), [t_psA], [t_v[hb]])
                        yield

                fin_i = [0]

                def fin_stages(h, t, ob):
                    hb = h % 2
                    f = fin_i[0] % NF
                    fin_i[0] += 1
                    tsl = slice(t * 128, (t + 1) * 128)

                    def st1():
                        s.op("dve", lambda v: v.reciprocal(out=rr[:, f, 0:2], in_=psO[ob][:, :, 128]), [t_psO[ob]], [t_rr[f]])
                        s.op("dve", lambda v: v.tensor_tensor(out=rr[:, f, 2:3], in0=rr[:, f, 1:2], in1=small[:, 0:1], op=ALU.mult),
                             [t_rr[f], t_small], [t_rr[f]])
                        s.op("dve", lambda v: v.tensor_scalar(out=osb[:, f, 1, :], in0=psO[ob][:, 1, 0:128], scalar1=rr[:, f, 2:3], scalar2=None, op0=ALU.mult),
                             [t_psO[ob], t_rr[f]], [t_osb[f]])
                        s.op("dve", lambda v: v.scalar_tensor_tensor(out=osb[:, f, 0, :], in0=psO[ob][:, 0, 0:128], scalar=rr[:, f, 0:1],
                                                                      in1=osb[:, f, 1, :], op0=ALU.mult, op1=ALU.add),
                             [t_psO[ob], t_rr[f], t_osb[f]], [t_osb[f]])
                        s.op("dve", lambda v: v.scalar_tensor_tensor(out=osb[:, f, 1, :], in0=osb[:, f, 0, :], scalar=1.0, in1=osb[:, f, 0, :],
                                                                      op0=ALU.mult, op1=ALU.mult, accum_out=fst[:, f, 0:1]),
                             [t_osb[f]], [t_osb[f], t_fst[f]])

                    def st2():
                        s.op("act", lambda a: a.activation(out=fst[:, f, 1:2], in_=fst[:, f, 0:1], func=AF.Ln, bias=EPS, scale=1.0 / 128.0),
                             [t_fst[f]], [t_fst[f]])
                        s.op("act", lambda a: a.activation(out=fst[:, f, 2:3], in_=fst[:, f, 1:2], func=AF.Exp, scale=-0.5),
                             [t_fst[f]], [t_fst[f]])

                    def st3():
                        s.op("dve", lambda v: v.tensor_scalar(out=fan[:, f, :], in0=osb[:, f, 0, :], scalar1=fst[:, f, 2:3], scalar2=None, op0=ALU.mult),
                             [t_osb[f], t_fst[f]], [t_fan[f]])

                    def st4():
                        s.op("pe", lambda pe: pe.transpose(out=psF[:, f * 128:(f + 1) * 128], in_=fan[:, f, :], identity=identb[:]),
                             [t_fan[f], t_cst], [t_psF])

                    def st5():
                        s.op("dve", lambda v: v.scalar_tensor_tensor(out=yT[:, h, tsl], in0=psF[:, f * 128:(f + 1) * 128],
                                                                      scalar=gcol[:, 1:2], in1=szT[:, hb, tsl], op0=ALU.mult, op1=ALU.mult),
                             [t_psF, t_sz[hb], t_gc], [t_yT[h]])
                    return [st1, st2, st3, st4, st5]

                h0 = 0 if "p3" not in skip else 8
                if h0 < 8:
                    for _ in proj_gen(h0):
                        pass
                for h in range(h0, 8):
                    hb = h % 2
                    gen = proj_gen(h + 1) if h + 1 < 8 else iter(())
                    blocks = [(t, c) for t in range(NT) for c in range(t + 1)]
                    pending = []

                    def score(i):
                        t, c = blocks[i]
                        sbk = i % 2
                        diag = (c == t)
                        fns = [lambda pe, m=m: pe.matmul(psS[sbk][:, m, 0:128], lhsT=kT[m * 64:(m + 1) * 64, hb, c * 128:(c + 1) * 128],
                                                         rhs=qT[m * 64:(m + 1) * 64, hb, t * 128:(t + 1) * 128], start=True, stop=not diag)
                               for m in range(2)]
                        if diag:
                            fns += [lambda pe, m=m: pe.matmul(psS[sbk][:, m, 0:128], lhsT=identb[:], rhs=negb[:], start=False, stop=True)
                                    for m in range(2)]
                        s.op("pe", fns, [t_k[hb], t_q[hb], t_cst], [t_psS[sbk]])

                    score(0)
                    for i, (t, c) in enumerate(blocks):
                        sbk = i % 2
                        pb = i % 3
                        ob = t % 2
                        if i + 1 < len(blocks):
                            score(i + 1)
                        bcol = cst[:, C_ALI + h * 16 + (t - c): C_ALI + h * 16 + (t - c) + 1]
                        s.op("act", lambda a: a.activation(out=pt[:, pb, :, :], in_=psS[sbk][:, :, 0:128], func=AF.Exp, bias=bcol, scale=0.125),
                             [t_psS[sbk], t_cst], [t_pt[pb]])
                        while pending and pending[0][0] <= i:
                            pending.pop(0)[1]()
                        if i % 8 == 4:
                            next(gen, None)
                        s.op("pe", [lambda pe, m=m: pe.matmul(psO[ob][:, m, 0:129], lhsT=pt[:, pb, m, :], rhs=vaug[:, hb, c, 0:129],
                                                              start=(c == 0 and m == 0), stop=(c == t and m == 1)) for m in range(2)],
                             [t_pt[pb], t_v[hb]], [t_psO[ob]])
                        if c == t:
                            fnext = fin_i[0] % NF
                            for e_ in [e for e in pending if e[2] == fnext]:
                                pending.remove(e_)
                                e_[1]()
                            for dly, st_ in zip((1, 6, 8, 10, 12), fin_stages(h, t, ob)):
                                pending.append([i + dly, st_, fnext])
                            pending.sort(key=lambda e: e[0])
                    for _ in gen:
                        pass
                    while pending:
                        pending.pop(0)[1]()
                s.barrier()
            if stop == "p3":
                if dbg:
                    for c in range(16):
                        s.dma("pool", dbg_y[:, c, :], yT[:, c, :], reads=[t_yT[c]])
                    for k in range(8):
                        s.dma("pool", dbg_h[:, k, :], hT[:, k, :], reads=[t_hT])
                s.finish("sp")
                print("instructions:", s.n_instr)
                return nc
            with ExitStack() as pes:
                sb = lambda name, shape, dt: pes.enter_context(nc.sbuf_tensor(uq(name), shape, dt))
                ps = lambda name, shape, dt: pes.enter_context(nc.psum_tensor(uq(name), shape, dt))
                ph = {}
                alloc_fin(pes, ph)
                psA = [ps("d_psA%d" % i, [128, 512], F32) for i in range(2)]
                t_psA = [TP(), TP()]
                psB = [ps("d_psB%d" % i, [128, 512], F32) for i in range(2)]
                t_psB = [TP(), TP()]
                psTr = ps("d_psT", [128, 1024], BF16)
                t_psTr = TP()
                psV = ps("d_psV", [128, 4, 128], F32)
                t_psV = TP()
                psW = ps("d_psW", [128, 4, 128], F32)
                t_psW = TP()
                pA = [0]

                wba = sb("d_wba", [128, 8, 8], BF16)
                t_wba = T()
                tok = sb("d_tok", [128, 8, 64], F32)
                t_tok = T()
                prm = sb("d_prm", [128, 16], F32)
                t_prm = T()
                gcolD = sb("d_gcol", [128, 1], F32)
                t_gcD = T()
                cw = sb("d_cw", [128, 3, 4], F32)
                t_cw = T()
                wd = sb("d_w", [128, 5, 8, 128], BF16)
                t_wd = [T() for _ in range(5)]
                raw = sb("d_raw", [128, S + 3], F32)
                t_raw = T()
                cv = sb("d_cv", [128, S], F32)
                t_cv = T()
                qnT = sb("d_qnT", [128, S], BF16)
                knT = sb("d_knT", [128, S], BF16)
                vcT = sb("d_vcT", [128, S], BF16)
                qdT = sb("d_qdT", [128, S], BF16)
                t_qn, t_kn, t_vc, t_qd = T(), T(), T(), T()
                szT = sb("d_szT", [128, S], BF16)
                t_sz = T()
                gcr = sb("d_gcr", [128, S], F32)
                t_gcr = T()
                egl = sb("d_egl", [128, 32], F32)
                t_egl = T()
                sd = sb("d_sd", [128, 2, 512], F32)
                t_sd = [T(), T()]
                tmpD = sb("d_tmpD", [128, 2, 4, 128], F32)
                t_tmpD = T()
                X = sb("d_X", [128, 2, 4, 128], BF16)
                Y = sb("d_Y", [128, 2, 4, 128], BF16)
                R = sb("d_R", [128, 2, 4, 128], BF16)
                t_X, t_Y, t_R = [T(), T()], [T(), T()], [T(), T()]
                aT = sb("d_aT", [128, 2, 4, 128], BF16)
                t_aT = [T(), T()]
                kbg = sb("d_kbg", [128, 4, 128], BF16)
                kdec = sb("d_kdec", [128, 2, 4, 128], BF16)
                vb = sb("d_vb", [128, 4, 128], BF16)
                t_kbg, t_kdec, t_vb = T(), [T(), T()], T()
                usb = sb("d_u", [128, 2, 4, 128], F32)
                t_u = [T(), T()]
                wT = sb("d_wT", [128, 2, 4, 128], BF16)
                t_wT = [T(), T()]
                vnew = sb("d_vnew", [128, 128], BF16)
                t_vnew = T()
                St = sb("d_S", [128, 128], F32)
                Sb = sb("d_Sb", [128, 2, 128], BF16)
                Se = sb("d_Se", [128, 128], F32)
                t_S, t_Sb, t_Se = T(), [T(), T()], T()
                osb = sb("d_o", [128, 2, 128], F32)
                t_osb = [T(), T()]

                def proj_fm(wt, tw, evac):
                    for tc in range(4):
                        b = pA[0] % 2
                        pA[0] += 1
                        s.op("pe", [lambda pe, k=k: pe.matmul(psA[b][:], lhsT=wt[:, k, :], rhs=hT[:, k, tc * 512:(tc + 1) * 512],
                                                              start=(k == 0), stop=(k == 7)) for k in range(8)],
                             [tw, t_hT], [t_psA[b]])
                        evac(psA[b], t_psA[b], tc)

                s.dma("pool", wba[:], w_in[l][:, O_DB:O_DB + 8].rearrange("(k p) n -> p k n", p=128), writes=[t_wba])
                s.dma("sp", prm[:, 0:4], dn_a_log[l:l + 1, :].partition_broadcast(128), writes=[t_prm])
                s.dma("sp", prm[:, 4:8], dn_dt_bias[l:l + 1, :].partition_broadcast(128), writes=[t_prm])
                s.op("act", lambda a: a.activation(out=prm[:, 8:12], in_=prm[:, 0:4], func=AF.Exp), [t_prm], [t_prm])
                s.op("dve", lambda v: v.tensor_scalar(out=prm[:, 8:12], in0=prm[:, 8:12], scalar1=-1.0, scalar2=None, op0=ALU.mult), [t_prm], [t_prm])
                s.dma("sp", gcolD[:, 0:1], dn_norm[l:l + 1, :].rearrange("o c -> c o"), writes=[t_gcD], allow_slow_non_contiguous=True)
                fns = []
                for t in range(NT):
                    for k in range(8):
                        fns.append(lambda pe, k=k, t=t: pe.matmul(psA[0][:, t * 8:(t + 1) * 8], lhsT=hT[:, k, t * 128:(t + 1) * 128],
                                                                  rhs=wba[:, k, :], start=(k == 0), stop=(k == 7)))
                s.op("pe", fns, [t_wba, t_hT], [t_psA[0]])
                pA[0] = 1
                ba = psA[0][:, 0:128].rearrange("p (t c) -> p t c", c=8)
                tk = lambda i: tok[:, i, :].rearrange("p (t c) -> p t c", c=4)
                s.op("act", lambda a: a.activation(out=tk(1), in_=ba[:, :, 0:4], func=AF.Sigmoid), [t_psA[0]], [t_tok])
                for hh in range(4):
                    s.op("act", lambda a, hh=hh: a.activation(out=tk(7)[:, :, hh], in_=ba[:, :, 4 + hh], func=AF.Exp, bias=prm[:, 4 + hh:5 + hh]),
                         [t_psA[0], t_prm], [t_tok])
                s.op("act", lambda a: a.activation(out=tok[:, 7, :], in_=tok[:, 7, :], func=AF.Ln, bias=1.0), [t_tok], [t_tok])
                for hh in range(4):
                    s.op("dve", lambda v, hh=hh: v.tensor_scalar(out=tk(2)[:, :, hh], in0=tk(7)[:, :, hh], scalar1=prm[:, 8 + hh:9 + hh], scalar2=None, op0=ALU.mult),
                         [t_tok, t_prm], [t_tok])
                s.op("pe", lambda pe: pe.matmul(psA[1][:, 0:64], lhsT=cst[:, C_BDI:C_BDI + 128], rhs=tok[:, 2, :], start=True, stop=True),
                     [t_tok, t_cst], [t_psA[1]])
                s.op("pe", lambda pe: pe.matmul(psA[1][:, 64:128], lhsT=cst[:, C_BLK:C_BLK + 128], rhs=tok[:, 2, :], start=True, stop=True),
                     [t_tok, t_cst], [t_psA[1]])
                s.op("dve", lambda v: v.tensor_copy(out=tok[:, 3, :], in_=psA[1][:, 0:64]), [t_psA[1]], [t_tok])
                s.op("dve", lambda v: v.tensor_copy(out=tok[:, 4, :], in_=psA[1][:, 64:128]), [t_psA[1]], [t_tok])
                s.op("act", lambda a: a.activation(out=tok[:, 5, :], in_=tok[:, 3, :], func=AF.Exp), [t_tok], [t_tok])
                s.op("dve", lambda v: v.tensor_tensor(out=tok[:, 5, :], in0=tok[:, 5, :], in1=tok[:, 1, :], op=ALU.mult), [t_tok], [t_tok])
                s.op("dve", lambda v: v.tensor_tensor(out=tok[:, 6, :], in0=tok[:, 4, :], in1=tok[:, 3, :], op=ALU.subtract), [t_tok], [t_tok])
                s.op("act", lambda a: a.activation(out=tok[:, 6, :], in_=tok[:, 6, :], func=AF.Exp), [t_tok], [t_tok])
                s.op("pool", lambda g: g.memset(raw[:, 0:3], 0.0), [], [t_raw])
                s.op("pool", lambda g: g.memset(vnew[:], 0.0), [], [t_vnew])
                col = lambda plane, t, hh: tok[:, plane, t * 4 + hh: t * 4 + hh + 1]
                chk("p4a")

                for h in range(0 if "p4" not in skip else 4, 4):
                    offs = [O_DQ, O_DK, O_DV, O_DZ]
                    for j in range(4):
                        load_w(wd[:, j], w_in[l][:, offs[j] + h * 128: offs[j] + (h + 1) * 128], t_wd[j])
                    load_w(wd[:, 4], w_rep[l][:, h * 128:(h + 1) * 128], t_wd[4])
                    for j in range(3):
                        s.dma("sp", cw[:, j, :], dn_conv[l][:, j * 512 + h * 128: j * 512 + (h + 1) * 128].rearrange("i c -> c i"),
                              writes=[t_cw], allow_slow_non_contiguous=True)
                    proj_fm(wd[:, 3], t_wd[3],
                            lambda p, tp, tc: s.op("act", lambda a: a.activation(out=szT[:, tc * 512:(tc + 1) * 512], in_=p[:], func=AF.Silu), [tp], [t_sz]))
                    proj_fm(wd[:, 4], t_wd[4],
                            lambda p, tp, tc: s.op("act", lambda a: a.activation(out=gcr[:, tc * 512:(tc + 1) * 512], in_=p[:], func=AF.Exp, bias=prm[:, 4 + h:5 + h]),
                                                   [tp, t_prm], [t_gcr]))
                    s.op("act", lambda a: a.activation(out=gcr[:], in_=gcr[:], func=AF.Ln, bias=1.0), [t_gcr], [t_gcr])
                    s.op("dve", lambda v: v.tensor_scalar(out=gcr[:], in0=gcr[:], scalar1=prm[:, 8 + h:9 + h], scalar2=None, op0=ALU.mult), [t_gcr, t_prm], [t_gcr])
                    s.op("dve", lambda v: v.tensor_tensor_scan(out=gcr[:], data0=scanmask[:], data1=gcr[:], initial=0.0, op0=ALU.mult, op1=ALU.add),
                         [t_gcr, t_cst], [t_gcr])
                    s.op("act", lambda a: a.activation(out=egl[:], in_=gcr[:].rearrange("p (n c) -> p n c", c=64)[:, :, 63], func=AF.Exp), [t_gcr], [t_egl])
                    for j in range(3):
                        proj_fm(wd[:, j], t_wd[j],
                                lambda p, tp, tc: s.op("act", lambda a: a.copy(out=raw[:, 3 + tc * 512: 3 + (tc + 1) * 512], in_=p[:]), [tp], [t_raw]))
                        s.op("dve", lambda v: v.tensor_scalar(out=cv[:], in0=raw[:, 3:S + 3], scalar1=cw[:, j, 3:4], scalar2=None, op0=ALU.mult),
                             [t_raw, t_cw], [t_cv])
                        for i in range(3):
                            s.op("dve", lambda v: v.scalar_tensor_tensor(out=cv[:], in0=raw[:, i:S + i], scalar=cw[:, j, i:i + 1], in1=cv[:],
                                                                          op0=ALU.mult, op1=ALU.add),
                                 [t_raw, t_cw, t_cv], [t_cv])
                        s.op("act", lambda a: a.activation(out=cv[:], in_=cv[:], func=AF.Silu), [t_cv], [t_cv])
                        if j == 2:
                            s.op("act", lambda a: a.copy(out=vcT[:], in_=cv[:]), [t_cv], [t_vc])
                            continue
                        s.op("pool", lambda g: g.tensor_tensor(out=raw[:, 3:S + 3], in0=cv[:], in1=cv[:], op=ALU.mult), [t_cv], [t_raw])
                        for tc in range(4):
                            b = pA[0] % 2
                            pA[0] += 1
                            s.op("pe", lambda pe: pe.matmul(psA[b][:], lhsT=ones_f[:], rhs=raw[:, 3 + tc * 512: 3 + (tc + 1) * 512], start=True, stop=True),
                                 [t_raw, t_cst], [t_psA[b]])
                            s.op("act", lambda a: a.activation(out=sd[:, b, :], in_=psA[b][:], func=AF.Ln, bias=EPS, scale=1.0), [t_psA[b]], [t_sd[b]])
                            s.op("act", lambda a: a.activation(out=sd[:, b, :], in_=sd[:, b, :], func=AF.Exp, scale=-0.5), [t_sd[b]], [t_sd[b]])
                            dst, td = (qnT, t_qn) if j == 0 else (knT, t_kn)
                            sc = 128.0 ** -0.5 if j == 0 else 1.0
                            s.op("dve", lambda v: v.scalar_tensor_tensor(out=dst[:, tc * 512:(tc + 1) * 512], in0=cv[:, tc * 512:(tc + 1) * 512], scalar=sc,
                                                                          in1=sd[:, b, :], op0=ALU.mult, op1=ALU.mult),
                                 [t_cv, t_sd[b]], [td])
                    s.op("act", lambda a: a.activation(out=cv[:], in_=gcr[:], func=AF.Exp), [t_gcr], [t_cv])
                    s.op("dve", lambda v: v.tensor_tensor(out=qdT[:], in0=qnT[:], in1=cv[:], op=ALU.mult), [t_qn, t_cv], [t_qd])
                    s.op("dve", lambda v: v.memset(St[:], 0.0), [], [t_S])
                    s.op("dve", lambda v: v.memset(Sb[:], 0.0), [], t_Sb)
                    chk("p4b")

                    sl = lambda t: slice(t * 128, (t + 1) * 128)

                    def stageA(tg):
                        g2 = tg % 2
                        tiles = [tg * 4 + u for u in range(4)]
                        bA = pA[0] % 2
                        pA[0] += 1
                        s.op("pe", [lambda pe, u=u, t=t: pe.matmul(psA[bA][:, u * 128:(u + 1) * 128], lhsT=knT[:, sl(t)], rhs=knT[:, sl(t)], start=True, stop=True)
                                    for u, t in enumerate(tiles)], [t_kn], [t_psA[bA]])
                        yield
                        s.op("pe", [lambda pe, u=u, t=t: pe.matmul(psB[0][:, u * 128:(u + 1) * 128], lhsT=knT[:, sl(t)], rhs=qnT[:, sl(t)], start=True, stop=True)
                                    for u, t in enumerate(tiles)], [t_kn, t_qn], [t_psB[0]])
                        yield
                        for u, t in enumerate(tiles):
                            s.op("dve", lambda v, u=u, t=t: v.tensor_scalar(out=tmpD[:, 1, u, :], in0=gcr[:, sl(t)], scalar1=col(3, t, h), scalar2=None,
                                                                             op0=ALU.subtract), [t_gcr, t_tok], [t_tmpD])
                            yield
                        s.op("dve", lambda v: v.tensor_scalar(out=tmpD[:, 0], in0=tmpD[:, 1], scalar1=0.0, scalar2=None, op0=ALU.max), [t_tmpD], [t_tmpD])
                        s.op("dve", lambda v: v.tensor_scalar(out=tmpD[:, 1], in0=tmpD[:, 1], scalar1=0.0, scalar2=None, op0=ALU.min), [t_tmpD], [t_tmpD])
                        yield
                        s.op("act", lambda a: a.activation(out=tmpD[:, 0], in_=tmpD[:, 0], func=AF.Exp, scale=-1.0), [t_tmpD], [t_tmpD])
                        s.op("act", lambda a: a.activation(out=tmpD[:, 1], in_=tmpD[:, 1], func=AF.Exp), [t_tmpD], [t_tmpD])
                        yield
                        s.op("dve", lambda v: v.tensor_tensor(out=tmpD[:, 0], in0=tmpD[:, 0], in1=bds4[:], op=ALU.mult), [t_tmpD, t_cst], [t_tmpD])
                        s.op("dve", lambda v: v.tensor_tensor(out=tmpD[:, 1], in0=tmpD[:, 1], in1=bdi4[:], op=ALU.mult), [t_tmpD, t_cst], [t_tmpD])
                        yield
                        for u, t in enumerate(tiles):
                            s.op("dve", lambda v, u=u, t=t: v.scalar_tensor_tensor(out=X[:, 0, u, :], in0=psA[bA][:, u * 128:(u + 1) * 128], scalar=col(1, t, h),
                                                                                    in1=tmpD[:, 0, u, :], op0=ALU.mult, op1=ALU.mult),
                                 [t_psA[bA], t_tok, t_tmpD], [t_X[0]])
                            yield
                        s.op("dve", lambda v: v.tensor_tensor(out=aT[:, g2], in0=psB[0][:].rearrange("p (u n) -> p u n", u=4), in1=tmpD[:, 1], op=ALU.mult),
                             [t_psB[0], t_tmpD], [t_aT[g2]])
                        yield
                        s.op("pe", [lambda pe, u=u: pe.transpose(out=psTr[:, u * 128:(u + 1) * 128], in_=X[:, 0, u, :], identity=identb[:]) for u in range(4)],
                             [t_X[0], t_cst], [t_psTr])
                        yield
                        s.op("act", lambda a: a.copy(out=Y[:, 0], in_=psTr[:, 0:512].rearrange("p (u n) -> p u n", u=4)), [t_psTr], [t_Y[0]])
                        s.op("dve", lambda v: v.scalar_tensor_tensor(out=R[:, 0], in0=psTr[:, 0:512].rearrange("p (u n) -> p u n", u=4), scalar=-1.0, in1=ident4[:],
                                                                      op0=ALU.mult, op1=ALU.add),
                             [t_psTr, t_cst], [t_R[0]])
                        yield
                        cur = 0
                        for p in range(1, 6):
                            nxt = 1 - cur
                            if p < 5:
                                s.op("pe", [lambda pe, u=u: pe.matmul(psB[1][:, u * 128:(u + 1) * 128], lhsT=X[:, cur, u, :], rhs=Y[:, cur, u, :], start=True, stop=True)
                                            for u in range(4)], [t_X[cur], t_Y[cur]], [t_psB[1]])
                            bX = pA[0] % 2
                            pA[0] += 1
                            s.op("pe", [lambda pe, u=u: pe.matmul(psA[bX][:, u * 128:(u + 1) * 128], lhsT=Y[:, cur, u, :], rhs=X[:, cur, u, :], start=True, stop=True)
                                        for u in range(4)], [t_X[cur], t_Y[cur]], [t_psA[bX]])
                            yield
                            s.op("dve", lambda v: v.tensor_copy(out=X[:, nxt], in_=psA[bX][:].rearrange("p (u n) -> p u n", u=4)), [t_psA[bX]], [t_X[nxt]])
                            if p < 5:
                                s.op("act", lambda a: a.copy(out=Y[:, nxt], in_=psB[1][:].rearrange("p (u n) -> p u n", u=4)), [t_psB[1]], [t_Y[nxt]])
                            yield
                            s.op("pe", [lambda pe, u=u: pe.matmul(psB[0][:, u * 128:(u + 1) * 128], lhsT=X[:, nxt, u, :], rhs=R[:, cur, u, :], start=True, stop=True)
                                        for u in range(4)], [t_X[nxt], t_R[cur]], [t_psB[0]])
                            yield
                            s.op("dve", lambda v: v.tensor_tensor(out=R[:, nxt], in0=psB[0][:].rearrange("p (u n) -> p u n", u=4), in1=R[:, cur], op=ALU.add),
                                 [t_psB[0], t_R[cur]], [t_R[nxt]])
                            yield
                            cur = nxt
                        TT = R[:, cur]
                        t_TT = t_R[cur]
                        s.op("pe", [lambda pe, u=u, t=t: pe.transpose(out=psTr[:, u * 128:(u + 1) * 128], in_=knT[:, sl(t)], identity=identb[:]) for u, t in enumerate(tiles)],
                             [t_kn, t_cst], [t_psTr])
                        s.op("pe", [lambda pe, u=u, t=t: pe.transpose(out=psTr[:, 512 + u * 128:512 + (u + 1) * 128], in_=vcT[:, sl(t)], identity=identb[:]) for u, t in enumerate(tiles)],
                             [t_vc, t_cst], [t_psTr])
                        yield
                        for u, t in enumerate(tiles):
                            s.op("dve", lambda v, u=u, t=t: v.tensor_scalar(out=kbg[:, u, :], in0=psTr[:, u * 128:(u + 1) * 128], scalar1=col(5, t, h), scalar2=None, op0=ALU.mult),
                                 [t_psTr, t_tok], [t_kbg])
                            s.op("act", lambda a, u=u, t=t: a.mul(out=kdec[:, g2, u, :], in_=psTr[:, u * 128:(u + 1) * 128], mul=col(6, t, h)),
                                 [t_psTr, t_tok], [t_kdec[g2]])
                            s.op("dve", lambda v, u=u, t=t: v.tensor_scalar(out=vb[:, u, :], in0=psTr[:, 512 + u * 128:512 + (u + 1) * 128], scalar1=col(1, t, h), scalar2=None, op0=ALU.mult),
                                 [t_psTr, t_tok], [t_vb])
                            yield
                        bU = pA[0] % 2
                        pA[0] += 1
                        s.op("pe", [lambda pe, u=u: pe.matmul(psA[bU][:, u * 128:(u + 1) * 128], lhsT=TT[:, u, :], rhs=vb[:, u, :], start=True, stop=True) for u in range(4)],
                             [t_TT, t_vb], [t_psA[bU]])
                        yield
                        s.op("act", lambda a: a.copy(out=usb[:, g2], in_=psA[bU][:].rearrange("p (u n) -> p u n", u=4)), [t_psA[bU]], [t_u[g2]])
                        s.op("pe", [lambda pe, u=u: pe.matmul(psB[1][:, u * 128:(u + 1) * 128], lhsT=kbg[:, u, :], rhs=TT[:, u, :], start=True, stop=True) for u in range(4)],
                             [t_TT, t_kbg], [t_psB[1]])
                        yield
                        s.op("dve", lambda v: v.tensor_copy(out=wT[:, g2], in_=psB[1][:].rearrange("p (u n) -> p u n", u=4)), [t_psB[1]], [t_wT[g2]])
                        yield

                    def stageB(tg):
                        g2 = tg % 2
                        tiles = [tg * 4 + u for u in range(4)]
                        for u, t in enumerate(tiles):
                            ob = t % 2
                            for hf in range(2):
                                n = t * 2 + hf
                                sc_, sn_ = n % 2, (n + 1) % 2
                                r = slice(hf * 64, hf * 64 + 64)
                                s.op("act", lambda a: a.mul(out=Se[:], in_=St[:], mul=egl[:, n:n + 1]), [t_S, t_egl], [t_Se])
                                s.op("pe", lambda pe: pe.matmul(psV[:, 0, :], lhsT=wT[:, g2, u, :], rhs=Sb[:, sc_, :], start=True, stop=True), [t_wT[g2], t_Sb[sc_]], [t_psV])
                                yield
                                s.op("dve", lambda v: v.scalar_tensor_tensor(out=vnew[r, :], in0=psV[r, 0, :], scalar=-1.0, in1=usb[r, g2, u, :], op0=ALU.mult, op1=ALU.add),
                                     [t_u[g2], t_psV], [t_vnew])
                                yield
                                s.op("pe", lambda pe: pe.matmul(psV[:, 1, :], lhsT=kdec[r, g2, u, :], rhs=vnew[r, :], start=True, stop=True),
                                     [t_kdec[g2], t_vnew], [t_psV])
                                yield
                                s.op("dve", lambda v: v.tensor_tensor(out=Sb[:, sn_, :], in0=psV[:, 1, :], in1=Se[:], op=ALU.add),
                                     [t_Se, t_psV], [t_Sb[sn_]])
                                s.op("dve", lambda v: v.tensor_tensor(out=St[:], in0=psV[:, 1, :], in1=Se[:], op=ALU.add),
                                     [t_Se, t_psV], [t_S])
                                yield
                                s.op("pe", [lambda pe: pe.matmul(psW[:, hf, :], lhsT=qdT[:, sl(t)], rhs=Sb[:, sc_, :], start=True, stop=False),
                                            lambda pe: pe.matmul(psW[:, hf, :], lhsT=aT[:, g2, u, :], rhs=vnew[:], start=False, stop=True)],
                                     [t_qd, t_Sb[sc_], t_aT[g2], t_vnew], [t_psW])
                                yield
                                s.op("act", lambda a: a.copy(out=osb[r, ob, :], in_=psW[r, hf, :]), [t_psW], [t_osb[ob]])
                                yield
                            finalize_tile(ph, osb[:, ob, :], t_osb[ob], gcolD[:, 0:1], szT[:, sl(t)], t_sz, 8 + h, t, [t_gcD])
                            yield

                    for _ in stageA(0):
                        pass
                    for tg in range(4):
                        gB = stageB(tg)
                        gA = stageA(tg + 1) if tg + 1 < 4 else iter(())
                        doneA = doneB = False
                        while not (doneA and doneB):
                            if not doneB:
                                try:
                                    next(gB)
                                except StopIteration:
                                    doneB = True
                            if not doneA:
                                try:
                                    next(gA)
                                except StopIteration:
                                    doneA = True
                s.barrier()

            if stop == "p4":
                if dbg:
                    for c in range(16):
                        s.dma("pool", dbg_y[:, c, :], yT[:, c, :], reads=[t_yT[c]])
                    for k in range(8):
                        s.dma("pool", dbg_h[:, k, :], hT[:, k, :], reads=[t_hT])
                s.finish("sp")
                print("instructions:", s.n_instr)
                return nc
            with ExitStack() as pes:
                sb = lambda name, shape, dt: pes.enter_context(nc.sbuf_tensor(uq(name), shape, dt))
                ps = lambda name, shape, dt: pes.enter_context(nc.psum_tensor(uq(name), shape, dt))
                ph = {}
                alloc_fin(pes, ph)
                psA = [ps("g_psA%d" % i, [128, 512], F32) for i in range(2)]
                t_psA = [TP(), TP()]
                psB = [ps("g_psB%d" % i, [128, 512], F32) for i in range(2)]
                t_psB = [TP(), TP()]
                psTr = ps("g_psT", [128, 1024], BF16)
                t_psTr = TP()
                psV = ps("g_psV", [128, 4, 128], F32)
                _tv = TP()
                t_psV = [_tv, _tv]
                pA = [0]
                wr = sb("g_wr", [128, 8, 16], BF16)
                t_wr = T()
                grT = sb("g_grT", [16, S], BF16)
                t_gr = T()
                w2f = sb("g_w2f", [16, 256], F32)
                w2 = sb("g_w2", [16, 256], BF16)
                t_w2 = T()
                gbc = sb("g_gb", [128, 2, 2], F32)
                t_gb = T()
                gcolG = sb("g_gcol", [128, 1], F32)
                t_gcG = T()
                wg = sb("g_w", [128, 6, 8, 128], BF16)
                t_wg = [T() for _ in range(6)]
                cl = sb("g_cl", [128, S], F32)
                t_cl = T()
                E = sb("g_E", [128, S], F32)
                Ei = sb("g_Ei", [128, S], F32)
                t_E, t_Ei = T(), T()
                qtT = sb("g_qtT", [128, S], BF16)
                ktT = sb("g_ktT", [128, S], BF16)
                t_qt, t_kt = T(), T()
                vtok = sb("g_v", [128, NT, 256], BF16)
                t_vt = T()
                ktok = sb("g_ktok", [128, NT, 128], BF16)
                t_ktok = T()
                szT = sb("g_szT", [128, 2, S], BF16)
                t_sz = T()
                attT = sb("g_attT", [128, 2, 2, 128], BF16)
                t_att = [T(), T()]
                eD = sb("g_eD", [128, 4, 128], F32)
                t_eD = [T() for _ in range(4)]
                St = sb("g_S", [128, 128], F32)
                Sb = sb("g_Sb", [128, 128], BF16)
                t_S, t_Sb = T(), T()
                osb = sb("g_o", [128, 2, 2, 128], F32)
                t_osb = [[T(), T()], [T(), T()]]

                def proj_fm(wt, tw, evac, m=128):
                    for tc in range(4):
                        b = pA[0] % 2
                        pA[0] += 1
                        s.op("pe", [lambda pe, k=k: pe.matmul(psA[b][0:m, :], lhsT=wt[:, k, 0:m], rhs=hT[:, k, tc * 512:(tc + 1) * 512],
                                                              start=(k == 0), stop=(k == 7)) for k in range(8)],
                             [tw, t_hT], [t_psA[b]])
                        evac(psA[b], t_psA[b], tc)

                s.dma("pool", wr[:], w_in[l][:, O_GR:O_GR + 16].rearrange("(k p) n -> p k n", p=128), writes=[t_wr])
                proj_fm(wr, t_wr, lambda p, tp, tc: s.op("act", lambda a: a.copy(out=grT[:, tc * 512:(tc + 1) * 512], in_=p[0:16, :]), [tp], [t_gr]), m=16)
                s.dma("sp", w2f[:], gla_w2[l], writes=[t_w2])
                s.op("dve", lambda v: v.tensor_copy(out=w2[:], in_=w2f[:]), [t_w2], [t_w2])
                for pr in range(2):
                    s.dma("sp", gbc[:, pr, 0:1], gla_b[l:l + 1, pr * 128:(pr + 1) * 128].rearrange("o c -> c o"), writes=[t_gb], allow_slow_non_contiguous=True)
                s.op("dve", lambda v: v.tensor_scalar(out=gbc[:, :, 1:2], in0=gbc[:, :, 0:1], scalar1=-1.0, scalar2=None, op0=ALU.mult), [t_gb], [t_gb])
                s.dma("sp", gcolG[:, 0:1], gla_norm[l:l + 1, :].rearrange("o c -> c o"), writes=[t_gcG], allow_slow_non_contiguous=True)
                sl = lambda t: slice(t * 128, (t + 1) * 128)

                for pr in range(0 if "p5" not in skip else 2, 2):
                    load_w(wg[:, 0], w_in[l][:, O_GQ + pr * 128: O_GQ + (pr + 1) * 128], t_wg[0])
                    load_w(wg[:, 1], w_in[l][:, O_GK + pr * 128: O_GK + (pr + 1) * 128], t_wg[1])
                    for j in range(2):
                        load_w(wg[:, 2 + j], w_in[l][:, O_GV + pr * 256 + j * 128: O_GV + pr * 256 + (j + 1) * 128], t_wg[2 + j])
                        load_w(wg[:, 4 + j], w_in[l][:, O_GZ + pr * 256 + j * 128: O_GZ + pr * 256 + (j + 1) * 128], t_wg[4 + j])
                    for tc in range(4):
                        b = pA[0] % 2
                        pA[0] += 1
                        s.op("pe", lambda pe: pe.matmul(psA[b][:], lhsT=w2[0:16, pr * 128:(pr + 1) * 128], rhs=grT[0:16, tc * 512:(tc + 1) * 512], start=True, stop=True),
                             [t_w2, t_gr], [t_psA[b]])
                        s.op("act", lambda a: a.activation(out=cl[:, tc * 512:(tc + 1) * 512], in_=psA[b][:], func=AF.Exp, bias=gbc[:, pr, 1:2], scale=-1.0),
                             [t_psA[b], t_gb], [t_cl])
                    s.op("act", lambda a: a.activation(out=cl[:], in_=cl[:], func=AF.Ln, bias=1.0), [t_cl], [t_cl])
                    s.op("dve", lambda v: v.tensor_tensor_scan(out=cl[:], data0=scanmask[:], data1=cl[:], initial=0.0, op0=ALU.mult, op1=ALU.add), [t_cl, t_cst], [t_cl])
                    s.op("act", lambda a: a.activation(out=E[:], in_=cl[:], func=AF.Exp, scale=-1.0 / 16.0), [t_cl], [t_E])
                    s.op("act", lambda a: a.activation(out=Ei[:], in_=cl[:], func=AF.Exp, scale=1.0 / 16.0), [t_cl], [t_Ei])
                    proj_fm(wg[:, 0], t_wg[0],
                            lambda p, tp, tc: s.op("dve", lambda v: v.scalar_tensor_tensor(out=qtT[:, tc * 512:(tc + 1) * 512], in0=p[:], scalar=0.125, in1=E[:, tc * 512:(tc + 1) * 512],
                                                                                            op0=ALU.mult, op1=ALU.mult), [tp, t_E], [t_qt]))
                    proj_fm(wg[:, 1], t_wg[1],
                            lambda p, tp, tc: s.op("dve", lambda v: v.tensor_tensor(out=ktT[:, tc * 512:(tc + 1) * 512], in0=p[:], in1=Ei[:, tc * 512:(tc + 1) * 512], op=ALU.mult),
                                                   [tp, t_Ei], [t_kt]))
                    for j in range(2):
                        proj_fm(wg[:, 4 + j], t_wg[4 + j],
                                lambda p, tp, tc, j=j: s.op("act", lambda a: a.activation(out=szT[:, j, tc * 512:(tc + 1) * 512], in_=p[:], func=AF.Silu), [tp], [t_sz]))
                    for tg in range(8):
                        b = pA[0] % 2
                        pA[0] += 1
                        fns = []
                        for u in range(2):
                            t = tg * 2 + u
                            for j in range(2):
                                for k in range(8):
                                    fns.append(lambda pe, k=k, t=t, u=u, j=j: pe.matmul(psA[b][:, u * 256 + j * 128: u * 256 + (j + 1) * 128], lhsT=hT[:, k, sl(t)],
                                                                                        rhs=wg[:, 2 + j, k, :], start=(k == 0), stop=(k == 7)))
                        s.op("pe", fns, [t_wg[2], t_wg[3], t_hT], [t_psA[b]])
                        s.op("dve", lambda v: v.tensor_copy(out=vtok[:, tg * 2:(tg + 1) * 2, :], in_=psA[b][:].rearrange("p (u n) -> p u n", u=2)), [t_psA[b]], [t_vt])
                    for tg in range(2):
                        s.op("pe", [lambda pe, u=u: pe.transpose(out=psTr[:, u * 128:(u + 1) * 128], in_=ktT[:, sl(tg * 8 + u)], identity=identb[:]) for u in range(8)],
                             [t_kt, t_cst], [t_psTr])
                        s.op("act", lambda a: a.copy(out=ktok[:, tg * 8:(tg + 1) * 8, :], in_=psTr[:].rearrange("p (u n) -> p u n", u=8)), [t_psTr], [t_ktok])
                    s.op("dve", lambda v: v.memset(St[:], 0.0), [], [t_S])
                    s.op("dve", lambda v: v.memset(Sb[:], 0.0), [], [t_Sb])
                    for t in range(NT):
                        ab = t % 2
                        bB = t % 2
                        s.op("pe", [lambda pe, hh=hh: pe.matmul(psB[hh][:, 0:128], lhsT=ktT[hh * 64:(hh + 1) * 64, sl(t)],
                                                                rhs=qtT[hh * 64:(hh + 1) * 64, sl(t)], start=True, stop=True) for hh in range(2)],
                             [t_kt, t_qt], [t_psB[0], t_psB[1]])
                        for hh in range(2):
                            s.op("dve", lambda v, hh=hh: v.tensor_tensor(out=attT[:, ab, hh, :], in0=psB[hh][:, 0:128], in1=bdi4[:, 0, :], op=ALU.mult),
                                 [t_psB[hh], t_cst], [t_att[ab]])
                        for hf in range(2):
                            n = t * 2 + hf
                            r = slice(hf * 64, hf * 64 + 64)
                            db = n % 4
                            dA = n % 2
                            s.op("pe", lambda pe: pe.matmul(psA[dA][:, 0:256], lhsT=ktok[r, t, :], rhs=vtok[r, t, :], start=True, stop=True),
                                 [t_ktok, t_vt], [t_psA[dA]])
                            ecol = E[:, n * 64 + 63: n * 64 + 64]
                            s.op("act", lambda a: a.mul(out=eD[0:64, db, :], in_=psA[dA][0:64, 0:128], mul=ecol[0:64, :]), [t_psA[dA], t_E], [t_eD[db]])
                            s.op("act", lambda a: a.mul(out=eD[64:128, db, :], in_=psA[dA][64:128, 128:256], mul=ecol[64:128, :]), [t_psA[dA], t_E], [t_eD[db]])
                            for hh in range(2):
                                hr = slice(hh * 64, hh * 64 + 64)
                                s.op("pe", [lambda pe: pe.matmul(psV[:, hf * 2 + hh, :], lhsT=qtT[hr, sl(t)], rhs=Sb[hr, :], start=True, stop=False),
                                            lambda pe: pe.matmul(psV[:, hf * 2 + hh, :], lhsT=attT[:, ab, hh, :], rhs=vtok[:, t, hh * 128:(hh + 1) * 128], start=False, stop=True)],
                                     [t_qt, t_Sb, t_att[ab], t_vt], [t_psV[hf]])
                                s.op("act", lambda a: a.copy(out=osb[r, ab, hh, :], in_=psV[r, hf * 2 + hh, :]), [t_psV[hf]], [t_osb[ab][hh]])
                            s.op("dve", lambda v: v.scalar_tensor_tensor(out=St[:], in0=St[:], scalar=ecol, in1=eD[:, db, :], op0=ALU.mult, op1=ALU.add),
                                 [t_S, t_E, t_eD[db]], [t_S])
                            s.op("dve", lambda v: v.tensor_copy(out=Sb[:], in_=St[:]), [t_S], [t_Sb])
                        for hh in range(2):
                            finalize_tile(ph, osb[:, ab, hh, :], t_osb[ab][hh], gcolG[:, 0:1], szT[:, hh, sl(t)], t_sz, 12 + pr * 2 + hh, t, [t_gcG])
                s.barrier()

            if stop == "p5":
                if dbg:
                    for c in range(16):
                        s.dma("pool", dbg_y[:, c, :], yT[:, c, :], reads=[t_yT[c]])
                    for k in range(8):
                        s.dma("pool", dbg_h[:, k, :], hT[:, k, :], reads=[t_hT])
                s.finish("sp")
                print("instructions:", s.n_instr)
                return nc
            if dbg and l == 0:
                for c in range(16):
                    s.dma("pool", dbg_y[:, c, :], yT[:, c, :], reads=[t_yT[c]])

            with ExitStack() as pes:
                sb = lambda name, shape, dt: pes.enter_context(nc.sbuf_tensor(uq(name), shape, dt))
                ps = lambda name, shape, dt: pes.enter_context(nc.psum_tensor(uq(name), shape, dt))
                wo = hT[:].rearrange("p k s -> p (k s)").rearrange("p (c n) -> p c n", c=16)
                wgt = sb("o_wg", [128, 8, D], BF16)
                wpp = sb("o_wp", [128, 2, D], BF16)
                t_wo, t_wgt, t_wpp = t_hT, T(), T()
                pTb = sb("o_pT", [128, 2, S], BF16)
                t_pT = T()
                gb2 = sb("o_gb2", [128, D], F32)
                t_gb2 = T()
                xb_ = sb("o_x", [128, 2, D], F32)
                t_x = [T(), T()]
                x1 = sb("o_x1", [128, 2, D], F32)
                t_x1 = [T(), T()]
                x1b = sb("o_x1b", [128, 2, D], BF16)
                t_x1b = [T(), T()]
                x1T = sb("o_x1T", [128, 2, D], BF16)
                t_x1T = [T(), T()]
                gate = sb("o_gate", [128, D], F32)
                t_gate = T()
                mm_ = sb("o_m", [128, D], F32)
                t_m = T()
                tmp = sb("o_tmp", [128, D], F32)
                t_tmp = T()
                junk = sb("o_junk", [128, D], BF16)
                t_j = T()
                st = sb("o_st", [128, 2, 8], F32)
                t_st = [T(), T()]
                psY = ps("o_psY", [128, 1024], F32)
                psG = ps("o_psG", [128, 1024], F32)
                psP = ps("o_psP", [128, 1024], F32)
                psX = ps("o_psX", [128, 1024], BF16)
                t_psY, t_psG, t_psP, t_psX = TP(), TP(), TP(), TP()
                load_w(wo, w_out[l], t_wo)
                load_w(wgt[:], ple_gate[l], t_wgt)
                load_w(wpp[:], ple_proj[l], t_wpp)
                load_w(pTb[:], pT_in[l], t_pT)
                s.dma("sp", gb1[:], post_gain[l:l + 1, :].partition_broadcast(128), writes=[t_gb1])
                s.dma("sp", gb2[:], ple_norm[l:l + 1, :].partition_broadcast(128), writes=[t_gb2])
                for t in range(NT):
                    b = t % 2
                    tsl = slice(t * 128, (t + 1) * 128)
                    s.dma("sp", xb_[:, b, :], x_src[tsl, :], writes=[t_x[b]])
                    fns = []
                    for nh in range(2):
                        for c in range(16):
                            fns.append(lambda pe, c=c, nh=nh: pe.matmul(psY[:, nh * 512:(nh + 1) * 512], lhsT=yT[:, c, tsl], rhs=wo[:, c, nh * 512:(nh + 1) * 512],
                                                                        start=(c == 0), stop=(c == 15)))
                    s.op("pe", fns, t_yT + [t_wo], [t_psY])
                    s.op("act", lambda a: a.activation(out=junk[:], in_=psY[:], func=AF.Square, accum_out=st[:, b, 0:1]), [t_psY], [t_j, t_st[b]])
                    s.op("act", lambda a: a.activation(out=st[:, b, 1:2], in_=st[:, b, 0:1], func=AF.Ln, bias=EPS, scale=1.0 / D), [t_st[b]], [t_st[b]])
                    s.op("act", lambda a: a.activation(out=st[:, b, 2:3], in_=st[:, b, 1:2], func=AF.Exp, scale=-0.5), [t_st[b]], [t_st[b]])
                    s.op("dve", lambda v: v.scalar_tensor_tensor(out=tmp[:], in0=psY[:], scalar=st[:, b, 2:3], in1=gb1[:], op0=ALU.mult, op1=ALU.mult),
                         [t_psY, t_st[b], t_gb1], [t_tmp])
                    s.op("pool", lambda g: g.tensor_tensor(out=x1[:, b, :], in0=tmp[:], in1=xb_[:, b, :], op=ALU.add), [t_tmp, t_x[b]], [t_x1[b]])
                    s.op("act", lambda a: a.copy(out=x1b[:, b, :], in_=x1[:, b, :]), [t_x1[b]], [t_x1b[b]])
                    s.op("pe", [lambda pe, k=k: pe.transpose(out=psX[:, k * 128:(k + 1) * 128], in_=x1b[:, b, k * 128:(k + 1) * 128], identity=identb[:]) for k in range(8)],
                         [t_x1b[b], t_cst], [t_psX])
                    s.op("dve", lambda v: v.tensor_copy(out=x1T[:, b, :], in_=psX[:]), [t_psX], [t_x1T[b]])
                    fns = []
                    for nh in range(2):
                        for k in range(8):
                            fns.append(lambda pe, k=k, nh=nh: pe.matmul(psG[:, nh * 512:(nh + 1) * 512], lhsT=x1T[:, b, k * 128:(k + 1) * 128], rhs=wgt[:, k, nh * 512:(nh + 1) * 512],
                                                                        start=(k == 0), stop=(k == 7)))
                    s.op("pe", fns, [t_x1T[b], t_wgt], [t_psG])
                    s.op("act", lambda a: a.activation(out=gate[:], in_=psG[:], func=AF.Sigmoid), [t_psG], [t_gate])
                    fns = []
                    for nh in range(2):
                        for k in range(2):
                            fns.append(lambda pe, k=k, nh=nh: pe.matmul(psP[:, nh * 512:(nh + 1) * 512], lhsT=pTb[:, k, tsl], rhs=wpp[:, k, nh * 512:(nh + 1) * 512],
                                                                        start=(k == 0), stop=(k == 1)))
                    s.op("pe", fns, [t_pT, t_wpp], [t_psP])
                    s.op("dve", lambda v: v.tensor_tensor(out=mm_[:], in0=psP[:], in1=gate[:], op=ALU.mult), [t_psP, t_gate], [t_m])
                    s.op("act", lambda a: a.activation(out=junk[:], in_=mm_[:], func=AF.Square, accum_out=st[:, b, 4:5]), [t_m], [t_j, t_st[b]])
                    s.op("act", lambda a: a.activation(out=st[:, b, 5:6], in_=st[:, b, 4:5], func=AF.Ln, bias=EPS, scale=1.0 / D), [t_st[b]], [t_st[b]])
                    s.op("act", lambda a: a.activation(out=st[:, b, 6:7], in_=st[:, b, 5:6], func=AF.Exp, scale=-0.5), [t_st[b]], [t_st[b]])
                    s.op("dve", lambda v: v.scalar_tensor_tensor(out=mm_[:], in0=mm_[:], scalar=st[:, b, 6:7], in1=gb2[:], op0=ALU.mult, op1=ALU.mult),
                         [t_m, t_st[b], t_gb2], [t_m])
                    s.op("pool", lambda g: g.tensor_tensor(out=x1[:, b, :], in0=x1[:, b, :], in1=mm_[:], op=ALU.add), [t_m, t_x1[b]], [t_x1[b]])
                    s.dma("sp", out[tsl, :], x1[:, b, :], reads=[t_x1[b]])
                s.barrier()
            x_src = out
        s.finish("sp")
        print("instructions:", s.n_instr, {k: v for k, v in s.cnt.items() if v})
    return nc


_CACHE = {}


def prep_inputs(inputs):
    f = lambda a: np.ascontiguousarray(np.asarray(a, dtype=np.float32))
    x = f(inputs["x"])
    p = f(inputs["p"])
    w_in = f(inputs["w_in"])
    w_rep = np.ascontiguousarray(np.repeat(w_in[:, :, O_DA:O_DA + 4], 128, axis=2))
    att_l = np.ascontiguousarray(np.stack([f(inputs["att_lq1"]), f(inputs["att_lk1"]), f(inputs["att_lq2"]), f(inputs["att_lk2"])], axis=1))
    shared = {
        "w_in": w_in, "w_rep": w_rep, "w_out": f(inputs["w_out"]), "ple_gate": f(inputs["ple_gate"]),
        "ple_proj": f(inputs["ple_proj"]), "pre_gain": f(inputs["pre_gain"]), "post_gain": f(inputs["post_gain"]),
        "ple_norm": f(inputs["ple_norm"]), "att_l": att_l, "att_subln": f(inputs["att_subln"]),
        "dn_conv": f(inputs["dn_conv"]), "dn_a_log": f(inputs["dn_a_log"]), "dn_dt_bias": f(inputs["dn_dt_bias"]),
        "dn_norm": f(inputs["dn_norm"]), "gla_w2": f(inputs["gla_w2"]), "gla_b": f(inputs["gla_b"]),
        "gla_norm": f(inputs["gla_norm"]), "consts": make_consts(),
    }
    maps = []
    for b in range(x.shape[0]):
        m = dict(shared)
        m["x"] = np.ascontiguousarray(x[b])
        m["pT"] = np.ascontiguousarray(p[:, b].transpose(0, 2, 1))
        maps.append(m)
    return maps


def kernel(**inputs):
    maps = prep_inputs(inputs)
    if "nc" not in _CACHE:
        _CACHE["nc"] = build_program()
    res = run_bass_kernel_spmd(_CACHE["nc"], maps, core_ids=list(range(8)))
    return np.stack([np.asarray(r["out"], dtype=np.float32) for r in res.results], axis=0)
```

```python
import math
from contextlib import ExitStack
import numpy as np
import concourse.bass as bass
import concourse.mybir as mybir
from concourse.bass_utils import run_bass_kernel_spmd

F32 = mybir.dt.float32
BF16 = mybir.dt.bfloat16
AF = mybir.ActivationFunctionType
ALU = mybir.AluOpType

S = 2048
D = 1024
NT = 16
DEPTH = 2
D_IN = 7704
EPS = 1e-6
O_AQ, O_AK, O_AV, O_AZ = 0, 1024, 2048, 3072
O_DQ, O_DK, O_DV, O_DZ, O_DB, O_DA = 4096, 4608, 5120, 5632, 6144, 6148
O_GQ, O_GK, O_GV, O_GZ, O_GR = 6152, 6408, 6664, 7176, 7688
C_ID, C_MATT, C_BDI, C_BDS, C_BLK, C_ALI, C_NEG, NC_CONST = 0, 128, 256, 384, 512, 640, 768, 896
ATT_W = [128] * 8


class T:
    __slots__ = ("w", "r", "x")

    def __init__(self, x=False):
        self.w = None
        self.r = {}
        self.x = x


def TP():
    return T(True)


class Sched:
    def __init__(self, nc, es, n_dma_sems=8):
        self.nc = nc
        self.eng = {"pe": nc.tensor, "act": nc.scalar, "dve": nc.vector,
                    "pool": nc.gpsimd, "sp": nc.sync}
        self.sem = {}
        self.cnt = {}
        for k in self.eng:
            self.sem[k] = es.enter_context(nc.semaphore("s_" + k))
            self.cnt[k] = 0
        self.seen = {k: {} for k in self.eng}
        self.dq = {}
        for q in ("sp", "pool"):
            sems = []
            for i in range(n_dma_sems):
                key = "d_%s%d" % (q, i)
                self.sem[key] = es.enter_context(nc.semaphore(key))
                self.cnt[key] = 0
                sems.append(key)
            self.dq[q] = [sems, 0]
        self.n_instr = 0

    def _wait(self, e, ev):
        key, val = ev
        if key == "pe" and e == "pe":
            return
        if self.seen[e].get(key, 0) >= val:
            return
        self.eng[e].wait_ge(self.sem[key], val)
        self.seen[e][key] = val

    def _deps(self, reads, writes):
        evs = {}
        for t in reads:
            if t.w is not None and evs.get(t.w[0], 0) < t.w[1]:
                evs[t.w[0]] = t.w[1]
        for t in writes:
            if t.w is not None and evs.get(t.w[0], 0) < t.w[1]:
                evs[t.w[0]] = t.w[1]
            for k, v in t.r.items():
                if evs.get(k, 0) < v:
                    evs[k] = v
        return evs

    def _commit(self, ev, reads, writes):
        k, v = ev
        for t in reads:
            if t.r.get(k, 0) < v:
                t.r[k] = v
        for t in writes:
            t.w = ev
            t.r = {}

    def op(self, e, fns, reads=(), writes=()):
        if callable(fns):
            fns = [fns]
        writes = list(writes) + [t for t in reads if t.x]
        reads = [t for t in reads if not t.x]
        for k, v in self._deps(reads, writes).items():
            self._wait(e, (k, v))
        h = self.eng[e]
        ins = None
        for f in fns:
            ins = f(h)
            self.n_instr += 1
        self.cnt[e] += 1
        ins.then_inc(self.sem[e], 1)
        ev = (e, self.cnt[e])
        self._commit(ev, reads, writes)
        return ev

    def dma(self, q, out, in_, reads=(), writes=(), **kw):
        sems, idx = self.dq[q]
        key = sems[idx]
        self.dq[q][1] = (idx + 1) % len(sems)
        if self.cnt[key] > 0:
            self._wait(q, (key, self.cnt[key]))
        for k, v in self._deps(reads, writes).items():
            self._wait(q, (k, v))
        ins = self.eng[q].dma_start(out=out, in_=in_, **kw)
        self.n_instr += 1
        self.cnt[key] += 16
        ins.then_inc(self.sem[key], 16)
        ev = (key, self.cnt[key])
        self._commit(ev, reads, writes)
        return ev

    def barrier(self):
        for e in self.eng:
            for k, v in self.cnt.items():
                if v > 0 and k != e:
                    self._wait(e, (k, v))

    def finish(self, e="sp"):
        for k, v in self.cnt.items():
            if v > 0 and k != e:
                self._wait(e, (k, v))


def make_consts():
    c = np.zeros((128, NC_CONST), np.float32)
    i = np.arange(128)
    c[:, C_ID:C_ID + 128] = np.eye(128, dtype=np.float32)
    c[:, C_MATT:C_MATT + 128] = (i[:, None] <= i[None, :]).astype(np.float32)
    same = (i[:, None] // 64) == (i[None, :] // 64)
    c[:, C_BDI:C_BDI + 128] = ((i[:, None] <= i[None, :]) & same).astype(np.float32)
    c[:, C_BDS:C_BDS + 128] = ((i[None, :] < i[:, None]) & same).astype(np.float32)
    c[:, C_BLK:C_BLK + 128] = same.astype(np.float32)
    c[:, C_NEG:C_NEG + 128] = np.where(i[:, None] > i[None, :], -30000.0, 0.0).astype(np.float32)
    slopes = 2.0 ** (-8.0 * np.arange(1, 9) / 8.0)
    for h in range(8):
        for dd in range(16):
            c[:, C_ALI + h * 16 + dd] = slopes[h] * (i - 127 - 128 * dd)
    return c


def build_program(depth=DEPTH, dbg=False, stop=None, skip=()):
    try:
        return _build_program(depth, dbg, stop, skip)
    except StopBuild as e:
        return e.nc


class StopBuild(Exception):
    def __init__(self, nc):
        self.nc = nc


def _build_program(depth=DEPTH, dbg=False, stop=None, skip=()):
    nc = bass.Bass("TRN2", target_bir_lowering=False)
    dr = lambda name, shape, kind="ExternalInput", dt=F32: nc.dram_tensor(name, shape, dt, kind=kind).ap()
    x_in = dr("x", [S, D])
    pT_in = dr("pT", [DEPTH, 256, S])
    w_in = dr("w_in", [DEPTH, D, D_IN])
    w_rep = dr("w_rep", [DEPTH, D, 512])
    w_out = dr("w_out", [DEPTH, 2048, D])
    ple_gate = dr("ple_gate", [DEPTH, D, D])
    ple_proj = dr("ple_proj", [DEPTH, 256, D])
    pre_gain = dr("pre_gain", [DEPTH, D])
    post_gain = dr("post_gain", [DEPTH, D])
    ple_norm = dr("ple_norm", [DEPTH, D])
    att_l = dr("att_l", [DEPTH, 4, 64])
    att_subln = dr("att_subln", [DEPTH, 128])
    dn_conv = dr("dn_conv", [DEPTH, 4, 1536])
    dn_a_log = dr("dn_a_log", [DEPTH, 4])
    dn_dt_bias = dr("dn_dt_bias", [DEPTH, 4])
    dn_norm = dr("dn_norm", [DEPTH, 128])
    gla_w2 = dr("gla_w2", [DEPTH, 16, 256])
    gla_b = dr("gla_b", [DEPTH, 256])
    gla_norm = dr("gla_norm", [DEPTH, 128])
    consts_in = dr("consts", [128, NC_CONST])
    out = dr("out", [S, D], kind="ExternalOutput")
    dbg_y = dr("dbg_y", [128, 16, S], kind="ExternalOutput") if dbg else None
    dbg_h = dr("dbg_h", [128, 8, S], kind="ExternalOutput") if dbg else None

    _uq = [0]


    def uq(name):
        _uq[0] += 1
        return "%s_%d" % (name, _uq[0])

    with ExitStack() as es:
        s = Sched(nc, es)
        sbp = lambda name, shape, dt: es.enter_context(nc.sbuf_tensor(uq(name), shape, dt))
        hT = sbp("hT", [128, 8, S], BF16)
        t_hT = T()
        yT = sbp("yT", [128, 16, S], BF16)
        t_yT = [T() for _ in range(16)]
        cst = sbp("cst", [128, NC_CONST], F32)
        t_cst = T()
        identb = sbp("identb", [128, 128], BF16)
        ident4 = sbp("ident4", [128, 4, 128], BF16)
        matt2 = sbp("matt2", [128, 2, 128], BF16)
        negb = sbp("negb", [128, 128], BF16)
        bdi4 = sbp("bdi4", [128, 4, 128], BF16)
        bds4 = sbp("bds4", [128, 4, 128], F32)
        ones_f = sbp("ones_f", [128, 128], F32)
        scanmask = sbp("scanmask", [128, S], F32)
        gb0 = sbp("gb0", [128, D], F32)
        t_gb0 = T()
        gb1 = sbp("gb1", [128, D], F32)
        t_gb1 = T()
        small = sbp("small", [128, 64], F32)
        t_small = T()

        ident = cst[:, C_ID:C_ID + 128]
        s.dma("sp", cst[:], consts_in, writes=[t_cst])
        s.op("dve", lambda v: v.tensor_copy(out=identb[:], in_=ident), [t_cst], [t_cst])
        for u in range(4):
            s.op("dve", lambda v, u=u: v.tensor_copy(out=ident4[:, u, :], in_=ident), [t_cst], [t_cst])
            s.op("dve", lambda v, u=u: v.tensor_copy(out=bdi4[:, u, :], in_=cst[:, C_BDI:C_BDI + 128]), [t_cst], [t_cst])
            s.op("dve", lambda v, u=u: v.tensor_copy(out=bds4[:, u, :], in_=cst[:, C_BDS:C_BDS + 128]), [t_cst], [t_cst])
        for u in range(2):
            s.op("dve", lambda v, u=u: v.tensor_copy(out=matt2[:, u, :], in_=cst[:, C_MATT:C_MATT + 128]), [t_cst], [t_cst])
        s.op("dve", lambda v: v.tensor_copy(out=negb[:], in_=cst[:, C_NEG:C_NEG + 128]), [t_cst], [t_cst])
        s.op("pool", lambda g: g.memset(ones_f[:], 1.0), [], [t_cst])
        s.op("pool", lambda g: g.memset(scanmask[:], 1.0), [], [t_cst])
        s.op("pool", lambda g: g.memset(scanmask[:].rearrange("p (c k) -> p c k", k=64)[:, :, 0:1], 0.0), [], [t_cst])

        def chk(name):
            if stop == name:
                s.finish("sp")
                print("STOP at", name, "instructions:", s.n_instr)
                raise StopBuild(nc)

        def load_w(dst, src2d, tw):
            src = src2d.rearrange("(k p) n -> p k n", p=128)
            nk = src.shape[1]
            per = max(1, 2048 // max(1, src.shape[2] * 4 // 512))
            per = min(per, nk)
            if src.shape[2] >= 1024:
                per = 1
            for k0 in range(0, nk, per):
                s.dma("pool", dst[:, k0:k0 + per], src[:, k0:k0 + per], writes=[tw])

        def finalize_tile(ph, o_ap, t_o, gaincol, szT_ap, t_sz, mix, t, extra_reads=()):
            junk, t_junk, st, t_st, an, t_an, psT, t_psT = ph["fin"]
            i = ph["fin_i"] = ph.get("fin_i", 0) + 1
            b = i % 2
            s.op("dve", lambda v: v.scalar_tensor_tensor(out=junk[:, b, :], in0=o_ap, scalar=1.0, in1=o_ap, op0=ALU.mult, op1=ALU.mult,
                                                          accum_out=st[:, b, 0:1]),
                 [t_o], [t_junk[b], t_st[b]])
            s.op("act", lambda a: a.activation(out=st[:, b, 1:2], in_=st[:, b, 0:1], func=AF.Ln, bias=EPS, scale=1.0 / 128.0),
                 [t_st[b]], [t_st[b]])
            s.op("act", lambda a: a.activation(out=st[:, b, 2:3], in_=st[:, b, 1:2], func=AF.Exp, scale=-0.5), [t_st[b]], [t_st[b]])
            s.op("dve", lambda v: v.tensor_scalar(out=an[:, b, :], in0=o_ap, scalar1=st[:, b, 2:3], scalar2=None, op0=ALU.mult),
                 [t_o, t_st[b]], [t_an[b]])
            s.op("pe", lambda pe: pe.transpose(out=psT[:, b * 128:(b + 1) * 128], in_=an[:, b, :], identity=identb[:]),
                 [t_an[b], t_cst], [t_psT[b]])
            s.op("dve", lambda v: v.scalar_tensor_tensor(out=yT[:, mix, t * 128:(t + 1) * 128], in0=psT[:, b * 128:(b + 1) * 128],
                                                          scalar=gaincol, in1=szT_ap, op0=ALU.mult, op1=ALU.mult),
                 [t_psT[b], t_sz] + list(extra_reads), [t_yT[mix]])

        def alloc_fin(pes, ph):
            sb = lambda name, shape, dt: pes.enter_context(nc.sbuf_tensor(uq(name), shape, dt))
            junk = sb("fjunk", [128, 2, 128], BF16)
            st = sb("fst", [128, 2, 4], F32)
            an = sb("fan", [128, 2, 128], BF16)
            psT = pes.enter_context(nc.psum_tensor(uq("fpsT"), [128, 1024], BF16))
            _tp = TP()
            ph["fin"] = (junk, [T(), T()], st, [T(), T()], an, [T(), T()], psT, [_tp, _tp])

        x_src = x_in
        for l in range(depth):
            lam_init = 0.8 - 0.6 * math.exp(-0.3 * l)
            with ExitStack() as pes:
                sb = lambda name, shape, dt: pes.enter_context(nc.sbuf_tensor(uq(name), shape, dt))
                xb_ = sb("p1x", [128, 2, D], F32)
                t_x = [T(), T()]
                hb = sb("p1h", [128, 2, D], BF16)
                t_hb = [T(), T()]
                junk = sb("p1j", [128, D], BF16)
                t_j = T()
                st = sb("p1s", [128, 2, 4], F32)
                t_st = [T(), T()]
                psT = pes.enter_context(nc.psum_tensor(uq("p1ps"), [128, 2, 1024], BF16))
                t_ps = [TP(), TP()]
                s.dma("sp", gb0[:], pre_gain[l:l + 1, :].partition_broadcast(128), writes=[t_gb0])
                for t in range(NT):
                    b = t % 2
                    s.dma("sp", xb_[:, b, :], x_src[t * 128:(t + 1) * 128, :], writes=[t_x[b]])
                    s.op("act", lambda a: a.activation(out=junk[:], in_=xb_[:, b, :], func=AF.Square, accum_out=st[:, b, 0:1]),
                         [t_x[b]], [t_j, t_st[b]])
                    s.op("act", lambda a: a.activation(out=st[:, b, 1:2], in_=st[:, b, 0:1], func=AF.Ln, bias=EPS, scale=1.0 / D),
                         [t_st[b]], [t_st[b]])
                    s.op("act", lambda a: a.activation(out=st[:, b, 2:3], in_=st[:, b, 1:2], func=AF.Exp, scale=-0.5), [t_st[b]], [t_st[b]])
                    s.op("dve", lambda v: v.scalar_tensor_tensor(out=hb[:, b, :], in0=xb_[:, b, :], scalar=st[:, b, 2:3], in1=gb0[:],
                                                                  op0=ALU.mult, op1=ALU.mult),
                         [t_x[b], t_st[b], t_gb0], [t_hb[b]])
                    s.op("pe", [lambda pe, k=k: pe.transpose(out=psT[:, b, k * 128:(k + 1) * 128], in_=hb[:, b, k * 128:(k + 1) * 128],
                                                             identity=identb[:]) for k in range(8)],
                         [t_hb[b], t_cst], [t_ps[b]])
                    s.op("act", lambda a: a.copy(out=hT[:, :, t * 128:(t + 1) * 128],
                                                 in_=psT[:, b, :].rearrange("p (k n) -> p k n", k=8)),
                         [t_ps[b]], [t_hT])
                s.barrier()

            if stop == "p1":
                if dbg:
                    for c in range(16):
                        s.dma("pool", dbg_y[:, c, :], yT[:, c, :], reads=[t_yT[c]])
                    for k in range(8):
                        s.dma("pool", dbg_h[:, k, :], hT[:, k, :], reads=[t_hT])
                s.finish("sp")
                print("instructions:", s.n_instr)
                return nc
            with ExitStack() as pes:
                sb = lambda name, shape, dt: pes.enter_context(nc.sbuf_tensor(uq(name), shape, dt))
                ps = lambda name, shape, dt: pes.enter_context(nc.psum_tensor(uq(name), shape, dt))
                wbuf = sb("a_w", [128, 2, 4, 8, 128], BF16)
                t_w = [[T() for _ in range(4)] for _ in range(2)]
                qT = sb("a_qT", [128, 2, S], BF16)
                t_q = [T(), T()]
                kT = sb("a_kT", [128, 2, S], BF16)
                t_k = [T(), T()]
                szT = sb("a_szT", [128, 2, S], BF16)
                t_sz = [T(), T()]
                vaug = sb("a_v", [128, 2, NT, 130], BF16)
                t_v = [T(), T()]
                pt = sb("a_p", [128, 3, 2, 128], BF16)
                t_pt = [T(), T(), T()]
                NF = 8
                osb = sb("a_o", [128, NF, 2, 128], F32)
                t_osb = [T() for _ in range(NF)]
                rr = sb("a_rr", [128, NF, 4], F32)
                t_rr = [T() for _ in range(NF)]
                fjunk = sb("a_fj", [128, NF, 128], BF16)
                t_fj = [T() for _ in range(NF)]
                fst = sb("a_fst", [128, NF, 4], F32)
                t_fst = [T() for _ in range(NF)]
                fan = sb("a_fan", [128, NF, 128], BF16)
                t_fan = [T() for _ in range(NF)]
                lq = sb("a_lq", [128, 4, 64], F32)
                t_lq = T()
                ztmp = sb("a_zt", [128, 512], F32)
                t_zt = T()
                gcol = sb("a_gc", [128, 2], F32)
                t_gc = T()
                psA = ps("a_psA", [128, 512], F32)
                t_psA = TP()
                psS = [ps("a_psS%d" % i, [128, 2, 512], F32) for i in range(2)]
                t_psS = [TP(), TP()]
                psO = [ps("a_psO%d" % i, [128, 2, 256], F32) for i in range(2)]
                t_psO = [TP(), TP()]
                psF = ps("a_psF", [128, 1024], BF16)
                t_psF = TP()

                s.dma("sp", lq[:], att_l[l:l + 1].partition_broadcast(128), writes=[t_lq])
                s.op("dve", lambda v: v.tensor_tensor(out=lq[:, 0, :], in0=lq[:, 0, :], in1=lq[:, 1, :], op=ALU.mult), [t_lq], [t_lq])
                s.op("dve", lambda v: v.tensor_tensor(out=lq[:, 2, :], in0=lq[:, 2, :], in1=lq[:, 3, :], op=ALU.mult), [t_lq], [t_lq])
                s.op("act", lambda a: a.activation(out=lq[:, 1, :], in_=lq[:, 0, :], func=AF.Copy, accum_out=small[:, 1:2]), [t_lq], [t_lq, t_small])
                s.op("act", lambda a: a.activation(out=lq[:, 3, :], in_=lq[:, 2, :], func=AF.Copy, accum_out=small[:, 2:3]), [t_lq], [t_lq, t_small])
                s.op("act", lambda a: a.activation(out=small[:, 3:5], in_=small[:, 1:3], func=AF.Exp), [t_small], [t_small])
                s.op("dve", lambda v: v.tensor_tensor(out=small[:, 5:6], in0=small[:, 4:5], in1=small[:, 3:4], op=ALU.subtract), [t_small], [t_small])
                s.op("dve", lambda v: v.tensor_scalar(out=small[:, 0:1], in0=small[:, 5:6], scalar1=-lam_init, scalar2=None, op0=ALU.add), [t_small], [t_small])
                s.dma("sp", gcol[:, 0:1], att_subln[l:l + 1, :].rearrange("o c -> c o"), writes=[t_gc], allow_slow_non_contiguous=True)
                s.op("dve", lambda v: v.tensor_scalar(out=gcol[:, 1:2], in0=gcol[:, 0:1], scalar1=1.0 - lam_init, scalar2=None, op0=ALU.mult), [t_gc], [t_gc])
                for hb in range(2):
                    s.op("pool", lambda g, hb=hb: g.memset(vaug[:, hb, :, 128:130], 1.0), [], [t_v[hb]])

                offs = [O_AQ, O_AK, O_AV, O_AZ]

                def proj_gen(h):
                    hb = h % 2
                    for j in range(4):
                        load_w(wbuf[:, hb, j], w_in[l][:, offs[j] + h * 128: offs[j] + (h + 1) * 128], t_w[hb][j])
                    yield
                    for j, dst, td in ((0, qT, t_q), (1, kT, t_k), (3, szT, t_sz)):
                        for tc in range(4):
                            s.op("pe", [lambda pe, k=k: pe.matmul(psA[:], lhsT=wbuf[:, hb, j, k, :], rhs=hT[:, k, tc * 512:(tc + 1) * 512],
                                                                  start=(k == 0), stop=(k == 7)) for k in range(8)],
                                 [t_w[hb][j], t_hT], [t_psA])
                            if j == 0:
                                s.op("dve", lambda v: v.tensor_copy(out=dst[:, hb, tc * 512:(tc + 1) * 512], in_=psA[:]), [t_psA], [td[hb]])
                            elif j == 1:
                                s.op("dve", lambda v: v.tensor_copy(out=dst[:, hb, tc * 512:(tc + 1) * 512], in_=psA[:]), [t_psA], [td[hb]])
                            else:
                                s.op("act", lambda a: a.activation(out=ztmp[:], in_=psA[:], func=AF.Exp, scale=-1.0), [t_psA], [t_zt])
                                s.op("act", lambda a: a.activation(out=ztmp[:], in_=ztmp[:], func=AF.Ln, bias=1.0), [t_zt], [t_zt])
                                s.op("act", lambda a: a.activation(out=ztmp[:], in_=ztmp[:], func=AF.Exp, scale=-1.0), [t_zt], [t_zt])
                                s.op("dve", lambda v: v.tensor_tensor(out=dst[:, hb, tc * 512:(tc + 1) * 512], in0=psA[:], in1=ztmp[:], op=ALU.mult),
                                     [t_psA, t_zt], [td[hb]])
                            yield
                    for tg in range(4):
                        fns = []
                        for u in range(4):
                            t = tg * 4 + u
                            for k in range(8):
                                fns.append(lambda pe, k=k, t=t, u=u: pe.matmul(psA[:, u * 128:(u + 1) * 128], lhsT=hT[:, k, t * 128:(t + 1) * 128],
                                                                              rhs=wbuf[:, hb, 2, k, :], start=(k == 0), stop=(k == 7)))
                        s.op("pe", fns, [t_w[hb][2], t_hT], [t_psA])
                        s.op("dve", lambda v: v.tensor_copy(out=vaug[:, hb, tg * 4:(tg + 1) * 4, 0:128],
                                                            in_=psA[:].rearrange("p (u n) -> p u n", u=4)), [t_psA], [t_v[hb]])
                        yield

                fin_i = [0]

                def fin_stages(h, t, ob):
                    hb = h % 2
                    f = fin_i[0] % NF
                    fin_i[0] += 1
                    tsl = slice(t * 128, (t + 1) * 128)

                    def st1():
                        s.op("dve", lambda v: v.reciprocal(out=rr[:, f, 0:2], in_=psO[ob][:, :, 128]), [t_psO[ob]], [t_rr[f]])
                        s.op("dve", lambda v: v.tensor_tensor(out=rr[:, f, 2:3], in0=rr[:, f, 1:2], in1=small[:, 0:1], op=ALU.mult),
                             [t_rr[f], t_small], [t_rr[f]])
                        s.op("dve", lambda v: v.tensor_scalar(out=osb[:, f, 1, :], in0=psO[ob][:, 1, 0:128], scalar1=rr[:, f, 2:3], scalar2=None, op0=ALU.mult),
                             [t_psO[ob], t_rr[f]], [t_osb[f]])
                        s.op("dve", lambda v: v.scalar_tensor_tensor(out=osb[:, f, 0, :], in0=psO[ob][:, 0, 0:128], scalar=rr[:, f, 0:1],
                                                                      in1=osb[:, f, 1, :], op0=ALU.mult, op1=ALU.add),
                             [t_psO[ob], t_rr[f], t_osb[f]], [t_osb[f]])
                        s.op("dve", lambda v: v.scalar_tensor_tensor(out=osb[:, f, 1, :], in0=osb[:, f, 0, :], scalar=1.0, in1=osb[:, f, 0, :],
                                                                      op0=ALU.mult, op1=ALU.mult, accum_out=fst[:, f, 0:1]),
                             [t_osb[f]], [t_osb[f], t_fst[f]])

                    def st2():
                        s.op("act", lambda a: a.activation(out=fst[:, f, 1:2], in_=fst[:, f, 0:1], func=AF.Ln, bias=EPS, scale=1.0 / 128.0),
                             [t_fst[f]], [t_fst[f]])
                        s.op("act", lambda a: a.activation(out=fst[:, f, 2:3], in_=fst[:, f, 1:2], func=AF.Exp, scale=-0.5),
                             [t_fst[f]], [t_fst[f]])

                    def st3():
                        s.op("dve", lambda v: v.tensor_scalar(out=fan[:, f, :], in0=osb[:, f, 0, :], scalar1=fst[:, f, 2:3], scalar2=None, op0=ALU.mult),
                             [t_osb[f], t_fst[f]], [t_fan[f]])

                    def st4():
                        s.op("pe", lambda pe: pe.transpose(out=psF[:, f * 128:(f + 1) * 128], in_=fan[:, f, :], identity=identb[:]),
                             [t_fan[f], t_cst], [t_psF])

                    def st5():
                        s.op("dve", lambda v: v.scalar_tensor_tensor(out=yT[:, h, tsl], in0=psF[:, f * 128:(f + 1) * 128],
                                                                      scalar=gcol[:, 1:2], in1=szT[:, hb, tsl], op0=ALU.mult, op1=ALU.mult),
                             [t_psF, t_sz[hb], t_gc], [t_yT[h]])
                    return [st1, st2, st3, st4, st5]

                h0 = 0 if "p3" not in skip else 8
                if h0 < 8:
                    for _ in proj_gen(h0):
                        pass
                for h in range(h0, 8):
                    hb = h % 2
                    gen = proj_gen(h + 1) if h + 1 < 8 else iter(())
                    blocks = [(t, c) for t in range(NT) for c in range(t + 1)]
                    pending = []

                    def score(i):
                        t, c = blocks[i]
                        sbk = i % 2
                        diag = (c == t)
                        fns = [lambda pe, m=m: pe.matmul(psS[sbk][:, m, 0:128], lhsT=kT[m * 64:(m + 1) * 64, hb, c * 128:(c + 1) * 128],
                                                         rhs=qT[m * 64:(m + 1) * 64, hb, t * 128:(t + 1) * 128], start=True, stop=not diag)
                               for m in range(2)]
                        if diag:
                            fns += [lambda pe, m=m: pe.matmul(psS[sbk][:, m, 0:128], lhsT=identb[:], rhs=negb[:], start=False, stop=True)
                                    for m in range(2)]
                        s.op("pe", fns, [t_k[hb], t_q[hb], t_cst], [t_psS[sbk]])

                    score(0)
                    for i, (t, c) in enumerate(blocks):
                        sbk = i % 2
                        pb = i % 3
                        ob = t % 2
                        if i + 1 < len(blocks):
                            score(i + 1)
                        bcol = cst[:, C_ALI + h * 16 + (t - c): C_ALI + h * 16 + (t - c) + 1]
                        s.op("act", lambda a: a.activation(out=pt[:, pb, :, :], in_=psS[sbk][:, :, 0:128], func=AF.Exp, bias=bcol, scale=0.125),
                             [t_psS[sbk], t_cst], [t_pt[pb]])
                        while pending and pending[0][0] <= i:
                            pending.pop(0)[1]()
                        if i % 8 == 4:
                            next(gen, None)
                        s.op("pe", [lambda pe, m=m: pe.matmul(psO[ob][:, m, 0:129], lhsT=pt[:, pb, m, :], rhs=vaug[:, hb, c, 0:129],
                                                              start=(c == 0 and m == 0), stop=(c == t and m == 1)) for m in range(2)],
                             [t_pt[pb], t_v[hb]], [t_psO[ob]])
                        if c == t:
                            fnext = fin_i[0] % NF
                            for e_ in [e for e in pending if e[2] == fnext]:
                                pending.remove(e_)
                                e_[1]()
                            for dly, st_ in zip((1, 6, 8, 10, 12), fin_stages(h, t, ob)):
                                pending.append([i + dly, st_, fnext])
                            pending.sort(key=lambda e: e[0])
                    for _ in gen:
                        pass
                    while pending:
                        pending.pop(0)[1]()
                s.barrier()
            if stop == "p3":
                if dbg:
                    for c in range(16):
                        s.dma("pool", dbg_y[:, c, :], yT[:, c, :], reads=[t_yT[c]])
                    for k in range(8):
                        s.dma("pool", dbg_h[:, k, :], hT[:, k, :], reads=[t_hT])
                s.finish("sp")
                print("instructions:", s.n_instr)
                return nc
            with ExitStack() as pes:
                sb = lambda name, shape, dt: pes.enter_context(nc.sbuf_tensor(uq(name), shape, dt))
                ps = lambda name, shape, dt: pes.enter_context(nc.psum_tensor(uq(name), shape, dt))
                ph = {}
                alloc_fin(pes, ph)
                psA = [ps("d_psA%d" % i, [128, 512], F32) for i in range(2)]
                t_psA = [TP(), TP()]
                psB = [ps("d_psB%d" % i, [128, 512], F32) for i in range(2)]
                t_psB = [TP(), TP()]
                psTr = ps("d_psT", [128, 1024], BF16)
                t_psTr = TP()
                psV = ps("d_psV", [128, 4, 128], F32)
                t_psV = TP()
                psW = ps("d_psW", [128, 4, 128], F32)
                t_psW = TP()
                pA = [0]

                wba = sb("d_wba", [128, 8, 8], BF16)
                t_wba = T()
                tok = sb("d_tok", [128, 8, 64], F32)
                t_tok = T()
                prm = sb("d_prm", [128, 16], F32)
                t_prm = T()
                gcolD = sb("d_gcol", [128, 1], F32)
                t_gcD = T()
                cw = sb("d_cw", [128, 3, 4], F32)
                t_cw = T()
                wd = sb("d_w", [128, 5, 8, 128], BF16)
                t_wd = [T() for _ in range(5)]
                raw = sb("d_raw", [128, S + 3], F32)
                t_raw = T()
                cv = sb("d_cv", [128, S], F32)
                t_cv = T()
                qnT = sb("d_qnT", [128, S], BF16)
                knT = sb("d_knT", [128, S], BF16)
                vcT = sb("d_vcT", [128, S], BF16)
                qdT = sb("d_qdT", [128, S], BF16)
                t_qn, t_kn, t_vc, t_qd = T(), T(), T(), T()
                szT = sb("d_szT", [128, S], BF16)
                t_sz = T()
                gcr = sb("d_gcr", [128, S], F32)
                t_gcr = T()
                egl = sb("d_egl", [128, 32], F32)
                t_egl = T()
                sd = sb("d_sd", [128, 2, 512], F32)
                t_sd = [T(), T()]
                tmpD = sb("d_tmpD", [128, 2, 4, 128], F32)
                t_tmpD = T()
                X = sb("d_X", [128, 2, 4, 128], BF16)
                Y = sb("d_Y", [128, 2, 4, 128], BF16)
                R = sb("d_R", [128, 2, 4, 128], BF16)
                t_X, t_Y, t_R = [T(), T()], [T(), T()], [T(), T()]
                aT = sb("d_aT", [128, 2, 4, 128], BF16)
                t_aT = [T(), T()]
                kbg = sb("d_kbg", [128, 4, 128], BF16)
                kdec = sb("d_kdec", [128, 2, 4, 128], BF16)
                vb = sb("d_vb", [128, 4, 128], BF16)
                t_kbg, t_kdec, t_vb = T(), [T(), T()], T()
                usb = sb("d_u", [128, 2, 4, 128], F32)
                t_u = [T(), T()]
                wT = sb("d_wT", [128, 2, 4, 128], BF16)
                t_wT = [T(), T()]
                vnew = sb("d_vnew", [128, 128], BF16)
                t_vnew = T()
                St = sb("d_S", [128, 128], F32)
                Sb = sb("d_Sb", [128, 2, 128], BF16)
                Se = sb("d_Se", [128, 128], F32)
                t_S, t_Sb, t_Se = T(), [T(), T()], T()
                osb = sb("d_o", [128, 2, 128], F32)
                t_osb = [T(), T()]

                def proj_fm(wt, tw, evac):
                    for tc in range(4):
                        b = pA[0] % 2
                        pA[0] += 1
                        s.op("pe", [lambda pe, k=k: pe.matmul(psA[b][:], lhsT=wt[:, k, :], rhs=hT[:, k, tc * 512:(tc + 1) * 512],
                                                              start=(k == 0), stop=(k == 7)) for k in range(8)],
                             [tw, t_hT], [t_psA[b]])
                        evac(psA[b], t_psA[b], tc)

                s.dma("pool", wba[:], w_in[l][:, O_DB:O_DB + 8].rearrange("(k p) n -> p k n", p=128), writes=[t_wba])
                s.dma("sp", prm[:, 0:4], dn_a_log[l:l + 1, :].partition_broadcast(128), writes=[t_prm])
                s.dma("sp", prm[:, 4:8], dn_dt_bias[l:l + 1, :].partition_broadcast(128), writes=[t_prm])
                s.op("act", lambda a: a.activation(out=prm[:, 8:12], in_=prm[:, 0:4], func=AF.Exp), [t_prm], [t_prm])
                s.op("dve", lambda v: v.tensor_scalar(out=prm[:, 8:12], in0=prm[:, 8:12], scalar1=-1.0, scalar2=None, op0=ALU.mult), [t_prm], [t_prm])
                s.dma("sp", gcolD[:, 0:1], dn_norm[l:l + 1, :].rearrange("o c -> c o"), writes=[t_gcD], allow_slow_non_contiguous=True)
                fns = []
                for t in range(NT):
                    for k in range(8):
                        fns.append(lambda pe, k=k, t=t: pe.matmul(psA[0][:, t * 8:(t + 1) * 8], lhsT=hT[:, k, t * 128:(t + 1) * 128],
                                                                  rhs=wba[:, k, :], start=(k == 0), stop=(k == 7)))
                s.op("pe", fns, [t_wba, t_hT], [t_psA[0]])
                pA[0] = 1
                ba = psA[0][:, 0:128].rearrange("p (t c) -> p t c", c=8)
                tk = lambda i: tok[:, i, :].rearrange("p (t c) -> p t c", c=4)
                s.op("act", lambda a: a.activation(out=tk(1), in_=ba[:, :, 0:4], func=AF.Sigmoid), [t_psA[0]], [t_tok])
                for hh in range(4):
                    s.op("act", lambda a, hh=hh: a.activation(out=tk(7)[:, :, hh], in_=ba[:, :, 4 + hh], func=AF.Exp, bias=prm[:, 4 + hh:5 + hh]),
                         [t_psA[0], t_prm], [t_tok])
                s.op("act", lambda a: a.activation(out=tok[:, 7, :], in_=tok[:, 7, :], func=AF.Ln, bias=1.0), [t_tok], [t_tok])
                for hh in range(4):
                    s.op("dve", lambda v, hh=hh: v.tensor_scalar(out=tk(2)[:, :, hh], in0=tk(7)[:, :, hh], scalar1=prm[:, 8 + hh:9 + hh], scalar2=None, op0=ALU.mult),
                         [t_tok, t_prm], [t_tok])
                s.op("pe", lambda pe: pe.matmul(psA[1][:, 0:64], lhsT=cst[:, C_BDI:C_BDI + 128], rhs=tok[:, 2, :], start=True, stop=True),
                     [t_tok, t_cst], [t_psA[1]])
                s.op("pe", lambda pe: pe.matmul(psA[1][:, 64:128], lhsT=cst[:, C_BLK:C_BLK + 128], rhs=tok[:, 2, :], start=True, stop=True),
                     [t_tok, t_cst], [t_psA[1]])
                s.op("dve", lambda v: v.tensor_copy(out=tok[:, 3, :], in_=psA[1][:, 0:64]), [t_psA[1]], [t_tok])
                s.op("dve", lambda v: v.tensor_copy(out=tok[:, 4, :], in_=psA[1][:, 64:128]), [t_psA[1]], [t_tok])
                s.op("act", lambda a: a.activation(out=tok[:, 5, :], in_=tok[:, 3, :], func=AF.Exp), [t_tok], [t_tok])
                s.op("dve", lambda v: v.tensor_tensor(out=tok[:, 5, :], in0=tok[:, 5, :], in1=tok[:, 1, :], op=ALU.mult), [t_tok], [t_tok])
                s.op("dve", lambda v: v.tensor_tensor(out=tok[:, 6, :], in0=tok[:, 4, :], in1=tok[:, 3, :], op=ALU.subtract), [t_tok], [t_tok])
                s.op("act", lambda a: a.activation(out=tok[:, 6, :], in_=tok[:, 6, :], func=AF.Exp), [t_tok], [t_tok])
                s.op("pool", lambda g: g.memset(raw[:, 0:3], 0.0), [], [t_raw])
                s.op("pool", lambda g: g.memset(vnew[:], 0.0), [], [t_vnew])
                col = lambda plane, t, hh: tok[:, plane, t * 4 + hh: t * 4 + hh + 1]
                chk("p4a")

                for h in range(0 if "p4" not in skip else 4, 4):
                    offs = [O_DQ, O_DK, O_DV, O_DZ]
                    for j in range(4):
                        load_w(wd[:, j], w_in[l][:, offs[j] + h * 128: offs[j] + (h + 1) * 128], t_wd[j])
                    load_w(wd[:, 4], w_rep[l][:, h * 128:(h + 1) * 128], t_wd[4])
                    for j in range(3):
                        s.dma("sp", cw[:, j, :], dn_conv[l][:, j * 512 + h * 128: j * 512 + (h + 1) * 128].rearrange("i c -> c i"),
                              writes=[t_cw], allow_slow_non_contiguous=True)
                    proj_fm(wd[:, 3], t_wd[3],
                            lambda p, tp, tc: s.op("act", lambda a: a.activation(out=szT[:, tc * 512:(tc + 1) * 512], in_=p[:], func=AF.Silu), [tp], [t_sz]))
                    proj_fm(wd[:, 4], t_wd[4],
                            lambda p, tp, tc: s.op("act", lambda a: a.activation(out=gcr[:, tc * 512:(tc + 1) * 512], in_=p[:], func=AF.Exp, bias=prm[:, 4 + h:5 + h]),
                                                   [tp, t_prm], [t_gcr]))
                    s.op("act", lambda a: a.activation(out=gcr[:], in_=gcr[:], func=AF.Ln, bias=1.0), [t_gcr], [t_gcr])
                    s.op("dve", lambda v: v.tensor_scalar(out=gcr[:], in0=gcr[:], scalar1=prm[:, 8 + h:9 + h], scalar2=None, op0=ALU.mult), [t_gcr, t_prm], [t_gcr])
                    s.op("dve", lambda v: v.tensor_tensor_scan(out=gcr[:], data0=scanmask[:], data1=gcr[:], initial=0.0, op0=ALU.mult, op1=ALU.add),
                         [t_gcr, t_cst], [t_gcr])
                    s.op("act", lambda a: a.activation(out=egl[:], in_=gcr[:].rearrange("p (n c) -> p n c", c=64)[:, :, 63], func=AF.Exp), [t_gcr], [t_egl])
                    for j in range(3):
                        proj_fm(wd[:, j], t_wd[j],
                                lambda p, tp, tc: s.op("act", lambda a: a.copy(out=raw[:, 3 + tc * 512: 3 + (tc + 1) * 512], in_=p[:]), [tp], [t_raw]))
                        s.op("dve", lambda v: v.tensor_scalar(out=cv[:], in0=raw[:, 3:S + 3], scalar1=cw[:, j, 3:4], scalar2=None, op0=ALU.mult),
                             [t_raw, t_cw], [t_cv])
                        for i in range(3):
                            s.op("dve", lambda v: v.scalar_tensor_tensor(out=cv[:], in0=raw[:, i:S + i], scalar=cw[:, j, i:i + 1], in1=cv[:],
                                                                          op0=ALU.mult, op1=ALU.add),
                                 [t_raw, t_cw, t_cv], [t_cv])
                        s.op("act", lambda a: a.activation(out=cv[:], in_=cv[:], func=AF.Silu), [t_cv], [t_cv])
                        if j == 2:
                            s.op("act", lambda a: a.copy(out=vcT[:], in_=cv[:]), [t_cv], [t_vc])
                            continue
                        s.op("pool", lambda g: g.tensor_tensor(out=raw[:, 3:S + 3], in0=cv[:], in1=cv[:], op=ALU.mult), [t_cv], [t_raw])
                        for tc in range(4):
                            b = pA[0] % 2
                            pA[0] += 1
                            s.op("pe", lambda pe: pe.matmul(psA[b][:], lhsT=ones_f[:], rhs=raw[:, 3 + tc * 512: 3 + (tc + 1) * 512], start=True, stop=True),
                                 [t_raw, t_cst], [t_psA[b]])
                            s.op("act", lambda a: a.activation(out=sd[:, b, :], in_=psA[b][:], func=AF.Ln, bias=EPS, scale=1.0), [t_psA[b]], [t_sd[b]])
                            s.op("act", lambda a: a.activation(out=sd[:, b, :], in_=sd[:, b, :], func=AF.Exp, scale=-0.5), [t_sd[b]], [t_sd[b]])
                            dst, td = (qnT, t_qn) if j == 0 else (knT, t_kn)
                            sc = 128.0 ** -0.5 if j == 0 else 1.0
                            s.op("dve", lambda v: v.scalar_tensor_tensor(out=dst[:, tc * 512:(tc + 1) * 512], in0=cv[:, tc * 512:(tc + 1) * 512], scalar=sc,
                                                                          in1=sd[:, b, :], op0=ALU.mult, op1=ALU.mult),
                                 [t_cv, t_sd[b]], [td])
                    s.op("act", lambda a: a.activation(out=cv[:], in_=gcr[:], func=AF.Exp), [t_gcr], [t_cv])
                    s.op("dve", lambda v: v.tensor_tensor(out=qdT[:], in0=qnT[:], in1=cv[:], op=ALU.mult), [t_qn, t_cv], [t_qd])
                    s.op("dve", lambda v: v.memset(St[:], 0.0), [], [t_S])
                    s.op("dve", lambda v: v.memset(Sb[:], 0.0), [], t_Sb)
                    chk("p4b")

                    sl = lambda t: slice(t * 128, (t + 1) * 128)

                    def stageA(tg):
                        g2 = tg % 2
                        tiles = [tg * 4 + u for u in range(4)]
                        bA = pA[0] % 2
                        pA[0] += 1
                        s.op("pe", [lambda pe, u=u, t=t: pe.matmul(psA[bA][:, u * 128:(u + 1) * 128], lhsT=knT[:, sl(t)], rhs=knT[:, sl(t)], start=True, stop=True)
                                    for u, t in enumerate(tiles)], [t_kn], [t_psA[bA]])
                        yield
                        s.op("pe", [lambda pe, u=u, t=t: pe.matmul(psB[0][:, u * 128:(u + 1) * 128], lhsT=knT[:, sl(t)], rhs=qnT[:, sl(t)], start=True, stop=True)
                                    for u, t in enumerate(tiles)], [t_kn, t_qn], [t_psB[0]])
                        yield
                        for u, t in enumerate(tiles):
                            s.op("dve", lambda v, u=u, t=t: v.tensor_scalar(out=tmpD[:, 1, u, :], in0=gcr[:, sl(t)], scalar1=col(3, t, h), scalar2=None,
                                                                             op0=ALU.subtract), [t_gcr, t_tok], [t_tmpD])
                            yield
                        s.op("dve", lambda v: v.tensor_scalar(out=tmpD[:, 0], in0=tmpD[:, 1], scalar1=0.0, scalar2=None, op0=ALU.max), [t_tmpD], [t_tmpD])
                        s.op("dve", lambda v: v.tensor_scalar(out=tmpD[:, 1], in0=tmpD[:, 1], scalar1=0.0, scalar2=None, op0=ALU.min), [t_tmpD], [t_tmpD])
                        yield
                        s.op("act", lambda a: a.activation(out=tmpD[:, 0], in_=tmpD[:, 0], func=AF.Exp, scale=-1.0), [t_tmpD], [t_tmpD])
                        s.op("act", lambda a: a.activation(out=tmpD[:, 1], in_=tmpD[:, 1], func=AF.Exp), [t_tmpD], [t_tmpD])
                        yield
                        s.op("dve", lambda v: v.tensor_tensor(out=tmpD[:, 0], in0=tmpD[:, 0], in1=bds4[:], op=ALU.mult), [t_tmpD, t_cst], [t_tmpD])
                        s.op("dve", lambda v: v.tensor_tensor(out=tmpD[:, 1], in0=tmpD[:, 1], in1=bdi4[:], op=ALU.mult), [t_tmpD, t_cst], [t_tmpD])
                        yield
                        for u, t in enumerate(tiles):
                            s.op("dve", lambda v, u=u, t=t: v.scalar_tensor_tensor(out=X[:, 0, u, :], in0=psA[bA][:, u * 128:(u + 1) * 128], scalar=col(1, t, h),
                                                                                    in1=tmpD[:, 0, u, :], op0=ALU.mult, op1=ALU.mult),
                                 [t_psA[bA], t_tok, t_tmpD], [t_X[0]])
                            yield
                        s.op("dve", lambda v: v.tensor_tensor(out=aT[:, g2], in0=psB[0][:].rearrange("p (u n) -> p u n", u=4), in1=tmpD[:, 1], op=ALU.mult),
                             [t_psB[0], t_tmpD], [t_aT[g2]])
                        yield
                        s.op("pe", [lambda pe, u=u: pe.transpose(out=psTr[:, u * 128:(u + 1) * 128], in_=X[:, 0, u, :], identity=identb[:]) for u in range(4)],
                             [t_X[0], t_cst], [t_psTr])
                        yield
                        s.op("act", lambda a: a.copy(out=Y[:, 0], in_=psTr[:, 0:512].rearrange("p (u n) -> p u n", u=4)), [t_psTr], [t_Y[0]])
                        s.op("dve", lambda v: v.scalar_tensor_tensor(out=R[:, 0], in0=psTr[:, 0:512].rearrange("p (u n) -> p u n", u=4), scalar=-1.0, in1=ident4[:],
                                                                      op0=ALU.mult, op1=ALU.add),
                             [t_psTr, t_cst], [t_R[0]])
                        yield
                        cur = 0
                        for p in range(1, 6):
                            nxt = 1 - cur
                            if p < 5:
                                s.op("pe", [lambda pe, u=u: pe.matmul(psB[1][:, u * 128:(u + 1) * 128], lhsT=X[:, cur, u, :], rhs=Y[:, cur, u, :], start=True, stop=True)
                                            for u in range(4)], [t_X[cur], t_Y[cur]], [t_psB[1]])
                            bX = pA[0] % 2
                            pA[0] += 1
                            s.op("pe", [lambda pe, u=u: pe.matmul(psA[bX][:, u * 128:(u + 1) * 128], lhsT=Y[:, cur, u, :], rhs=X[:, cur, u, :], start=True, stop=True)
                                        for u in range(4)], [t_X[cur], t_Y[cur]], [t_psA[bX]])
                            yield
                            s.op("dve", lambda v: v.tensor_copy(out=X[:, nxt], in_=psA[bX][:].rearrange("p (u n) -> p u n", u=4)), [t_psA[bX]], [t_X[nxt]])
                            if p < 5:
                                s.op("act", lambda a: a.copy(out=Y[:, nxt], in_=psB[1][:].rearrange("p (u n) -> p u n", u=4)), [t_psB[1]], [t_Y[nxt]])
                            yield
                            s.op("pe", [lambda pe, u=u: pe.matmul(psB[0][:, u * 128:(u + 1) * 128], lhsT=X[:, nxt, u, :], rhs=R[:, cur, u, :], start=True, stop=True)
                                        for u in range(4)], [t_X[nxt], t_R[cur]], [t_psB[0]])
                            yield
                            s.op("dve", lambda v: v.tensor_tensor(out=R[:, nxt], in0=psB[0][:].rearrange("p (u n) -> p u n", u=4), in1=R[:, cur], op=ALU.add),
                                 [t_psB[0], t_R[cur]], [t_R[nxt]])
                            yield
                            cur = nxt
                        TT = R[:, cur]
                        t_TT = t_R[cur]
                        s.op("pe", [lambda pe, u=u, t=t: pe.transpose(out=psTr[:, u * 128:(u + 1) * 128], in_=knT[:, sl(t)], identity=identb[:]) for u, t in enumerate(tiles)],
                             [t_kn, t_cst], [t_psTr])
                        s.op("pe", [lambda pe, u=u, t=t: pe.transpose(out=psTr[:, 512 + u * 128:512 + (u + 1) * 128], in_=vcT[:, sl(t)], identity=identb[:]) for u, t in enumerate(tiles)],
                             [t_vc, t_cst], [t_psTr])
                        yield
                        for u, t in enumerate(tiles):
                            s.op("dve", lambda v, u=u, t=t: v.tensor_scalar(out=kbg[:, u, :], in0=psTr[:, u * 128:(u + 1) * 128], scalar1=col(5, t, h), scalar2=None, op0=ALU.mult),
                                 [t_psTr, t_tok], [t_kbg])
                            s.op("act", lambda a, u=u, t=t: a.mul(out=kdec[:, g2, u, :], in_=psTr[:, u * 128:(u + 1) * 128], mul=col(6, t, h)),
                                 [t_psTr, t_tok], [t_kdec[g2]])
                            s.op("dve", lambda v, u=u, t=t: v.tensor_scalar(out=vb[:, u, :], in0=psTr[:, 512 + u * 128:512 + (u + 1) * 128], scalar1=col(1, t, h), scalar2=None, op0=ALU.mult),
                                 [t_psTr, t_tok], [t_vb])
                            yield
                        bU = pA[0] % 2
                        pA[0] += 1
                        s.op("pe", [lambda pe, u=u: pe.matmul(psA[bU][:, u * 128:(u + 1) * 128], lhsT=TT[:, u, :], rhs=vb[:, u, :], start=True, stop=True) for u in range(4)],
                             [t_TT, t_vb], [t_psA[bU]])
                        yield
                        s.op("act", lambda a: a.copy(out=usb[:, g2], in_=psA[bU][:].rearrange("p (u n) -> p u n", u=4)), [t_psA[bU]], [t_u[g2]])
                        s.op("pe", [lambda pe, u=u: pe.matmul(psB[1][:, u * 128:(u + 1) * 128], lhsT=kbg[:, u, :], rhs=TT[:, u, :], start=True, stop=True) for u in range(4)],
                             [t_TT, t_kbg], [t_psB[1]])
                        yield
                        s.op("dve", lambda v: v.tensor_copy(out=wT[:, g2], in_=psB[1][:].rearrange("p (u n) -> p u n", u=4)), [t_psB[1]], [t_wT[g2]])
                        yield

                    def stageB(tg):
                        g2 = tg % 2
                        tiles = [tg * 4 + u for u in range(4)]
                        for u, t in enumerate(tiles):
                            ob = t % 2
                            for hf in range(2):
                                n = t * 2 + hf
                                sc_, sn_ = n % 2, (n + 1) % 2
                                r = slice(hf * 64, hf * 64 + 64)
                                s.op("act", lambda a: a.mul(out=Se[:], in_=St[:], mul=egl[:, n:n + 1]), [t_S, t_egl], [t_Se])
                                s.op("pe", lambda pe: pe.matmul(psV[:, 0, :], lhsT=wT[:, g2, u, :], rhs=Sb[:, sc_, :], start=True, stop=True), [t_wT[g2], t_Sb[sc_]], [t_psV])
                                yield
                                s.op("dve", lambda v: v.scalar_tensor_tensor(out=vnew[r, :], in0=psV[r, 0, :], scalar=-1.0, in1=usb[r, g2, u, :], op0=ALU.mult, op1=ALU.add),
                                     [t_u[g2], t_psV], [t_vnew])
                                yield
                                s.op("pe", lambda pe: pe.matmul(psV[:, 1, :], lhsT=kdec[r, g2, u, :], rhs=vnew[r, :], start=True, stop=True),
                                     [t_kdec[g2], t_vnew], [t_psV])
                                yield
                                s.op("dve", lambda v: v.tensor_tensor(out=Sb[:, sn_, :], in0=psV[:, 1, :], in1=Se[:], op=ALU.add),
                                     [t_Se, t_psV], [t_Sb[sn_]])
                                s.op("dve", lambda v: v.tensor_tensor(out=St[:], in0=psV[:, 1, :], in1=Se[:], op=ALU.add),
                                     [t_Se, t_psV], [t_S])
                                yield
                                s.op("pe", [lambda pe: pe.matmul(psW[:, hf, :], lhsT=qdT[:, sl(t)], rhs=Sb[:, sc_, :], start=True, stop=False),
                                            lambda pe: pe.matmul(psW[:, hf, :], lhsT=aT[:, g2, u, :], rhs=vnew[:], start=False, stop=True)],
                                     [t_qd, t_Sb[sc_], t_aT[g2], t_vnew], [t_psW])
                                yield
                                s.op("act", lambda a: a.copy(out=osb[r, ob, :], in_=psW[r, hf, :]), [t_psW], [t_osb[ob]])
                                yield
                            finalize_tile(ph, osb[:, ob, :], t_osb[ob], gcolD[:, 0:1], szT[:, sl(t)], t_sz, 8 + h, t, [t_gcD])
                            yield

                    for _ in stageA(0):
                        pass
                    for tg in range(4):
                        gB = stageB(tg)
                        gA = stageA(tg + 1) if tg + 1 < 4 else iter(())
                        doneA = doneB = False
                        while not (doneA and doneB):
                            if not doneB:
                                try:
                                    next(gB)
                                except StopIteration:
                                    doneB = True
                            if not doneA:
                                try:
                                    next(gA)
                                except StopIteration:
                                    doneA = True
                s.barrier()

            if stop == "p4":
                if dbg:
                    for c in range(16):
                        s.dma("pool", dbg_y[:, c, :], yT[:, c, :], reads=[t_yT[c]])
                    for k in range(8):
                        s.dma("pool", dbg_h[:, k, :], hT[:, k, :], reads=[t_hT])
                s.finish("sp")
                print("instructions:", s.n_instr)
                return nc
            with ExitStack() as pes:
                sb = lambda name, shape, dt: pes.enter_context(nc.sbuf_tensor(uq(name), shape, dt))
                ps = lambda name, shape, dt: pes.enter_context(nc.psum_tensor(uq(name), shape, dt))
                ph = {}
                alloc_fin(pes, ph)
                psA = [ps("g_psA%d" % i, [128, 512], F32) for i in range(2)]
                t_psA = [TP(), TP()]
                psB = [ps("g_psB%d" % i, [128, 512], F32) for i in range(2)]
                t_psB = [TP(), TP()]
                psTr = ps("g_psT", [128, 1024], BF16)
                t_psTr = TP()
                psV = ps("g_psV", [128, 4, 128], F32)
                _tv = TP()
                t_psV = [_tv, _tv]
                pA = [0]
                wr = sb("g_wr", [128, 8, 16], BF16)
                t_wr = T()
                grT = sb("g_grT", [16, S], BF16)
                t_gr = T()
                w2f = sb("g_w2f", [16, 256], F32)
                w2 = sb("g_w2", [16, 256], BF16)
                t_w2 = T()
                gbc = sb("g_gb", [128, 2, 2], F32)
                t_gb = T()
                gcolG = sb("g_gcol", [128, 1], F32)
                t_gcG = T()
                wg = sb("g_w", [128, 6, 8, 128], BF16)
                t_wg = [T() for _ in range(6)]
                cl = sb("g_cl", [128, S], F32)
                t_cl = T()
                E = sb("g_E", [128, S], F32)
                Ei = sb("g_Ei", [128, S], F32)
                t_E, t_Ei = T(), T()
                qtT = sb("g_qtT", [128, S], BF16)
                ktT = sb("g_ktT", [128, S], BF16)
                t_qt, t_kt = T(), T()
                vtok = sb("g_v", [128, NT, 256], BF16)
                t_vt = T()
                ktok = sb("g_ktok", [128, NT, 128], BF16)
                t_ktok = T()
                szT = sb("g_szT", [128, 2, S], BF16)
                t_sz = T()
                attT = sb("g_attT", [128, 2, 2, 128], BF16)
                t_att = [T(), T()]
                eD = sb("g_eD", [128, 4, 128], F32)
                t_eD = [T() for _ in range(4)]
                St = sb("g_S", [128, 128], F32)
                Sb = sb("g_Sb", [128, 128], BF16)
                t_S, t_Sb = T(), T()
                osb = sb("g_o", [128, 2, 2, 128], F32)
                t_osb = [[T(), T()], [T(), T()]]

                def proj_fm(wt, tw, evac, m=128):
                    for tc in range(4):
                        b = pA[0] % 2
                        pA[0] += 1
                        s.op("pe", [lambda pe, k=k: pe.matmul(psA[b][0:m, :], lhsT=wt[:, k, 0:m], rhs=hT[:, k, tc * 512:(tc + 1) * 512],
                                                              start=(k == 0), stop=(k == 7)) for k in range(8)],
                             [tw, t_hT], [t_psA[b]])
                        evac(psA[b], t_psA[b], tc)

                s.dma("pool", wr[:], w_in[l][:, O_GR:O_GR + 16].rearrange("(k p) n -> p k n", p=128), writes=[t_wr])
                proj_fm(wr, t_wr, lambda p, tp, tc: s.op("act", lambda a: a.copy(out=grT[:, tc * 512:(tc + 1) * 512], in_=p[0:16, :]), [tp], [t_gr]), m=16)
                s.dma("sp", w2f[:], gla_w2[l], writes=[t_w2])
                s.op("dve", lambda v: v.tensor_copy(out=w2[:], in_=w2f[:]), [t_w2], [t_w2])
                for pr in range(2):
                    s.dma("sp", gbc[:, pr, 0:1], gla_b[l:l + 1, pr * 128:(pr + 1) * 128].rearrange("o c -> c o"), writes=[t_gb], allow_slow_non_contiguous=True)
                s.op("dve", lambda v: v.tensor_scalar(out=gbc[:, :, 1:2], in0=gbc[:, :, 0:1], scalar1=-1.0, scalar2=None, op0=ALU.mult), [t_gb], [t_gb])
                s.dma("sp", gcolG[:, 0:1], gla_norm[l:l + 1, :].rearrange("o c -> c o"), writes=[t_gcG], allow_slow_non_contiguous=True)
                sl = lambda t: slice(t * 128, (t + 1) * 128)

                wo_loaded = [False]
                for pr in range(0 if "p5" not in skip else 2, 2):
                    load_w(wg[:, 0], w_in[l][:, O_GQ + pr * 128: O_GQ + (pr + 1) * 128], t_wg[0])
                    load_w(wg[:, 1], w_in[l][:, O_GK + pr * 128: O_GK + (pr + 1) * 128], t_wg[1])
                    for j in range(2):
                        load_w(wg[:, 2 + j], w_in[l][:, O_GV + pr * 256 + j * 128: O_GV + pr * 256 + (j + 1) * 128], t_wg[2 + j])
                        load_w(wg[:, 4 + j], w_in[l][:, O_GZ + pr * 256 + j * 128: O_GZ + pr * 256 + (j + 1) * 128], t_wg[4 + j])
                    for tc in range(4):
                        b = pA[0] % 2
                        pA[0] += 1
                        s.op("pe", lambda pe: pe.matmul(psA[b][:], lhsT=w2[0:16, pr * 128:(pr + 1) * 128], rhs=grT[0:16, tc * 512:(tc + 1) * 512], start=True, stop=True),
                             [t_w2, t_gr], [t_psA[b]])
                        s.op("act", lambda a: a.activation(out=cl[:, tc * 512:(tc + 1) * 512], in_=psA[b][:], func=AF.Exp, bias=gbc[:, pr, 1:2], scale=-1.0),
                             [t_psA[b], t_gb], [t_cl])
                    s.op("act", lambda a: a.activation(out=cl[:], in_=cl[:], func=AF.Ln, bias=1.0), [t_cl], [t_cl])
                    s.op("dve", lambda v: v.tensor_tensor_scan(out=cl[:], data0=scanmask[:], data1=cl[:], initial=0.0, op0=ALU.mult, op1=ALU.add), [t_cl, t_cst], [t_cl])
                    s.op("act", lambda a: a.activation(out=E[:], in_=cl[:], func=AF.Exp, scale=-1.0 / 16.0), [t_cl], [t_E])
                    s.op("act", lambda a: a.activation(out=Ei[:], in_=cl[:], func=AF.Exp, scale=1.0 / 16.0), [t_cl], [t_Ei])
                    proj_fm(wg[:, 0], t_wg[0],
                            lambda p, tp, tc: s.op("dve", lambda v: v.scalar_tensor_tensor(out=qtT[:, tc * 512:(tc + 1) * 512], in0=p[:], scalar=0.125, in1=E[:, tc * 512:(tc + 1) * 512],
                                                                                            op0=ALU.mult, op1=ALU.mult), [tp, t_E], [t_qt]))
                    proj_fm(wg[:, 1], t_wg[1],
                            lambda p, tp, tc: s.op("dve", lambda v: v.tensor_tensor(out=ktT[:, tc * 512:(tc + 1) * 512], in0=p[:], in1=Ei[:, tc * 512:(tc + 1) * 512], op=ALU.mult),
                                                   [tp, t_Ei], [t_kt]))
                    for j in range(2):
                        proj_fm(wg[:, 4 + j], t_wg[4 + j],
                                lambda p, tp, tc, j=j: s.op("act", lambda a: a.activation(out=szT[:, j, tc * 512:(tc + 1) * 512], in_=p[:], func=AF.Silu), [tp], [t_sz]))
                    for tg in range(8):
                        b = pA[0] % 2
                        pA[0] += 1
                        fns = []
                        for u in range(2):
                            t = tg * 2 + u
                            for j in range(2):
                                for k in range(8):
                                    fns.append(lambda pe, k=k, t=t, u=u, j=j: pe.matmul(psA[b][:, u * 256 + j * 128: u * 256 + (j + 1) * 128], lhsT=hT[:, k, sl(t)],
                                                                                        rhs=wg[:, 2 + j, k, :], start=(k == 0), stop=(k == 7)))
                        s.op("pe", fns, [t_wg[2], t_wg[3], t_hT], [t_psA[b]])
                        s.op("dve", lambda v: v.tensor_copy(out=vtok[:, tg * 2:(tg + 1) * 2, :], in_=psA[b][:].rearrange("p (u n) -> p u n", u=2)), [t_psA[b]], [t_vt])
                    for tg in range(2):
                        s.op("pe", [lambda pe, u=u: pe.transpose(out=psTr[:, u * 128:(u + 1) * 128], in_=ktT[:, sl(tg * 8 + u)], identity=identb[:]) for u in range(8)],
                             [t_kt, t_cst], [t_psTr])
                        s.op("act", lambda a: a.copy(out=ktok[:, tg * 8:(tg + 1) * 8, :], in_=psTr[:].rearrange("p (u n) -> p u n", u=8)), [t_psTr], [t_ktok])
                    s.op("dve", lambda v: v.memset(St[:], 0.0), [], [t_S])
                    s.op("dve", lambda v: v.memset(Sb[:], 0.0), [], [t_Sb])
                    if pr == 1:
                        load_w(hT[:].rearrange("p k s -> p (k s)").rearrange("p (c n) -> p c n", c=16), w_out[l], t_hT)
                        wo_loaded[0] = True
                    for t in range(NT):
                        ab = t % 2
                        bB = t % 2
                        s.op("pe", [lambda pe, hh=hh: pe.matmul(psB[hh][:, 0:128], lhsT=ktT[hh * 64:(hh + 1) * 64, sl(t)],
                                                                rhs=qtT[hh * 64:(hh + 1) * 64, sl(t)], start=True, stop=True) for hh in range(2)],
                             [t_kt, t_qt], [t_psB[0], t_psB[1]])
                        for hh in range(2):
                            s.op("dve", lambda v, hh=hh: v.tensor_tensor(out=attT[:, ab, hh, :], in0=psB[hh][:, 0:128], in1=bdi4[:, 0, :], op=ALU.mult),
                                 [t_psB[hh], t_cst], [t_att[ab]])
                        for hf in range(2):
                            n = t * 2 + hf
                            r = slice(hf * 64, hf * 64 + 64)
                            db = n % 4
                            dA = n % 2
                            s.op("pe", lambda pe: pe.matmul(psA[dA][:, 0:256], lhsT=ktok[r, t, :], rhs=vtok[r, t, :], start=True, stop=True),
                                 [t_ktok, t_vt], [t_psA[dA]])
                            ecol = E[:, n * 64 + 63: n * 64 + 64]
                            s.op("act", lambda a: a.mul(out=eD[0:64, db, :], in_=psA[dA][0:64, 0:128], mul=ecol[0:64, :]), [t_psA[dA], t_E], [t_eD[db]])
                            s.op("act", lambda a: a.mul(out=eD[64:128, db, :], in_=psA[dA][64:128, 128:256], mul=ecol[64:128, :]), [t_psA[dA], t_E], [t_eD[db]])
                            for hh in range(2):
                                hr = slice(hh * 64, hh * 64 + 64)
                                s.op("pe", [lambda pe: pe.matmul(psV[:, hf * 2 + hh, :], lhsT=qtT[hr, sl(t)], rhs=Sb[hr, :], start=True, stop=False),
                                            lambda pe: pe.matmul(psV[:, hf * 2 + hh, :], lhsT=attT[:, ab, hh, :], rhs=vtok[:, t, hh * 128:(hh + 1) * 128], start=False, stop=True)],
                                     [t_qt, t_Sb, t_att[ab], t_vt], [t_psV[hf]])
                                s.op("act", lambda a: a.copy(out=osb[r, ab, hh, :], in_=psV[r, hf * 2 + hh, :]), [t_psV[hf]], [t_osb[ab][hh]])
                            s.op("dve", lambda v: v.scalar_tensor_tensor(out=St[:], in0=St[:], scalar=ecol, in1=eD[:, db, :], op0=ALU.mult, op1=ALU.add),
                                 [t_S, t_E, t_eD[db]], [t_S])
                            s.op("dve", lambda v: v.tensor_copy(out=Sb[:], in_=St[:]), [t_S], [t_Sb])
                        for hh in range(2):
                            finalize_tile(ph, osb[:, ab, hh, :], t_osb[ab][hh], gcolG[:, 0:1], szT[:, hh, sl(t)], t_sz, 12 + pr * 2 + hh, t, [t_gcG])
                s.barrier()

            if stop == "p5":
                if dbg:
                    for c in range(16):
                        s.dma("pool", dbg_y[:, c, :], yT[:, c, :], reads=[t_yT[c]])
                    for k in range(8):
                        s.dma("pool", dbg_h[:, k, :], hT[:, k, :], reads=[t_hT])
                s.finish("sp")
                print("instructions:", s.n_instr)
                return nc
            if dbg and l == 0:
                for c in range(16):
                    s.dma("pool", dbg_y[:, c, :], yT[:, c, :], reads=[t_yT[c]])

            with ExitStack() as pes:
                sb = lambda name, shape, dt: pes.enter_context(nc.sbuf_tensor(uq(name), shape, dt))
                ps = lambda name, shape, dt: pes.enter_context(nc.psum_tensor(uq(name), shape, dt))
                wo = hT[:].rearrange("p k s -> p (k s)").rearrange("p (c n) -> p c n", c=16)
                wgt = sb("o_wg", [128, 8, D], BF16)
                wpp = sb("o_wp", [128, 2, D], BF16)
                t_wo, t_wgt, t_wpp = t_hT, T(), T()
                pTb = sb("o_pT", [128, 2, S], BF16)
                t_pT = T()
                gb2 = sb("o_gb2", [128, D], F32)
                t_gb2 = T()
                xb_ = sb("o_x", [128, 2, D], F32)
                t_x = [T(), T()]
                x1 = sb("o_x1", [128, 2, D], F32)
                t_x1 = [T(), T()]
                x1b = sb("o_x1b", [128, 2, D], BF16)
                t_x1b = [T(), T()]
                x1T = sb("o_x1T", [128, 2, D], BF16)
                t_x1T = [T(), T()]
                junk = sb("o_junk", [128, D], BF16)
                t_j = T()
                st = sb("o_st", [128, 2, 8], F32)
                t_st = [T(), T()]
                psY = [ps("o_psY%d" % i, [128, 1024], F32) for i in range(2)]
                psG = ps("o_psG", [128, 1024], F32)
                psX = ps("o_psX", [128, 1024], BF16)
                t_psY, t_psG, t_psX = [TP(), TP()], TP(), TP()
                tmp2 = sb("o_tmp2", [128, 2, D], F32)
                t_tmp2 = [T(), T()]
                gate2 = sb("o_gate2", [128, 2, D], F32)
                t_gate2 = [T(), T()]
                if not wo_loaded[0]:
                    load_w(wo, w_out[l], t_wo)
                load_w(wgt[:], ple_gate[l], t_wgt)
                load_w(wpp[:], ple_proj[l], t_wpp)
                load_w(pTb[:], pT_in[l], t_pT)
                s.dma("sp", gb1[:], post_gain[l:l + 1, :].partition_broadcast(128), writes=[t_gb1])
                s.dma("sp", gb2[:], ple_norm[l:l + 1, :].partition_broadcast(128), writes=[t_gb2])

                def emit_y(t):
                    b = t % 2
                    tsl = slice(t * 128, (t + 1) * 128)
                    s.dma("sp", xb_[:, b, :], x_src[tsl, :], writes=[t_x[b]])
                    fns = []
                    for nh in range(2):
                        for c in range(16):
                            fns.append(lambda pe, c=c, nh=nh: pe.matmul(psY[b][:, nh * 512:(nh + 1) * 512], lhsT=yT[:, c, tsl], rhs=wo[:, c, nh * 512:(nh + 1) * 512],
                                                                        start=(c == 0), stop=(c == 15)))
                    s.op("pe", fns, t_yT + [t_wo], [t_psY[b]])

                emit_y(0)
                for t in range(NT):
                    b = t % 2
                    tsl = slice(t * 128, (t + 1) * 128)
                    if t + 1 < NT:
                        emit_y(t + 1)
                    s.op("act", lambda a: a.activation(out=junk[:], in_=psY[b][:], func=AF.Square, accum_out=st[:, b, 0:1]), [t_psY[b]], [t_j, t_st[b]])
                    s.op("act", lambda a: a.activation(out=st[:, b, 1:2], in_=st[:, b, 0:1], func=AF.Ln, bias=EPS, scale=1.0 / D), [t_st[b]], [t_st[b]])
                    s.op("act", lambda a: a.activation(out=st[:, b, 2:3], in_=st[:, b, 1:2], func=AF.Exp, scale=-0.5), [t_st[b]], [t_st[b]])
                    s.op("dve", lambda v: v.scalar_tensor_tensor(out=tmp2[:, b, :], in0=psY[b][:], scalar=st[:, b, 2:3], in1=gb1[:], op0=ALU.mult, op1=ALU.mult),
                         [t_psY[b], t_st[b], t_gb1], [t_tmp2[b]])
                    s.op("pool", lambda g: g.tensor_tensor(out=x1[:, b, :], in0=tmp2[:, b, :], in1=xb_[:, b, :], op=ALU.add), [t_tmp2[b], t_x[b]], [t_x1[b]])
                    s.op("act", lambda a: a.copy(out=x1b[:, b, :], in_=x1[:, b, :]), [t_x1[b]], [t_x1b[b]])
                    s.op("pe", [lambda pe, k=k: pe.transpose(out=psX[:, k * 128:(k + 1) * 128], in_=x1b[:, b, k * 128:(k + 1) * 128], identity=identb[:]) for k in range(8)],
                         [t_x1b[b], t_cst], [t_psX])
                    s.op("dve", lambda v: v.tensor_copy(out=x1T[:, b, :], in_=psX[:]), [t_psX], [t_x1T[b]])
                    fns = []
                    for nh in range(2):
                        for k in range(8):
                            fns.append(lambda pe, k=k, nh=nh: pe.matmul(psG[:, nh * 512:(nh + 1) * 512], lhsT=x1T[:, b, k * 128:(k + 1) * 128], rhs=wgt[:, k, nh * 512:(nh + 1) * 512],
                                                                        start=(k == 0), stop=(k == 7)))
                    s.op("pe", fns, [t_x1T[b], t_wgt], [t_psG])
                    s.op("act", lambda a: a.activation(out=gate2[:, b, :], in_=psG[:], func=AF.Sigmoid), [t_psG], [t_gate2[b]])
                    fns = []
                    for nh in range(2):
                        for k in range(2):
                            fns.append(lambda pe, k=k, nh=nh: pe.matmul(psG[:, nh * 512:(nh + 1) * 512], lhsT=pTb[:, k, tsl], rhs=wpp[:, k, nh * 512:(nh + 1) * 512],
                                                                        start=(k == 0), stop=(k == 1)))
                    s.op("pe", fns, [t_pT, t_wpp], [t_psG])
                    s.op("dve", lambda v: v.tensor_tensor(out=tmp2[:, b, :], in0=psG[:], in1=gate2[:, b, :], op=ALU.mult), [t_psG, t_gate2[b]], [t_tmp2[b]])
                    s.op("dve", lambda v: v.scalar_tensor_tensor(out=junk[:], in0=tmp2[:, b, :], scalar=1.0, in1=tmp2[:, b, :], op0=ALU.mult, op1=ALU.mult,
                                                                  accum_out=st[:, b, 4:5]), [t_tmp2[b]], [t_j, t_st[b]])
                    s.op("act", lambda a: a.activation(out=st[:, b, 5:6], in_=st[:, b, 4:5], func=AF.Ln, bias=EPS, scale=1.0 / D), [t_st[b]], [t_st[b]])
                    s.op("act", lambda a: a.activation(out=st[:, b, 6:7], in_=st[:, b, 5:6], func=AF.Exp, scale=-0.5), [t_st[b]], [t_st[b]])
                    s.op("dve", lambda v: v.scalar_tensor_tensor(out=tmp2[:, b, :], in0=tmp2[:, b, :], scalar=st[:, b, 6:7], in1=gb2[:], op0=ALU.mult, op1=ALU.mult),
                         [t_tmp2[b], t_st[b], t_gb2], [t_tmp2[b]])
                    s.op("pool", lambda g: g.tensor_tensor(out=x1[:, b, :], in0=x1[:, b, :], in1=tmp2[:, b, :], op=ALU.add), [t_tmp2[b], t_x1[b]], [t_x1[b]])
                    s.dma("sp", out[tsl, :], x1[:, b, :], reads=[t_x1[b]])
                s.barrier()
            x_src = out
        s.finish("sp")
        print("instructions:", s.n_instr, {k: v for k, v in s.cnt.items() if v})
    return nc


_CACHE = {}


def prep_inputs(inputs):
    f = lambda a: np.ascontiguousarray(np.asarray(a, dtype=np.float32))
    x = f(inputs["x"])
    p = f(inputs["p"])
    w_in = f(inputs["w_in"])
    w_rep = np.ascontiguousarray(np.repeat(w_in[:, :, O_DA:O_DA + 4], 128, axis=2))
    att_l = np.ascontiguousarray(np.stack([f(inputs["att_lq1"]), f(inputs["att_lk1"]), f(inputs["att_lq2"]), f(inputs["att_lk2"])], axis=1))
    shared = {
        "w_in": w_in, "w_rep": w_rep, "w_out": f(inputs["w_out"]), "ple_gate": f(inputs["ple_gate"]),
        "ple_proj": f(inputs["ple_proj"]), "pre_gain": f(inputs["pre_gain"]), "post_gain": f(inputs["post_gain"]),
        "ple_norm": f(inputs["ple_norm"]), "att_l": att_l, "att_subln": f(inputs["att_subln"]),
        "dn_conv": f(inputs["dn_conv"]), "dn_a_log": f(inputs["dn_a_log"]), "dn_dt_bias": f(inputs["dn_dt_bias"]),
        "dn_norm": f(inputs["dn_norm"]), "gla_w2": f(inputs["gla_w2"]), "gla_b": f(inputs["gla_b"]),
        "gla_norm": f(inputs["gla_norm"]), "consts": make_consts(),
    }
    maps = []
    for b in range(x.shape[0]):
        m = dict(shared)
        m["x"] = np.ascontiguousarray(x[b])
        m["pT"] = np.ascontiguousarray(p[:, b].transpose(0, 2, 1))
        maps.append(m)
    return maps


def kernel(**inputs):
    maps = prep_inputs(inputs)
    if "nc" not in _CACHE:
        _CACHE["nc"] = build_program()
    res = run_bass_kernel_spmd(_CACHE["nc"], maps, core_ids=list(range(8)))
    return np.stack([np.asarray(r["out"], dtype=np.float32) for r in res.results], axis=0)
```
